# Optimizing a Trainium2 kernel written in Bass

```python
import jax, jax.numpy as jnp
from jax import lax
import numpy as np

D_MODEL = 2048
BATCH = 2
SEQ = 16384
DEPTH = 1

POOL_WIDTH = D_MODEL // 2
POOL_WINDOWS = (2, 4, 8, 16)
POOL_GROUPS = len(POOL_WINDOWS)
POOL_GROUP_DIM = POOL_WIDTH // POOL_GROUPS
RET_HEADS = 8
RET_QK_DIM = D_MODEL // 16
RET_V_DIM = D_MODEL // 16
RET_QK_WIDTH = RET_HEADS * RET_QK_DIM
RET_V_WIDTH = RET_HEADS * RET_V_DIM
RET_CHUNK = 128
ROPE_BASE = 10000.0
N_GROUPS = 4
EXPERTS_PER_GROUP = 8
N_EXPERTS = N_GROUPS * EXPERTS_PER_GROUP
TOP_K = 2
D_EXPERT = D_MODEL // 2
MOE_BLOCK = 128
N_ADA = 6
EPS = 1e-6
IN_SPLITS = (POOL_WIDTH, RET_QK_WIDTH, RET_QK_WIDTH, RET_V_WIDTH, RET_V_WIDTH, D_MODEL, D_MODEL)
IN_WIDTH = sum(IN_SPLITS)
IN_OFFSETS = tuple(int(v) for v in np.cumsum(IN_SPLITS)[:-1])

kernel_name = "hybrid_pool_retention_hmoe_adaln"


def rms_norm(x, gain):
    xf = x.astype(jnp.float32)
    y = xf * lax.rsqrt(jnp.mean(xf * xf, axis=-1, keepdims=True) + EPS)
    return (y * gain.astype(jnp.float32)).astype(x.dtype)


def modulate(h, shift, scale):
    return h * (1.0 + scale[:, None, :]) + shift[:, None, :]


def rotary(t, positions):
    half = t.shape[-1] // 2
    inv_freq = ROPE_BASE ** (-jnp.arange(half, dtype=jnp.float32) / half)
    ang = positions.astype(jnp.float32)[..., None] * inv_freq
    cos = jnp.cos(ang)[:, :, None, :]
    sin = jnp.sin(ang)[:, :, None, :]
    t = t.astype(jnp.float32)
    t1, t2 = t[..., :half], t[..., half:]
    return jnp.concatenate([t1 * cos - t2 * sin, t2 * cos + t1 * sin], axis=-1)


def pool_mixer(a, w_pool, pool_scale):
    B, S, _ = a.shape
    af = a.astype(jnp.float32).reshape(B, S, POOL_GROUPS, POOL_GROUP_DIM)
    cs = jnp.cumsum(af, axis=1)
    t = jnp.arange(S)
    outs = []
    for g, w in enumerate(POOL_WINDOWS):
        csg = cs[:, :, g]
        lagged = jnp.pad(csg, ((0, 0), (w, 0), (0, 0)))[:, :S]
        count = jnp.minimum(t + 1, w).astype(jnp.float32)[None, :, None]
        pooled = (csg - lagged) / count - af[:, :, g]
        outs.append(jnp.einsum('bsc,cd->bsd', pooled.astype(a.dtype), w_pool[g]))
    return jnp.concatenate(outs, axis=-1) * pool_scale


def retention(q, k, v, g):
    B, S, H, dk = q.shape
    dv = v.shape[-1]
    C = RET_CHUNK
    N = S // C
    log_gamma = jnp.log1p(-jnp.exp2(-5.0 - jnp.arange(H, dtype=jnp.float32)))
    idx = jnp.arange(C, dtype=jnp.float32)
    diff = idx[:, None] - idx[None, :]
    decay_mask = jnp.where(diff >= 0, jnp.exp(log_gamma[:, None, None] * jnp.maximum(diff, 0.0)), 0.0)
    q_decay = jnp.exp(log_gamma[:, None] * (idx + 1.0))
    k_decay = jnp.exp(log_gamma[:, None] * (C - 1.0 - idx))
    chunk_decay = jnp.exp(log_gamma * C)

    def to_chunks(t):
        return t.astype(jnp.float32).reshape(B, N, C, H, -1).transpose(1, 0, 3, 2, 4)

    def step(state, inp):
        qc, kc, vc = inp
        scores = jnp.einsum('bhid,bhjd->bhij', qc, kc) * decay_mask
        o = jnp.einsum('bhij,bhje->bhie', scores, vc)
        o = o + jnp.einsum('bhid,bhde->bhie', qc * q_decay[:, :, None], state)
        state = state * chunk_decay[:, None, None] + jnp.einsum('bhjd,bhje->bhde', kc * k_decay[:, :, None], vc)
        return state, o

    state0 = jnp.zeros((B, H, dk, dv), jnp.float32)
    _, o = lax.scan(step, state0, (to_chunks(q), to_chunks(k), to_chunks(v)))
    o = o.transpose(1, 0, 3, 2, 4).reshape(B, S, H, dv)
    o = o * lax.rsqrt(jnp.mean(o * o, axis=-1, keepdims=True) + EPS)
    o = o.reshape(B, S, H * dv) * jax.nn.silu(g.astype(jnp.float32))
    return o.astype(g.dtype)


def hierarchical_moe(h, w_group, b_group, w_router, b_router, w1, w3, w2):
    B, S, D = h.shape
    xt = h.reshape(-1, D)
    T = xt.shape[0]
    group_logits = jnp.einsum('td,dg->tg', xt, w_group).astype(jnp.float32) + b_group
    group_probs = jax.nn.softmax(group_logits, axis=-1)
    grp = jnp.argmax(group_logits, axis=-1)
    p_grp = jnp.take_along_axis(group_probs, grp[:, None], axis=-1)[:, 0]
    exp_logits = (jnp.einsum('td,de->te', xt, w_router).astype(jnp.float32) + b_router)
    exp_logits = exp_logits.reshape(T, N_GROUPS, EXPERTS_PER_GROUP)
    exp_logits = jnp.take_along_axis(exp_logits, grp[:, None, None], axis=1)[:, 0]
    top_v, top_i = lax.top_k(exp_logits, TOP_K)
    gate_w = jax.nn.softmax(top_v, axis=-1) * p_grp[:, None]
    expert = grp[:, None] * EXPERTS_PER_GROUP + top_i

    flat_e = expert.reshape(-1)
    flat_w = gate_w.reshape(-1)
    flat_tok = jnp.repeat(jnp.arange(T), TOP_K)
    order = jnp.argsort(flat_e)
    e_s, tok_s, w_s = flat_e[order], flat_tok[order], flat_w[order]
    counts = jnp.bincount(flat_e, length=N_EXPERTS)
    padded = (counts + MOE_BLOCK - 1) // MOE_BLOCK * MOE_BLOCK
    start = jnp.cumsum(counts) - counts
    pend = jnp.cumsum(padded)
    pstart = pend - padded
    A = T * TOP_K
    dest = pstart[e_s] + jnp.arange(A) - start[e_s]
    P = (A + MOE_BLOCK - 1) // MOE_BLOCK * MOE_BLOCK + N_EXPERTS * MOE_BLOCK
    nb = P // MOE_BLOCK
    buf = jnp.zeros((P, D), h.dtype).at[dest].set(xt[tok_s])
    block_e = jnp.clip(jnp.searchsorted(pend, jnp.arange(nb) * MOE_BLOCK, side='right'), 0, N_EXPERTS - 1)

    def expert_block(args):
        xb, e = args
        hid = jax.nn.silu(xb @ w1[e]) * (xb @ w3[e])
        return hid @ w2[e]

    yb = lax.map(expert_block, (buf.reshape(nb, MOE_BLOCK, D), block_e)).reshape(P, D)
    y = jnp.zeros((T, D), h.dtype).at[tok_s].add(yb[dest] * w_s[:, None].astype(h.dtype))
    return y.reshape(B, S, D)


def setup_inputs(seed: int = 0) -> dict:
    key = jax.random.key(seed)
    ks = jax.random.split(key, 24)
    f32 = jnp.float32
    D, L = D_MODEL, DEPTH
    nrm = lambda k, shape, fan_in: jax.random.normal(k, shape, f32) * (fan_in ** -0.5)
    return {
        "x": jax.random.normal(ks[0], (BATCH, SEQ, D), f32),
        "c": jax.random.normal(ks[1], (BATCH, D), f32),
        "positions": jnp.broadcast_to(jnp.arange(SEQ, dtype=jnp.int32)[None, :], (BATCH, SEQ)),
        "w_ada": nrm(ks[2], (L, D, N_ADA * D), D) * 0.5,
        "b_ada": jax.random.normal(ks[3], (L, N_ADA * D), f32) * 0.01,
        "norm1_gain": 1.0 + 0.05 * jax.random.normal(ks[4], (L, D), f32),
        "w_in": nrm(ks[5], (L, D, IN_WIDTH), D),
        "w_pool": nrm(ks[6], (L, POOL_GROUPS, POOL_GROUP_DIM, POOL_GROUP_DIM), POOL_GROUP_DIM),
        "pool_scale": 1.0 + 0.05 * jax.random.normal(ks[7], (L, POOL_WIDTH), f32),
        "w_branch_pool": nrm(ks[8], (L, POOL_WIDTH, D), POOL_WIDTH),
        "w_branch_ret": nrm(ks[9], (L, RET_V_WIDTH, D), RET_V_WIDTH),
        "w_out": nrm(ks[10], (L, D, D), D),
        "norm2_gain": 1.0 + 0.05 * jax.random.normal(ks[11], (L, D), f32),
        "w_group": nrm(ks[12], (L, D, N_GROUPS), D),
        "b_group": jax.random.normal(ks[13], (L, N_GROUPS), f32) * 0.01,
        "w_router": nrm(ks[14], (L, D, N_EXPERTS), D),
        "b_router": jax.random.normal(ks[15], (L, N_EXPERTS), f32) * 0.01,
        "w1": nrm(ks[16], (L, N_EXPERTS, D, D_EXPERT), D),
        "w3": nrm(ks[17], (L, N_EXPERTS, D, D_EXPERT), D),
        "w2": nrm(ks[18], (L, N_EXPERTS, D_EXPERT, D), D_EXPERT),
        "final_gain": 1.0 + 0.05 * jax.random.normal(ks[19], (D,), f32),
    }


def reference(x, c, positions, w_ada, b_ada, norm1_gain, w_in, w_pool, pool_scale,
              w_branch_pool, w_branch_ret, w_out, norm2_gain, w_group, b_group,
              w_router, b_router, w1, w3, w2, final_gain):
    B, S, D = x.shape
    c_act = jax.nn.silu(c)
    for l in range(DEPTH):
        mod = (c_act @ w_ada[l] + b_ada[l]).reshape(B, N_ADA, D)
        shift1, scale1, gate1, shift2, scale2, gate2 = [mod[:, i] for i in range(N_ADA)]

        h = modulate(rms_norm(x, norm1_gain[l]), shift1, scale1)
        proj = jnp.einsum('bsd,de->bse', h, w_in[l])
        a, q, k, v, rg, ga, gb = jnp.split(proj, IN_OFFSETS, axis=-1)
        q = rotary(q.reshape(B, S, RET_HEADS, RET_QK_DIM), positions)
        k = rotary(k.reshape(B, S, RET_HEADS, RET_QK_DIM), positions) * (RET_QK_DIM ** -0.5)
        v = v.reshape(B, S, RET_HEADS, RET_V_DIM)
        y_pool = jnp.einsum('bsc,cd->bsd', pool_mixer(a, w_pool[l], pool_scale[l]), w_branch_pool[l])
        y_ret = jnp.einsum('bsc,cd->bsd', retention(q, k, v, rg), w_branch_ret[l])
        merged = jax.nn.sigmoid(ga) * y_pool + jax.nn.sigmoid(gb) * y_ret
        x = x + gate1[:, None, :] * jnp.einsum('bsd,de->bse', merged, w_out[l])

        h2 = modulate(rms_norm(x, norm2_gain[l]), shift2, scale2)
        y_moe = hierarchical_moe(h2, w_group[l], b_group[l], w_router[l], b_router[l], w1[l], w3[l], w2[l])
        x = x + gate2[:, None, :] * y_moe
    return rms_norm(x, final_gain)
```

```python
import math
from contextlib import ExitStack

import numpy as np
import concourse.bass as bass
import concourse.mybir as mybir
from concourse.bass_utils import run_bass_kernel_spmd

F32 = mybir.dt.float32
BF16 = mybir.dt.bfloat16
I32 = mybir.dt.int32
AF = mybir.ActivationFunctionType
ALU = mybir.AluOpType
AX = mybir.AxisListType

ENGS = ("pe", "act", "dve", "pool", "sp")


class Buf:
    __slots__ = ("name", "writers", "readers", "prev_readers")

    def __init__(self, name):
        self.name = name
        self.writers = []
        self.readers = []
        self.prev_readers = []


class Op:
    __slots__ = ("eng", "fn", "deps", "is_dma", "sem", "val", "sig", "sigidx", "group", "done", "pos")

    def __init__(self, eng, fn, is_dma):
        self.eng = eng
        self.fn = fn
        self.deps = {}
        self.is_dma = is_dma
        self.sem = None
        self.val = 0
        self.sig = False
        self.sigidx = 0
        self.group = None
        self.done = False
        self.pos = 0


class Prog:
    def __init__(self, nc, es):
        self.nc = nc
        self.es = es
        self.streams = {e: [] for e in ENGS}
        self.eng_sem = {e: es.enter_context(nc.semaphore("prog_" + e)) for e in ("pe", "act", "dve", "pool")}
        self.eng_cnt = {e: 0 for e in ("pe", "act", "dve", "pool")}
        self.done_sem = es.enter_context(nc.semaphore("phase_done"))
        self.done_cnt = 0
        self.dma_pool = {"sp": [], "pool": []}
        self.dma_keys = {}
        self.dma_used = {"sp": 0, "pool": 0}
        self.group_ops = {}

    def _track(self, op, reads, writes):
        for b in reads:
            for w in b.writers:
                op.deps[w] = True
            b.readers.append(op)
        for b in writes:
            for r in b.readers:
                if r is not op:
                    op.deps.setdefault(r, False)
            for r in b.prev_readers:
                if r is not op:
                    op.deps.setdefault(r, False)
            for w in b.writers:
                op.deps.setdefault(w, False)
            if b.readers:
                b.prev_readers = b.readers
                b.readers = []
                b.writers = [op]
            else:
                b.writers.append(op)

    def op(self, eng, fn, reads=(), writes=()):
        o = Op(eng, fn, False)
        self._track(o, reads, writes)
        o.pos = len(self.streams[eng])
        self.streams[eng].append(o)
        return o

    def _dma_sem(self, key, queue):
        k = (queue, key)
        if k not in self.dma_keys:
            idx = self.dma_used[queue]
            self.dma_used[queue] += 1
            pool = self.dma_pool[queue]
            if idx >= len(pool):
                s = self.es.enter_context(self.nc.semaphore("dma_%s%d" % (queue, idx)))
                pool.append([s, 0])
            self.dma_keys[k] = pool[idx]
        return self.dma_keys[k]

    def dma(self, queue, fn, reads=(), writes=(), key=None, group=None):
        o = Op(queue, fn, True)
        self._track(o, reads, writes)
        ent = self._dma_sem(("g", group) if group is not None else key, queue)
        ent[1] += 1
        o.sem = ent[0]
        o.val = 16 * ent[1]
        if group is not None:
            o.group = group
            self.group_ops.setdefault(group, []).append(o)
        self.streams[queue].append(o)
        return o

    def emit(self):
        nc = self.nc
        for ops in self.group_ops.values():
            final = max(o.val for o in ops)
            for o in ops:
                o.val = final
        def needed(o, d, is_raw):
            if d.done:
                return False
            if d.group is not None and d.group == o.group:
                return False
            if d.is_dma or o.is_dma:
                return True
            if d.eng != o.eng:
                return True
            return d.eng != "pe"
        for e in ENGS:
            for o in self.streams[e]:
                latest = {}
                for d, is_raw in o.deps.items():
                    if not d.is_dma and needed(o, d, is_raw):
                        if d.eng not in latest or latest[d.eng].pos < d.pos:
                            latest[d.eng] = d
                for d in latest.values():
                    d.sig = True
        last = {}
        for e in ("pe", "act", "dve", "pool"):
            for o in reversed(self.streams[e]):
                if not o.is_dma:
                    o.sig = True
                    last[e] = o
                    break
        for e in ("pe", "act", "dve", "pool"):
            c = self.eng_cnt[e]
            for o in self.streams[e]:
                if not o.is_dma and o.sig:
                    c += 1
                    o.sigidx = c
                    o.sem = self.eng_sem[e]
                    o.val = c
            self.eng_cnt[e] = c
        self.done_cnt += 1
        done_val = self.done_cnt
        final_dma = [(ent[0], 16 * ent[1]) for q in ("sp", "pool") for ent in self.dma_pool[q] if ent[1] > 0]
        final_eng = [(self.eng_sem[e], self.eng_cnt[e]) for e in ("pe", "act", "dve", "pool") if self.eng_cnt[e] > 0]
        streams = self.streams
        done_sem = self.done_sem

        def run(eng_name, eng):
            waited = {}
            for o in streams[eng_name]:
                req = {}
                latest = {}
                for d, is_raw in o.deps.items():
                    if not needed(o, d, is_raw):
                        continue
                    if not d.is_dma:
                        if d.eng not in latest or latest[d.eng].pos < d.pos:
                            latest[d.eng] = d
                        continue
                    k = id(d.sem)
                    if k not in req or req[k][1] < d.val:
                        req[k] = (d.sem, d.val)
                for d in latest.values():
                    k = id(d.sem)
                    if k not in req or req[k][1] < d.val:
                        req[k] = (d.sem, d.val)
                for k, (sem, val) in req.items():
                    if waited.get(k, -1) < val:
                        eng.wait_ge(sem, val)
                        waited[k] = val
                ins = o.fn(eng)
                if o.is_dma:
                    ins.then_inc(o.sem, 16)
                elif o.sig:
                    ins.then_inc(o.sem, 1)
            if eng_name == "sp":
                for s, v in final_eng + final_dma:
                    eng.wait_ge(s, v)
                eng.sem_inc(done_sem, 1)
            else:
                eng.wait_ge(done_sem, done_val)

        with nc.Block() as block:
            @block.tensor
            def _(eng):
                run("pe", eng)

            @block.scalar
            def _(eng):
                run("act", eng)

            @block.vector
            def _(eng):
                run("dve", eng)

            @block.gpsimd
            def _(eng):
                run("pool", eng)

            @block.sync
            def _(eng):
                run("sp", eng)
        for e in ENGS:
            for o in self.streams[e]:
                o.done = True
                o.fn = None
        self.streams = {e: [] for e in ENGS}
        self.dma_keys = {}
        self.dma_used = {"sp": 0, "pool": 0}
        self.group_ops = {}


D = 2048
KC = 16
H = 8
NG = 4
F = 1024
FC = 8
IN_W = 9216
OFF_A, OFF_Q, OFF_K, OFF_V, OFF_RG, OFF_GA, OFF_GB = 0, 1024, 2048, 3072, 4096, 5120, 7168
EPS = 1e-6
PI_LO = 3.141592
TWO_PI = 2.0 * math.pi
CW1 = 6.28125
CW2 = TWO_PI - CW1
POOL_WINDOWS = (2, 4, 8, 16)


class Cfg:
    def __init__(self, nmain=32, npre=96, tch=4, epg=8, cap_blocks=3, nwin=1):
        self.nmain, self.npre, self.tch, self.epg, self.nb, self.nwin = nmain, npre, tch, epg, cap_blocks, nwin
        self.ne = NG * epg
        self.wcap = cap_blocks * 128
        self.cap = cap_blocks * 128 * nwin
        assert nwin == 1
        self.ns = (2 * 128 * nmain) // self.cap
        self.rows = (self.ne + self.ns) * self.cap
        self.nrt = NG + self.ne
        assert nmain % tch == 0


class T:
    __slots__ = ("h", "b")

    def __init__(self, h, name):
        self.h = h
        self.b = Buf(name)

    def __getitem__(self, k):
        return self.h[k]


class Ring:
    def __init__(self, items):
        self.items = items
        self.i = 0

    def next(self):
        t = self.items[self.i % len(self.items)]
        self.i += 1
        return t


def _bufs(lst):
    return [x.b if isinstance(x, T) else x for x in lst]


class K:
    def __init__(self, cfg, debug=False, stop_after=9):
        self.cfg = cfg
        self.debug = debug
        self.stop_after = stop_after
        self.nc = bass.Bass("TRN2", target_bir_lowering=False)
        self.dram = {}
        self.dbuf = {}

    def din(self, name, shape, dt=F32):
        self.dram[name] = self.nc.dram_tensor(name, list(shape), dt, kind="ExternalInput").ap()
        return self.dram[name]

    def dout(self, name, shape, dt=F32):
        self.dram[name] = self.nc.dram_tensor(name, list(shape), dt, kind="ExternalOutput").ap()
        return self.dram[name]

    def dscr(self, name, shape, dt):
        self.dram[name] = self.nc.dram_tensor(name, list(shape), dt, kind="Internal").ap()
        return self.dram[name]

    def op(self, eng, fn, reads=(), writes=()):
        return self.P.op(eng, fn, _bufs(reads), _bufs(writes))

    def dma(self, q, fn, reads=(), writes=(), key=None, group=None):
        if isinstance(key, T):
            key = key.b
        return self.P.dma(q, fn, _bufs(reads), _bufs(writes), key=key, group=group)

    def load(self, q, dst, dst_ap, src_ap, reads=(), group=None):
        return self.dma(q, lambda e: e.dma_start(out=dst_ap, in_=src_ap), reads=reads, writes=[dst], key=dst, group=group)

    def store(self, q, src, dst_ap, src_ap, writes=()):
        return self.dma(q, lambda e: e.dma_start(out=dst_ap, in_=src_ap), reads=[src], writes=writes, key=("st", id(src.b)))

    def build(self):
        cfg, nc = self.cfg, self.nc
        NM, NP_, NE, C, NRT = cfg.nmain, cfg.npre, cfg.ne, cfg.cap, cfg.nrt
        d = self.dram
        self.din("x_main", [NM * 128, D])
        self.din("x_pre", [NP_ * 128, D])
        self.din("pos_main", [128, NM], I32)
        self.din("pos_pre", [128, NP_], I32)
        self.din("flags_pre", [128, NP_])
        self.din("c_col", [128, KC])
        self.din("b_ada_bc", [128, 6 * D])
        self.din("n1g_fm", [128, KC])
        self.din("n2g_fm", [128, KC])
        self.din("fgain_bc", [128, D])
        self.din("pscale_fm", [128, 8])
        self.din("brt_bc", [128, NRT])
        self.din("w_ada", [D, 6 * D])
        self.din("w_in", [D, IN_W])
        self.din("w_pool", [4, 256, 256])
        self.din("w_bp", [1024, D])
        self.din("w_br", [1024, D])
        self.din("w_out", [D, D])
        self.din("w_rt", [D, NRT])
        self.din("w1", [NE * 512, KC * 256])
        self.din("w3", [NE * 512, KC * 256])
        self.din("w2", [NE * 512, FC * 512])
        self.din("iota_p", [128, 1])
        self.din("ident_f", [128, 128])
        self.din("bands", [128, 16, 128])
        self.din("causalT", [128, 128])
        self.din("qscaleT", [128, H, 128])
        self.din("kuscaleT", [128, H, 128])
        self.din("kdscale", [128, H])
        self.din("tri_u", [128, 128])
        self.din("iota_e", [128, NE])
        self.din("invf_bc", [128, 64])
        self.dout("out", [NM * 128, D])
        self.dscr("w_out_g", [D, D], BF16)
        self.dscr("w_in_b", [D, IN_W], BF16)
        self.dscr("w_bp_b", [1024, D], BF16)
        self.dscr("w_br_b", [1024, D], BF16)
        self.dscr("gate2_scr", [128, D], F32)
        self.dscr("x1_scr", [NM * 128, D], F32)
        self.dscr("xs_scr", [cfg.rows, D], BF16)
        self.dscr("ys_scr", [cfg.rows, D], F32)
        if self.debug:
            self.dout("dbg_x1", [NM * 128, D])
            self.dout("dbg_dest", [128, NM, 2], I32)
            self.dout("dbg_gw", [128, NM, 2])
            self.dout("dbg_mod", [128, 4, KC])
            self.dout("dbg_state", [128, H, 128])
        for n in ("w_out_g", "gate2_scr", "xs_scr", "ys_scr", "w_in_b", "w_bp_b", "w_br_b"):
            self.dbuf[n] = Buf(n)
        self.x1_bufs = [Buf("x1scr%d" % c) for c in range(NM)]
        self.dest_bufs = [Buf("dest%d" % c) for c in range(NM)]
        self.gw_bufs = [Buf("gw%d" % c) for c in range(NM)]
        self.rt_bufs = [Buf("rt%d" % c) for c in range(NM)]
        gam = [1.0 - 2.0 ** (-5.0 - h) for h in range(H)]
        self.cd = [float(np.float32(g ** 128)) for g in gam]

        with ExitStack() as es:
            self.es = es
            self.P = Prog(nc, es)
            P = self.P
            sbp = lambda n, sh, dt=F32: T(es.enter_context(nc.sbuf_tensor(n, list(sh), dt)), n)
            self.g1eff = sbp("g1eff", [128, KC]); self.shift1 = sbp("shift1", [128, KC])
            self.g2eff = sbp("g2eff", [128, KC]); self.shift2 = sbp("shift2", [128, KC])
            self.ident_f = sbp("ident_f_sb", [128, 128]); self.ident_b = sbp("ident_b_sb", [128, 128], BF16)
            self.epsb = sbp("epsb", [128, 1])
            self.state = sbp("state", [128, H, 128]); self.state_bf = sbp("state_bf", [128, H, 128], BF16)
            self.a_prev0 = sbp("a_prev0", [128, 1024], BF16)
            self.dest_all = sbp("dest_all", [128, NM, 2], I32); self.gw_all = sbp("gw_all", [128, NM, 2])
            self.destf_all = sbp("destf_all", [128, NM, 2]); self.rank_all = sbp("rank_all", [128, NM, 2]); self.eidx_all = sbp("eidx_all", [128, NM, 2])
            self.rstd2_all = sbp("rstd2_all", [128, NM]); self.slot_idx = sbp("slot_idx", [128, 4 * max(cfg.ns, 1)], I32)
            self.load("sp", self.ident_f, self.ident_f[:, :], d["ident_f"][:, :], group="c0")
            self.load("pool", self.ident_b, self.ident_b[:, :], d["ident_f"][:, :], group="c0p")
            self.op("dve", lambda e: e.memset(self.epsb[:, :], EPS), writes=[self.epsb])
            self.op("dve", lambda e: e.memset(self.state[:, :, :], 0.0), writes=[self.state])
            for i, ph in enumerate((self.phase0, self.phase1, self.phase2, self.phase3, self.phase4)):
                if i <= self.stop_after:
                    ph()
        return nc

    def bound_reg(self, e, val):
        key = (self.P.done_cnt, val)
        if getattr(self, "_breg_key", None) != key:
            r = e.alloc_register("idma_bound%d" % self.P.done_cnt)
            e.reg_mov(r, val)
            self._breg_key = key
            self._breg = r
        return self._breg

    def rstd_from_ssq(self, ssq_ap, ssq_t, rt, rstd, scale):
        self.op("act", lambda e: e.activation(out=rt[:, :], in_=ssq_ap, func=AF.Sqrt, scale=scale, bias=self.epsb[:, 0:1]),
                reads=[ssq_t, self.epsb], writes=[rt])
        self.op("dve", lambda e: e.reciprocal(out=rstd[:, :], in_=rt[:, :]), reads=[rt], writes=[rstd])

    def norm_transpose(self, x_ap, xt, junk, small, xnb, ptrs, hT, col0, q="sp"):
        ssq, rt, rstd = small
        self.load(q, xt, xt[:, :], x_ap)
        self.op("act", lambda e: e.activation(out=junk[:, :], in_=xt[:, :], func=AF.Square, accum_out=ssq[:, 0:1]),
                reads=[xt], writes=[junk, ssq])
        self.rstd_from_ssq(ssq[:, 0:1], ssq, rt, rstd, 1.0 / D)
        self.op("dve", lambda e: e.tensor_scalar(out=xnb[:, :], in0=xt[:, :], scalar1=rstd[:, 0:1], scalar2=None, op0=ALU.mult),
                reads=[xt, rstd], writes=[xnb])
        for kc in range(KC):
            pt = ptrs[kc // 8]
            self.op("pe", (lambda kc, pt: lambda e: e.transpose(out=pt[:, (kc % 8) * 128:(kc % 8 + 1) * 128], in_=xnb[:, kc * 128:(kc + 1) * 128], identity=self.ident_b[:, :]))(kc, pt),
                    reads=[xnb, self.ident_b], writes=[pt])
        self.evac_mod(ptrs, hT, col0, self.g1eff, self.shift1)

    def evac_mod(self, ptrs, dst, col0, geff, shift):
        for kc in range(KC):
            pt = ptrs[kc // 8]
            src = pt[:, (kc % 8) * 128:(kc % 8 + 1) * 128]
            out = dst[:, kc, col0:col0 + 128]
            if kc % 2 == 0:
                self.op("act", (lambda out, src, kc: lambda e: e.activation(out=out, in_=src, func=AF.Identity, scale=geff[:, kc:kc + 1], bias=shift[:, kc:kc + 1]))(out, src, kc),
                        reads=[pt, geff, shift], writes=[dst])
            else:
                self.op("dve", (lambda out, src, kc: lambda e: e.tensor_scalar(out=out, in0=src, scalar1=geff[:, kc:kc + 1], scalar2=shift[:, kc:kc + 1], op0=ALU.mult, op1=ALU.add))(out, src, kc),
                        reads=[pt, geff, shift], writes=[dst])

    def trig(self, pos_f, col, tw, cos_t, sin_t):
        invf = self.invf
        ang, y, ki, kf, ra, r1, rs, c1, tt, c2 = (tw[n] for n in ("ang", "y", "ki", "kf", "ra", "r1", "rs", "c1", "tt", "c2"))
        o = self.op
        o("dve", lambda e: e.tensor_scalar(out=ang[:, :], in0=invf[:, :], scalar1=pos_f[:, col:col + 1], scalar2=None, op0=ALU.mult), reads=[invf, pos_f], writes=[ang])
        o("dve", lambda e: e.tensor_scalar(out=y[:, :], in0=ang[:, :], scalar1=1.0 / TWO_PI, scalar2=None, op0=ALU.mult), reads=[ang], writes=[y])
        o("dve", lambda e: e.tensor_copy(out=ki[:, :], in_=y[:, :]), reads=[y], writes=[ki])
        o("dve", lambda e: e.tensor_copy(out=kf[:, :], in_=ki[:, :]), reads=[ki], writes=[kf])
        o("dve", lambda e: e.scalar_tensor_tensor(out=ra[:, :], in0=kf[:, :], scalar=-CW1, in1=ang[:, :], op0=ALU.mult, op1=ALU.add), reads=[kf, ang], writes=[ra])
        o("dve", lambda e: e.scalar_tensor_tensor(out=r1[:, :], in0=kf[:, :], scalar=-CW2, in1=ra[:, :], op0=ALU.mult, op1=ALU.add), reads=[kf, ra], writes=[r1])
        o("dve", lambda e: e.tensor_scalar(out=rs[:, :], in0=r1[:, :], scalar1=PI_LO, scalar2=-PI_LO, op0=ALU.min, op1=ALU.max), reads=[r1], writes=[rs])
        o("act", lambda e: e.activation(out=sin_t[:, :], in_=rs[:, :], func=AF.Sin), reads=[rs], writes=[sin_t])
        o("dve", lambda e: e.tensor_scalar(out=c1[:, :], in0=r1[:, :], scalar1=math.pi / 2, scalar2=None, op0=ALU.add), reads=[r1], writes=[c1])
        o("dve", lambda e: e.tensor_scalar(out=tt[:, :], in0=c1[:, :], scalar1=math.pi, scalar2=-TWO_PI, op0=ALU.is_gt, op1=ALU.mult), reads=[c1], writes=[tt])
        o("dve", lambda e: e.tensor_tensor(out=c2[:, :], in0=c1[:, :], in1=tt[:, :], op=ALU.add), reads=[c1, tt], writes=[c2])
        o("dve", lambda e: e.tensor_scalar(out=c2[:, :], in0=c2[:, :], scalar1=PI_LO, scalar2=-PI_LO, op0=ALU.min, op1=ALU.max), reads=[c2], writes=[c2])
        o("act", lambda e: e.activation(out=cos_t[:, :], in_=c2[:, :], func=AF.Sin), reads=[c2], writes=[cos_t])

    def rotary(self, pt, nh, cos_t, sin_t, tring, dst, dcol0):
        pv = pt[:, 0:nh * 128].rearrange("p (h t f) -> p h t f", h=nh, t=2)
        dv = dst[:, dcol0:dcol0 + nh * 128].rearrange("p (h t f) -> p h t f", h=nh, t=2)
        cb = cos_t[:, :].unsqueeze(1).to_broadcast([128, nh, 64])
        sb_ = sin_t[:, :].unsqueeze(1).to_broadcast([128, nh, 64])
        o = self.op
        for half in (0, 1):
            tA = tring.next()
            tB = tring.next()
            a3 = tA[:, 0:nh * 64].rearrange("p (h f) -> p h f", h=nh)
            b3 = tB[:, 0:nh * 64].rearrange("p (h f) -> p h f", h=nh)
            o("dve", (lambda half, a3: lambda e: e.tensor_tensor(out=a3, in0=pv[:, :, half, :], in1=cb, op=ALU.mult))(half, a3), reads=[pt, cos_t], writes=[tA])
            o("dve", (lambda half, b3: lambda e: e.tensor_tensor(out=b3, in0=pv[:, :, 1 - half, :], in1=sb_, op=ALU.mult))(half, b3), reads=[pt, sin_t], writes=[tB])
            opc = ALU.subtract if half == 0 else ALU.add
            o("pool", (lambda half, opc, a3, b3: lambda e: e.tensor_tensor(out=dv[:, :, half, :], in0=a3, in1=b3, op=opc))(half, opc, a3, b3), reads=[tA, tB], writes=[dst])

    def phase0(self):
        nc, d, o = self.nc, self.dram, self.op
        with ExitStack() as pes:
            sb = lambda n, sh, dt=F32: T(pes.enter_context(nc.sbuf_tensor("p0_" + n, list(sh), dt)), n)
            ps = lambda n, sh, dt=F32: T(pes.enter_context(nc.psum_tensor("p0_" + n, list(sh), dt)), n)
            c_col = sb("c_col", [128, KC]); c_act = sb("c_act", [128, KC])
            ones_f = sb("ones_f", [128, 128]); cbc = sb("cbc", [128, KC, 128])
            mod_bc = sb("mod_bc", [128, 6 * D])
            n1g = sb("n1g", [128, KC]); n2g = sb("n2g", [128, KC])
            self.load("sp", c_col, c_col[:, :], d["c_col"][:, :])
            self.load("sp", n1g, n1g[:, :], d["n1g_fm"][:, :])
            self.load("sp", n2g, n2g[:, :], d["n2g_fm"][:, :])
            o("act", lambda e: e.activation(out=c_act[:, :], in_=c_col[:, :], func=AF.Silu), reads=[c_col], writes=[c_act])
            o("pool", lambda e: e.memset(ones_f[:, :], 1.0), writes=[ones_f])
            for kc in range(KC):
                o("dve", (lambda kc: lambda e: e.tensor_scalar(out=cbc[:, kc, :], in0=ones_f[:, :], scalar1=c_act[:, kc:kc + 1], scalar2=None, op0=ALU.mult))(kc),
                  reads=[ones_f, c_act], writes=[cbc])
            wring = Ring([sb("wada%d" % i, [128, KC, 256]) for i in range(2)])
            bring = Ring([sb("bada%d" % i, [128, 256]) for i in range(2)])
            pring = Ring([ps("pada%d" % i, [128, 512]) for i in range(2)])
            for j in range(6 * D // 256):
                w = wring.next(); b = bring.next(); pt = pring.next()
                self.load("sp", w, w[:, :, :], d["w_ada"][:, j * 256:(j + 1) * 256].rearrange("(k p) n -> p k n", p=128))
                self.load("sp", b, b[:, :], d["b_ada_bc"][:, j * 256:(j + 1) * 256])
                for kc in range(KC):
                    o("pe", (lambda kc, w, pt: lambda e: e.matmul(out=pt[:, 0:256], lhsT=cbc[:, kc, :], rhs=w[:, kc, :], start=(kc == 0), stop=(kc == KC - 1)))(kc, w, pt),
                      reads=[cbc, w], writes=[pt])
                o("dve", (lambda j, pt, b: lambda e: e.tensor_tensor(out=mod_bc[:, j * 256:(j + 1) * 256], in0=pt[:, 0:256], in1=b[:, :], op=ALU.add))(j, pt, b),
                  reads=[pt, b], writes=[mod_bc])
            tmp = sb("diag_tmp", [128, KC, 128])
            fm = [sb("fm%d" % i, [128, KC]) for i in range(6)]
            identb3 = self.ident_f[:, :].unsqueeze(1).to_broadcast([128, KC, 128])
            for i in (0, 1, 3, 4):
                o("dve", (lambda i: lambda e: e.tensor_tensor(out=tmp[:, :, :], in0=mod_bc[:, i * D:(i + 1) * D].rearrange("p (k n) -> p k n", k=KC), in1=identb3, op=ALU.mult))(i),
                  reads=[mod_bc, self.ident_f], writes=[tmp])
                o("dve", (lambda i: lambda e: e.tensor_reduce(out=fm[i][:, :], in_=tmp[:, :, :], axis=AX.X, op=ALU.add))(i), reads=[tmp], writes=[fm[i]])
            o("dve", lambda e: e.tensor_copy(out=self.shift1[:, :], in_=fm[0][:, :]), reads=[fm[0]], writes=[self.shift1])
            o("dve", lambda e: e.scalar_tensor_tensor(out=self.g1eff[:, :], in0=fm[1][:, :], scalar=1.0, in1=n1g[:, :], op0=ALU.add, op1=ALU.mult), reads=[fm[1], n1g], writes=[self.g1eff])
            o("dve", lambda e: e.tensor_copy(out=self.shift2[:, :], in_=fm[3][:, :]), reads=[fm[3]], writes=[self.shift2])
            o("dve", lambda e: e.scalar_tensor_tensor(out=self.g2eff[:, :], in0=fm[4][:, :], scalar=1.0, in1=n2g[:, :], op0=ALU.add, op1=ALU.mult), reads=[fm[4], n2g], writes=[self.g2eff])
            if self.debug:
                for i, t in enumerate((self.g1eff, self.shift1, self.g2eff, self.shift2)):
                    self.store("sp", t, d["dbg_mod"][:, i, :], t[:, :])
            cvr = Ring([sb("cv%d" % i, [128, KC, 512], BF16) for i in range(2)])
            for j in range(IN_W // 512):
                cv = cvr.next()
                self.load("pool", cv, cv[:, :, :], d["w_in"][:, j * 512:(j + 1) * 512].rearrange("(k p) n -> p k n", p=128))
                self.store("sp", cv, d["w_in_b"][:, j * 512:(j + 1) * 512].rearrange("(k p) n -> p k n", p=128), cv[:, :, :], writes=[self.dbuf["w_in_b"]])
            for nm in ("w_bp", "w_br"):
                for j in range(2):
                    cv = cvr.next()
                    self.load("pool", cv, cv[:, 0:8, :].rearrange("p k n -> p (k n)").rearrange("p (k n) -> p k n", k=8) if False else cv[:, 0:8, :], d[nm][:, j * 1024:j * 1024 + 512].rearrange("(k p) n -> p k n", p=128))
                    self.load("pool", cv, cv[:, 8:16, :], d[nm][:, j * 1024 + 512:(j + 1) * 1024].rearrange("(k p) n -> p k n", p=128))
                    self.store("sp", cv, d[nm + "_b"][:, j * 1024:j * 1024 + 512].rearrange("(k p) n -> p k n", p=128), cv[:, 0:8, :], writes=[self.dbuf[nm + "_b"]])
                    self.store("sp", cv, d[nm + "_b"][:, j * 1024 + 512:(j + 1) * 1024].rearrange("(k p) n -> p k n", p=128), cv[:, 8:16, :], writes=[self.dbuf[nm + "_b"]])
            zt = sb("zeros", [128, 4 * D], BF16)
            o("pool", lambda e: e.memset(zt[:, :], 0.0), writes=[zt])
            nrows = self.cfg.rows
            for r0 in range(0, nrows, 512):
                nr = min(512, nrows - r0)
                self.dma("sp", (lambda r0, nr: lambda e: e.dma_start(out=d["xs_scr"][r0:r0 + nr, :].rearrange("(p j) d -> p (j d)", p=128), in_=zt[:, 0:(nr // 128) * D]))(r0, nr),
                         reads=[zt], writes=[self.dbuf["xs_scr"]], group="zero")
            self.dma("sp", lambda e: e.dma_start(out=d["gate2_scr"][:, :], in_=mod_bc[:, 5 * D:6 * D]), reads=[mod_bc], writes=[self.dbuf["gate2_scr"]], key=("st", "g2"))
            woring = Ring([sb("wo%d" % i, [128, D]) for i in range(2)])
            wgring = Ring([sb("wg%d" % i, [128, D], BF16) for i in range(2)])
            for kc in range(KC):
                wo = woring.next(); wg = wgring.next()
                self.load("sp", wo, wo[:, :], d["w_out"][kc * 128:(kc + 1) * 128, :])
                o("dve", (lambda wo, wg: lambda e: e.tensor_tensor(out=wg[:, :], in0=wo[:, :], in1=mod_bc[:, 2 * D:3 * D], op=ALU.mult))(wo, wg), reads=[wo, mod_bc], writes=[wg])
                self.store("sp", wg, d["w_out_g"][kc * 128:(kc + 1) * 128, :], wg[:, :], writes=[self.dbuf["w_out_g"]])
            self.P.emit()

    def load_consts_ret(self, sb, need_q):
        d = self.dram
        self.invf = sb("invf", [128, 64])
        self.load("sp", self.invf, self.invf[:, :], d["invf_bc"][:, :], group="c1")
        self.kdscale = sb("kdscale", [128, H])
        self.load("sp", self.kdscale, self.kdscale[:, :], d["kdscale"][:, :], group="c1")

    def make_trig_work(self, sb, tag):
        tw = {}
        for n in ("ang", "y", "kf", "ra", "r1", "rs", "c1", "tt", "c2"):
            tw[n] = sb(tag + n, [128, 64])
        tw["ki"] = sb(tag + "ki", [128, 64], I32)
        return tw

    def phase1(self):
        nc, d, o, cfg = self.nc, self.dram, self.op, self.cfg
        NP_ = cfg.npre
        if NP_ == 0:
            return
        with ExitStack() as pes:
            sb = lambda n, sh, dt=F32: T(pes.enter_context(nc.sbuf_tensor("p1_" + n, list(sh), dt)), n)
            ps = lambda n, sh, dt=F32: T(pes.enter_context(nc.psum_tensor("p1_" + n, list(sh), dt)), n)
            self.load_consts_ret(sb, False)
            wkv = sb("wkv", [128, KC, 2048], BF16)
            wa = sb("wa", [128, KC, 1024], BF16)
            for j in range(4):
                self.load("sp", wkv, wkv[:, :, j * 512:(j + 1) * 512], d["w_in_b"][:, OFF_K + j * 512:OFF_K + (j + 1) * 512].rearrange("(k p) n -> p k n", p=128), reads=[self.dbuf["w_in_b"]], group="c1")
            for j in range(2):
                self.load("sp", wa, wa[:, :, j * 512:(j + 1) * 512], d["w_in_b"][:, OFF_A + j * 512:OFF_A + (j + 1) * 512].rearrange("(k p) n -> p k n", p=128), reads=[self.dbuf["w_in_b"]], group="c1")
            pos_i = sb("pos_i", [128, NP_], I32); pos_f = sb("pos_f", [128, NP_]); flags = sb("flags", [128, NP_])
            self.load("sp", pos_i, pos_i[:, :], d["pos_pre"][:, :], group="c1")
            self.load("sp", flags, flags[:, :], d["flags_pre"][:, :], group="c1")
            o("dve", lambda e: e.tensor_copy(out=pos_f[:, :], in_=pos_i[:, :]), reads=[pos_i], writes=[pos_f])
            xring = Ring([sb("x%d" % i, [128, D]) for i in range(3)])
            smalls = Ring([(sb("ssq%d" % i, [128, 1]), sb("rt%d" % i, [128, 1]), sb("rstd%d" % i, [128, 1])) for i in range(2)])
            xnbs = Ring([sb("xnb%d" % i, [128, D], BF16) for i in range(2)])
            hTs = Ring([sb("hT%d" % i, [128, KC, 128], BF16) for i in range(2)])
            ptrs = [ps("ptr%d" % i, [128, 1024], BF16) for i in range(2)]
            pfr = Ring([ps("pf%d" % i, [128, 512]) for i in range(6)])
            tws = Ring([self.make_trig_work(sb, "tw%d" % i) for i in range(2)])
            coss = Ring([sb("cos%d" % i, [128, 64]) for i in range(2)])
            sins = Ring([sb("sin%d" % i, [128, 64]) for i in range(2)])
            tring = Ring([sb("rt_tmp%d" % i, [128, 256]) for i in range(8)])
            krots = Ring([sb("krot%d" % i, [128, 1024], BF16) for i in range(2)])
            kds = Ring([sb("kd%d" % i, [128, 1024], BF16) for i in range(2)])
            vbs = Ring([sb("vb%d" % i, [128, 1024], BF16) for i in range(2)])
            kdb = self.kdscale[:, :].unsqueeze(2).to_broadcast([128, H, 128])
            def pre_chunk(p):
                xt = xring.next(); small = smalls.next(); xnb = xnbs.next(); hT = hTs.next()
                self.norm_transpose(d["x_pre"][p * 128:(p + 1) * 128, :], xt, xnb, small, xnb, ptrs, hT, 0)
                cos_t = coss.next(); sin_t = sins.next()
                self.trig(pos_f, p, tws.next(), cos_t, sin_t)
                krot = krots.next(); kd = kds.next(); vb = vbs.next()
                for j in range(2):
                    pk = pfr.next()
                    for kc in range(KC):
                        o("pe", (lambda j, kc, pk: lambda e: e.matmul(out=pk[:, :], lhsT=hT[:, kc, :], rhs=wkv[:, kc, j * 512:(j + 1) * 512], start=(kc == 0), stop=(kc == KC - 1)))(j, kc, pk),
                          reads=[hT, wkv], writes=[pk])
                    self.rotary(pk, 4, cos_t, sin_t, tring, krot, j * 512)
                for j in range(2):
                    pv = pfr.next()
                    for kc in range(KC):
                        o("pe", (lambda j, kc, pv: lambda e: e.matmul(out=pv[:, :], lhsT=hT[:, kc, :], rhs=wkv[:, kc, 1024 + j * 512:1024 + (j + 1) * 512], start=(kc == 0), stop=(kc == KC - 1)))(j, kc, pv),
                          reads=[hT, wkv], writes=[pv])
                    o("act", (lambda j, pv: lambda e: e.activation(out=vb[:, j * 512:(j + 1) * 512], in_=pv[:, :], func=AF.Copy, scale=flags[:, p:p + 1]))(j, pv),
                      reads=[pv, flags], writes=[vb])
                o("pool", lambda e: e.tensor_tensor(out=kd[:, :].rearrange("p (h f) -> p h f", h=H), in0=krot[:, :].rearrange("p (h f) -> p h f", h=H), in1=kdb, op=ALU.mult),
                  reads=[krot, self.kdscale], writes=[kd])
                self.state_update(kd, vb, [pfr.next(), pfr.next()])
                if p == NP_ - 1:
                    for j in range(2):
                        pa = pfr.next()
                        for kc in range(KC):
                            o("pe", (lambda j, kc, pa: lambda e: e.matmul(out=pa[:, :], lhsT=hT[:, kc, :], rhs=wa[:, kc, j * 512:(j + 1) * 512], start=(kc == 0), stop=(kc == KC - 1)))(j, kc, pa),
                              reads=[hT, wa], writes=[pa])
                        o("act", (lambda j, pa: lambda e: e.activation(out=self.a_prev0[:, j * 512:(j + 1) * 512], in_=pa[:, :], func=AF.Copy))(j, pa),
                          reads=[pa], writes=[self.a_prev0])

            for p in range(NP_):
                pre_chunk(p)
            if self.debug:
                self.dma("sp", lambda e: e.dma_start(out=d["dbg_state"][:, :, :], in_=self.state[:, :, :]), reads=[self.state], key=("st", "dbgstate1"))
            self.P.emit()

    def state_update(self, kd, vb, pst):
        o = self.op
        for h in range(H):
            pt = pst[h // 4]
            o("pe", (lambda h, pt: lambda e: e.matmul(out=pt[:, (h % 4) * 128:(h % 4 + 1) * 128], lhsT=kd[:, h * 128:(h + 1) * 128], rhs=vb[:, h * 128:(h + 1) * 128], start=True, stop=True))(h, pt),
              reads=[kd, vb], writes=[pt])
        for h in range(H):
            pt = pst[h // 4]
            o("dve", (lambda h, pt: lambda e: e.scalar_tensor_tensor(out=self.state[:, h, :], in0=self.state[:, h, :], scalar=self.cd[h], in1=pt[:, (h % 4) * 128:(h % 4 + 1) * 128], op0=ALU.mult, op1=ALU.add))(h, pt),
              reads=[self.state, pt], writes=[self.state])

    def phase2(self):
        nc, d, o, cfg = self.nc, self.dram, self.op, self.cfg
        NM, TCH, NE, C, NRT = cfg.nmain, cfg.tch, cfg.ne, cfg.cap, cfg.nrt
        TT = TCH * 128
        with ExitStack() as pes:
            sb = lambda n, sh, dt=F32: T(pes.enter_context(nc.sbuf_tensor("p2_" + n, list(sh), dt)), n)
            ps = lambda n, sh, dt=F32: T(pes.enter_context(nc.psum_tensor("p2_" + n, list(sh), dt)), n)
            self.load_consts_ret(sb, True)
            bands = sb("bands", [128, 16, 128], BF16)
            self.load("pool", bands, bands[:, :, :], d["bands"][:, :, :], group="c2p")
            causal = sb("causal", [128, 128]); qsc = sb("qsc", [128, H, 128]); kusc = sb("kusc", [128, H, 128])
            tri = sb("tri", [128, 128], BF16); ones_b = sb("ones_b", [128, 128], BF16)
            iota_e = sb("iota_e", [128, NE]); brt = sb("brt", [128, NRT]); pscale = sb("pscale", [128, 8])
            wrt = sb("wrt", [128, KC, NRT]); wpool = sb("wpool", [128, 8, 256], BF16)
            pos_i = sb("pos_i", [128, NM], I32); pos_f = sb("pos_f", [128, NM])
            for t, src in ((causal, d["causalT"][:, :]), (qsc, d["qscaleT"][:, :, :]), (kusc, d["kuscaleT"][:, :, :]), (iota_e, d["iota_e"][:, :]),
                           (brt, d["brt_bc"][:, :]), (pscale, d["pscale_fm"][:, :]), (pos_i, d["pos_main"][:, :]),
                           (wrt, d["w_rt"][:, :].rearrange("(k p) n -> p k n", p=128))):
                self.load("sp", t, t[(slice(None),) * len(src.shape)], src, group="c2")
            self.load("pool", tri, tri[:, :], d["tri_u"][:, :], group="c2p")
            self.load("pool", wpool, wpool[:, :, :], d["w_pool"][:, :, :].rearrange("g (hh p) n -> p (g hh) n", p=128), group="c2p")
            o("pool", lambda e: e.memset(ones_b[:, :], 1.0), writes=[ones_b])
            o("dve", lambda e: e.tensor_copy(out=pos_f[:, :], in_=pos_i[:, :]), reads=[pos_i], writes=[pos_f])
            o("pool", lambda e: e.tensor_copy(out=self.state_bf[:, :, :], in_=self.state[:, :, :]), reads=[self.state], writes=[self.state_bf])
            msum = sb("msum", [128, NE], BF16)
            o("dve", lambda e: e.memset(msum[:, :], 0.0), writes=[msum])
            xring = Ring([sb("x%d" % i, [128, D]) for i in range(2)])
            smalls = Ring([(sb("ssq%d" % i, [128, 1]), sb("rt%d" % i, [128, 1]), sb("rstd%d" % i, [128, 1])) for i in range(2)])
            xnbs = Ring([sb("xnb%d" % i, [128, D], BF16) for i in range(2)])
            hT = sb("hT", [128, KC, TT], BF16)
            ptrs = [ps("ptr%d" % i, [128, 1024], BF16) for i in range(2)]
            pf = Ring([ps("pf%d" % i, [128, 512]) for i in range(6)])
            wr = Ring([sb("wslot%d" % i, [128, 4096], BF16) for i in range(3)])
            tws = Ring([self.make_trig_work(sb, "tw%d" % i) for i in range(1)])
            cos_l = [sb("cos%d" % i, [128, 64]) for i in range(TCH)]
            sin_l = [sb("sin%d" % i, [128, 64]) for i in range(TCH)]
            tring = Ring([sb("rt_tmp%d" % i, [128, 128]) for i in range(8)])
            a_ring = Ring([sb("a_tok%d" % i, [128, 1024], BF16) for i in range(TCH + 1)])
            bufA = sb("bufA", [128, 8, TT], BF16)
            bufB = sb("bufB", [128, 8, TT], BF16)
            mgT = sb("mgT", [128, KC, TT], BF16)
            sgt_r = Ring([sb("sgt%d" % i, [128, TT]) for i in range(2)])
            tmp2_r = Ring([sb("tmp2_%d" % i, [128, TT], BF16) for i in range(2)])
            qrot = [sb("qrot%d" % i, [128, 1024], BF16) for i in range(TCH)]
            krot = [sb("krot%d" % i, [128, 1024], BF16) for i in range(TCH)]
            kd_l = [sb("kd%d" % i, [128, 1024], BF16) for i in range(TCH)]
            v_l = [sb("v%d" % i, [128, 1024], BF16) for i in range(TCH)]
            srg_l = [sb("srg%d" % i, [128, 1024], BF16) for i in range(TCH)]
            qdT = sb("qdT", [128, H, TT], BF16); kuT = sb("kuT", [128, H, TT], BF16); retT = sb("retT", [128, H, TT], BF16)
            sbf = sb("sbf", [128, H, 128], BF16)
            sqj = sb("sqj", [128, 128], BF16); ont = sb("ont", [128, 1024]); ret_tok = sb("ret_tok", [128, 1024], BF16)
            ssqh = sb("ssqh", [128, H]); rth = sb("rth", [128, H]); rstdh = sb("rstdh", [128, H])
            xp_r = Ring([sb("xp%d" % i, [128, 256]) for i in range(4)])
            xnp_r = Ring([sb("xnp%d" % i, [128, 256]) for i in range(4)])
            junk2 = sb("junk2", [128, 256], BF16)
            ssq2 = [sb("ssq2_%d" % i, [128, 8]) for i in range(TCH)]
            x1t_r = xring
            xn2bf_r = xnbs
            h2T_r = Ring([sb("h2T%d" % i, [128, 4, 128]) for i in range(2)])
            rsm = {n: sb("rs_" + n, [128, w]) for n, w in (("ssum", 1), ("rt2", 1), ("rstd2", 1), ("lg", 4), ("gmax", 1), ("ngmax", 1), ("ohg", 4), ("eg", 4), ("sume", 1), ("pgrp", 1),
                                                               ("pen", 4), ("lem", NE), ("m1", 1), ("oh1", NE), ("lem2", NE), ("m2", 1), ("oh2", NE), ("dd", 1), ("ed", 1), ("den", 1), ("rr", 1),
                                                               ("mm", NE), ("prod", NE), ("rank", 2), ("eidx", 2), ("ovf", 2), ("destf", 2), ("pfx", NE))}
            mbf = sb("mbf", [128, NE], BF16)
            sidx_r = Ring([sb("sidx%d" % i, [128, 2], I32) for i in range(4)])
            kdb = self.kdscale[:, :].unsqueeze(2).to_broadcast([128, H, 128])
            self.p2_sbuf_left = nc.sbuf_bytes_remaining

            def wload(q, src_ap, k, reads=()):
                slot = wr.next()
                view = slot[:, :].rearrange("p (k n) -> p k n", k=k)
                self.load(q, slot, view, src_ap, reads=reads)
                return slot, view

            def tile_body(ti, a_prev):
                gch = [ti * TCH + lc for lc in range(TCH)]
                for lc, c in enumerate(gch):
                    xnb_ = xnbs.next()
                    self.norm_transpose(d["x_main"][c * 128:(c + 1) * 128, :], xring.next(), xnb_, smalls.next(), xnb_, ptrs, hT, lc * 128)
                    self.trig(pos_f, c, tws.next(), cos_l[lc], sin_l[lc])
                a_l = [a_ring.next() for _ in range(TCH)]
                for u in range(4):
                    slot, wv = wload("sp", d["w_in_b"][:, OFF_A + u * 256:OFF_A + (u + 1) * 256].rearrange("(k p) n -> p k n", p=128), KC, reads=[self.dbuf["w_in_b"]])
                    for lc in range(TCH):
                        pt = pf.next()
                        for kc in range(KC):
                            o("pe", (lambda kc, pt, wv, lc: lambda e: e.matmul(out=pt[:, 0:256], lhsT=hT[:, kc, lc * 128:(lc + 1) * 128], rhs=wv[:, kc, :], start=(kc == 0), stop=(kc == KC - 1)))(kc, pt, wv, lc),
                              reads=[hT, slot], writes=[pt])
                        o("act", (lambda pt, lc, u: lambda e: e.activation(out=a_l[lc][:, u * 256:(u + 1) * 256], in_=pt[:, 0:256], func=AF.Copy))(pt, lc, u), reads=[pt], writes=[a_l[lc]])
                for lc, c in enumerate(gch):
                    var = 8 if c == 0 else 0
                    acur = a_l[lc]; aprv = a_prev if lc == 0 else a_l[lc - 1]
                    for jb in range(2):
                        pt = pf.next()
                        for q4 in range(4):
                            j = jb * 4 + q4; g = j // 2
                            o("pe", (lambda pt, q4, j, g, acur, var: lambda e: e.matmul(out=pt[:, q4 * 128:(q4 + 1) * 128], lhsT=acur[:, j * 128:(j + 1) * 128], rhs=bands[:, var + g * 2, :], start=True, stop=False))(pt, q4, j, g, acur, var),
                              reads=[acur, bands], writes=[pt])
                            o("pe", (lambda pt, q4, j, g, aprv, var: lambda e: e.matmul(out=pt[:, q4 * 128:(q4 + 1) * 128], lhsT=aprv[:, j * 128:(j + 1) * 128], rhs=bands[:, var + g * 2 + 1, :], start=False, stop=True))(pt, q4, j, g, aprv, var),
                              reads=[aprv, bands], writes=[pt])
                        o("dve", (lambda pt, jb, lc: lambda e: e.tensor_copy(out=bufA[:, jb * 4:(jb + 1) * 4, lc * 128:(lc + 1) * 128], in_=pt[:, :].rearrange("p (j t) -> p j t", j=4)))(pt, jb, lc),
                          reads=[pt], writes=[bufA])
                a_prev_next = a_l[TCH - 1]
                for jo in range(8):
                    g, dh = jo // 2, jo % 2
                    pt = pf.next()
                    for hh in range(2):
                        o("pe", (lambda pt, g, dh, hh: lambda e: e.matmul(out=pt[:, 0:TT], lhsT=wpool[:, g * 2 + hh, dh * 128:(dh + 1) * 128], rhs=bufA[:, g * 2 + hh, :], start=(hh == 0), stop=(hh == 1)))(pt, g, dh, hh),
                          reads=[wpool, bufA], writes=[pt])
                    o("act", (lambda pt, jo: lambda e: e.activation(out=bufB[:, jo, :], in_=pt[:, 0:TT], func=AF.Copy, scale=pscale[:, jo:jo + 1]))(pt, jo), reads=[pt, pscale], writes=[bufB])
                self.gated_branch(OFF_GA, "w_bp", bufB, mgT, True, wload, pf, hT, sgt_r, tmp2_r, TT)
                for which, off, rot_l in (("q", OFF_Q, qrot), ("k", OFF_K, krot)):
                    for u in range(4):
                        slot, wv = wload("sp", d["w_in_b"][:, off + u * 256:off + (u + 1) * 256].rearrange("(k p) n -> p k n", p=128), KC, reads=[self.dbuf["w_in_b"]])
                        for lc in range(TCH):
                            pt = pf.next()
                            for kc in range(KC):
                                o("pe", (lambda kc, pt, wv, lc: lambda e: e.matmul(out=pt[:, 0:256], lhsT=hT[:, kc, lc * 128:(lc + 1) * 128], rhs=wv[:, kc, :], start=(kc == 0), stop=(kc == KC - 1)))(kc, pt, wv, lc),
                                  reads=[hT, slot], writes=[pt])
                            self.rotary(pt, 2, cos_l[lc], sin_l[lc], tring, rot_l[lc], u * 256)
                for lc in range(TCH):
                    for rot_l, dstT, sc in ((qrot, qdT, qsc), (krot, kuT, kusc)):
                        pt = ptrs[0] if rot_l is qrot else ptrs[1]
                        for h in range(H):
                            o("pe", (lambda pt, h, r: lambda e: e.transpose(out=pt[:, h * 128:(h + 1) * 128], in_=r[:, h * 128:(h + 1) * 128], identity=self.ident_b[:, :]))(pt, h, rot_l[lc]),
                              reads=[rot_l[lc], self.ident_b], writes=[pt])
                        o("dve", (lambda pt, dstT, sc, lc: lambda e: e.tensor_tensor(out=dstT[:, :, lc * 128:(lc + 1) * 128], in0=pt[:, :].rearrange("p (h t) -> p h t", h=H), in1=sc[:, :, :], op=ALU.mult))(pt, dstT, sc, lc),
                          reads=[pt, sc], writes=[dstT])
                    o("pool", (lambda lc: lambda e: e.tensor_tensor(out=kd_l[lc][:, :].rearrange("p (h f) -> p h f", h=H), in0=krot[lc][:, :].rearrange("p (h f) -> p h f", h=H), in1=kdb, op=ALU.mult))(lc),
                      reads=[krot[lc], self.kdscale], writes=[kd_l[lc]])
                for off, dst_l, fn in ((OFF_V, v_l, AF.Copy), (OFF_RG, srg_l, AF.Silu)):
                    for u in range(4):
                        slot, wv = wload("sp", d["w_in_b"][:, off + u * 256:off + (u + 1) * 256].rearrange("(k p) n -> p k n", p=128), KC, reads=[self.dbuf["w_in_b"]])
                        for lc in range(TCH):
                            pt = pf.next()
                            for kc in range(KC):
                                o("pe", (lambda kc, pt, wv, lc: lambda e: e.matmul(out=pt[:, 0:256], lhsT=hT[:, kc, lc * 128:(lc + 1) * 128], rhs=wv[:, kc, :], start=(kc == 0), stop=(kc == KC - 1)))(kc, pt, wv, lc),
                                  reads=[hT, slot], writes=[pt])
                            o("act", (lambda pt, lc, u, dst_l, fn: lambda e: e.activation(out=dst_l[lc][:, u * 256:(u + 1) * 256], in_=pt[:, 0:256], func=fn))(pt, lc, u, dst_l, fn), reads=[pt], writes=[dst_l[lc]])
                def ret_chunk(lc):
                    cs = slice(lc * 128, (lc + 1) * 128)
                    pS = [pf.next(), pf.next()]
                    for h in range(H):
                        o("pe", (lambda h, cs: lambda e: e.matmul(out=pS[h // 4][:, (h % 4) * 128:(h % 4 + 1) * 128], lhsT=kuT[:, h, cs], rhs=qdT[:, h, cs], start=True, stop=True))(h, cs),
                          reads=[kuT, qdT], writes=[pS[h // 4]])
                    for jb in range(2):
                        o("dve", (lambda jb: lambda e: e.tensor_tensor(out=sbf[:, jb * 4:(jb + 1) * 4, :], in0=pS[jb][:, :].rearrange("p (h t) -> p h t", h=4), in1=causal[:, :].unsqueeze(1).to_broadcast([128, 4, 128]), op=ALU.mult))(jb),
                          reads=[pS[jb], causal], writes=[sbf])
                    pO = [pf.next(), pf.next()]
                    for h in range(H):
                        o("pe", (lambda h: lambda e: e.matmul(out=pO[h // 4][:, (h % 4) * 128:(h % 4 + 1) * 128], lhsT=sbf[:, h, :], rhs=v_l[lc][:, h * 128:(h + 1) * 128], start=True, stop=False))(h),
                          reads=[sbf, v_l[lc]], writes=[pO[h // 4]])
                        o("pe", (lambda h, cs: lambda e: e.matmul(out=pO[h // 4][:, (h % 4) * 128:(h % 4 + 1) * 128], lhsT=qdT[:, h, cs], rhs=self.state_bf[:, h, :], start=False, stop=True))(h, cs),
                          reads=[qdT, self.state_bf], writes=[pO[h // 4]])
                    for h in range(H):
                        o("act", (lambda h: lambda e: e.activation(out=sqj[:, :], in_=pO[h // 4][:, (h % 4) * 128:(h % 4 + 1) * 128], func=AF.Square, accum_out=ssqh[:, h:h + 1]))(h),
                          reads=[pO[h // 4]], writes=[sqj, ssqh])
                    self.rstd_from_ssq(ssqh[:, :], ssqh, rth, rstdh, 1.0 / 128)
                    for jb in range(2):
                        o("dve", (lambda jb: lambda e: e.tensor_tensor(out=ont[:, jb * 512:(jb + 1) * 512].rearrange("p (h f) -> p h f", h=4), in0=pO[jb][:, :].rearrange("p (h f) -> p h f", h=4),
                                                                      in1=rstdh[:, jb * 4:(jb + 1) * 4].unsqueeze(2).to_broadcast([128, 4, 128]), op=ALU.mult))(jb),
                          reads=[pO[jb], rstdh], writes=[ont])
                    o("pool", (lambda lc: lambda e: e.tensor_tensor(out=ret_tok[:, :], in0=ont[:, :], in1=srg_l[lc][:, :], op=ALU.mult))(lc), reads=[ont, srg_l[lc]], writes=[ret_tok])
                    pt = ptrs[0]
                    for h in range(H):
                        o("pe", (lambda pt, h: lambda e: e.transpose(out=pt[:, h * 128:(h + 1) * 128], in_=ret_tok[:, h * 128:(h + 1) * 128], identity=self.ident_b[:, :]))(pt, h),
                          reads=[ret_tok, self.ident_b], writes=[pt])
                    o("act", (lambda pt, cs: lambda e: e.activation(out=retT[:, :, cs], in_=pt[:, :].rearrange("p (h t) -> p h t", h=H), func=AF.Copy))(pt, cs), reads=[pt], writes=[retT])
                    pU = [pf.next(), pf.next()]
                    self.state_update(kd_l[lc], v_l[lc], pU)
                    o("pool", lambda e: e.tensor_copy(out=self.state_bf[:, :, :], in_=self.state[:, :, :]), reads=[self.state], writes=[self.state_bf])
                for lc in range(TCH):
                    ret_chunk(lc)
                self.gated_branch(OFF_GB, "w_br", retT, mgT, False, wload, pf, hT, sgt_r, tmp2_r, TT)
                for u in range(8):
                    slot, wv = wload("sp", d["w_out_g"][:, u * 256:(u + 1) * 256].rearrange("(k p) n -> p k n", p=128), KC, reads=[self.dbuf["w_out_g"]])
                    for lc, c in enumerate(gch):
                        pt = pf.next()
                        for kc in range(KC):
                            o("pe", (lambda kc, pt, wv, lc: lambda e: e.matmul(out=pt[:, 0:256], lhsT=mgT[:, kc, lc * 128:(lc + 1) * 128], rhs=wv[:, kc, :], start=(kc == 0), stop=(kc == KC - 1)))(kc, pt, wv, lc),
                              reads=[mgT, slot], writes=[pt])
                        xp = xp_r.next(); xnp = xnp_r.next()
                        self.load("sp", xp, xp[:, :], d["x_main"][c * 128:(c + 1) * 128, u * 256:(u + 1) * 256])
                        o("dve", (lambda pt, xp, xnp: lambda e: e.tensor_tensor(out=xnp[:, :], in0=pt[:, 0:256], in1=xp[:, :], op=ALU.add))(pt, xp, xnp), reads=[pt, xp], writes=[xnp])
                        o("act", (lambda xnp, lc, u: lambda e: e.activation(out=junk2[:, :], in_=xnp[:, :], func=AF.Square, accum_out=ssq2[lc][:, u:u + 1]))(xnp, lc, u), reads=[xnp], writes=[junk2, ssq2[lc]])
                        self.store("sp", xnp, d["x1_scr"][c * 128:(c + 1) * 128, u * 256:(u + 1) * 256], xnp[:, :], writes=[self.x1_bufs[c]])
                        if self.debug:
                            self.dma("sp", (lambda xnp, c, u: lambda e: e.dma_start(out=d["dbg_x1"][c * 128:(c + 1) * 128, u * 256:(u + 1) * 256], in_=xnp[:, :]))(xnp, c, u), reads=[xnp], key=("stdbg", id(xnp.b)))
                for lc, c in enumerate(gch):
                    self.route_chunk(lc, c, ssq2[lc], rsm, x1t_r.next(), xn2bf_r.next(), h2T_r, pf, wrt, brt, iota_e, tri, ones_b, msum, mbf, sidx_r)
                return a_prev_next

            a_prev = self.a_prev0
            for ti in range(NM // TCH):
                a_prev = tile_body(ti, a_prev)
            self.overflow_dispatch(sb, pf, ones_b, msum, iota_e, x1t_r, xn2bf_r, sidx_r)
            self.P.emit()

    def gated_branch(self, off, wname, rhsT, mgT, first, wload, pf, hT, sgt_r, tmp2_r, TT):
        d, o = self.dram, self.op
        gslot = gv = bslot = bv = None
        for nch in range(KC):
            if nch % 2 == 0:
                gslot, gv = wload("sp", d["w_in_b"][:, off + (nch // 2) * 256:off + (nch // 2 + 1) * 256].rearrange("(k p) n -> p k n", p=128), KC, reads=[self.dbuf["w_in_b"]])
            if nch % 4 == 0:
                bslot, bv = wload("sp", d[wname + "_b"][:, (nch // 4) * 512:(nch // 4 + 1) * 512].rearrange("(k p) n -> p k n", p=128), 8, reads=[self.dbuf[wname + "_b"]])
            pA = pf.next()
            for kc in range(KC):
                o("pe", (lambda kc, pA, gv, nch: lambda e: e.matmul(out=pA[:, 0:TT], lhsT=gv[:, kc, (nch % 2) * 128:(nch % 2 + 1) * 128], rhs=hT[:, kc, :], start=(kc == 0), stop=(kc == KC - 1)))(kc, pA, gv, nch),
                  reads=[gslot, hT], writes=[pA])
            pB = pf.next()
            for kc in range(8):
                o("pe", (lambda kc, pB, bv, nch: lambda e: e.matmul(out=pB[:, 0:TT], lhsT=bv[:, kc, (nch % 4) * 128:(nch % 4 + 1) * 128], rhs=rhsT[:, kc, :], start=(kc == 0), stop=(kc == 7)))(kc, pB, bv, nch),
                  reads=[bslot, rhsT], writes=[pB])
            sgt = sgt_r.next()
            o("act", (lambda pA, sgt: lambda e: e.activation(out=sgt[:, :], in_=pA[:, 0:TT], func=AF.Sigmoid))(pA, sgt), reads=[pA], writes=[sgt])
            if first:
                o("dve", (lambda pB, sgt, nch: lambda e: e.tensor_tensor(out=mgT[:, nch, :], in0=pB[:, 0:TT], in1=sgt[:, :], op=ALU.mult))(pB, sgt, nch), reads=[pB, sgt], writes=[mgT])
            else:
                tmp2 = tmp2_r.next()
                o("dve", (lambda pB, sgt, tmp2: lambda e: e.tensor_tensor(out=tmp2[:, :], in0=pB[:, 0:TT], in1=sgt[:, :], op=ALU.mult))(pB, sgt, tmp2), reads=[pB, sgt], writes=[tmp2])
                o("pool", (lambda tmp2, nch: lambda e: e.tensor_tensor(out=mgT[:, nch, :], in0=mgT[:, nch, :], in1=tmp2[:, :], op=ALU.add))(tmp2, nch), reads=[mgT, tmp2], writes=[mgT])

    def route_chunk(self, lc, c, ssq2_t, R, x1t, xn2bf, h2T_r, pf, wrt, brt, iota_e, tri, ones_b, msum, mbf, sidx_r):
        d, o, cfg = self.dram, self.op, self.cfg
        NE, C, NRT, EPG = cfg.ne, cfg.cap, cfg.nrt, cfg.epg
        ROWS = cfg.rows
        o("dve", lambda e: e.tensor_reduce(out=R["ssum"][:, :], in_=ssq2_t[:, :], axis=AX.X, op=ALU.add), reads=[ssq2_t], writes=[R["ssum"]])
        self.rstd_from_ssq(R["ssum"][:, :], R["ssum"], R["rt2"], R["rstd2"], 1.0 / D)
        o("dve", lambda e: e.tensor_copy(out=self.rstd2_all[:, c:c + 1], in_=R["rstd2"][:, :]), reads=[R["rstd2"]], writes=[self.rt_bufs[c]])
        self.load("sp", x1t, x1t[:, :], d["x1_scr"][c * 128:(c + 1) * 128, :], reads=[self.x1_bufs[c]])
        o("dve", lambda e: e.tensor_scalar(out=x1t[:, :], in0=x1t[:, :], scalar1=R["rstd2"][:, 0:1], scalar2=None, op0=ALU.mult), reads=[x1t, R["rstd2"]], writes=[x1t])
        o("act", lambda e: e.activation(out=xn2bf[:, :], in_=x1t[:, :], func=AF.Copy), reads=[x1t], writes=[xn2bf])
        pL = pf.next()
        for grp in range(4):
            pt = pf.next()
            for q4 in range(4):
                kc = grp * 4 + q4
                o("pe", (lambda pt, q4, kc: lambda e: e.transpose(out=pt[:, q4 * 128:(q4 + 1) * 128], in_=x1t[:, kc * 128:(kc + 1) * 128], identity=self.ident_f[:, :]))(pt, q4, kc),
                  reads=[x1t, self.ident_f], writes=[pt])
            h2 = h2T_r.next()
            for q4 in range(4):
                kc = grp * 4 + q4
                if q4 % 2 == 0:
                    o("act", (lambda pt, q4, kc, h2: lambda e: e.activation(out=h2[:, q4, :], in_=pt[:, q4 * 128:(q4 + 1) * 128], func=AF.Identity, scale=self.g2eff[:, kc:kc + 1], bias=self.shift2[:, kc:kc + 1]))(pt, q4, kc, h2),
                      reads=[pt, self.g2eff, self.shift2], writes=[h2])
                else:
                    o("dve", (lambda pt, q4, kc, h2: lambda e: e.tensor_scalar(out=h2[:, q4, :], in0=pt[:, q4 * 128:(q4 + 1) * 128], scalar1=self.g2eff[:, kc:kc + 1], scalar2=self.shift2[:, kc:kc + 1], op0=ALU.mult, op1=ALU.add))(pt, q4, kc, h2),
                      reads=[pt, self.g2eff, self.shift2], writes=[h2])
            for q4 in range(4):
                kc = grp * 4 + q4
                o("pe", (lambda q4, kc, h2: lambda e: e.matmul(out=pL[:, 0:NRT], lhsT=h2[:, q4, :], rhs=wrt[:, kc, :], start=(kc == 0), stop=(kc == KC - 1)))(q4, kc, h2),
                  reads=[h2, wrt], writes=[pL])
        def ts(out, in0, s1, s2, op0, op1=None, reads=(), writes=()):
            if op1 is None:
                o("dve", lambda e: e.tensor_scalar(out=out, in0=in0, scalar1=s1, scalar2=None, op0=op0), reads=reads, writes=writes)
            else:
                o("dve", lambda e: e.tensor_scalar(out=out, in0=in0, scalar1=s1, scalar2=s2, op0=op0, op1=op1), reads=reads, writes=writes)

        def tt(out, in0, in1, op, reads=(), writes=()):
            o("dve", lambda e: e.tensor_tensor(out=out, in0=in0, in1=in1, op=op), reads=reads, writes=writes)

        def red(out, in_, op, reads=(), writes=()):
            o("dve", lambda e: e.tensor_reduce(out=out, in_=in_, axis=AX.X, op=op), reads=reads, writes=writes)
        lg, gmax, ngmax, ohg, eg, sume, pgrp, pen = (R[n] for n in ("lg", "gmax", "ngmax", "ohg", "eg", "sume", "pgrp", "pen"))
        lem, m1, oh1, lem2, m2, oh2, dd, ed, den, rr = (R[n] for n in ("lem", "m1", "oh1", "lem2", "m2", "oh2", "dd", "ed", "den", "rr"))
        mm, prod, rank, eidx, ovf, destf, pfx = (R[n] for n in ("mm", "prod", "rank", "eidx", "ovf", "destf", "pfx"))
        gwb, dsb = self.gw_bufs[c], self.dest_bufs[c]
        tt(lg[:, :], pL[:, 0:4], brt[:, 0:4], ALU.add, [pL, brt], [lg])
        red(gmax[:, :], lg[:, :], ALU.max, [lg], [gmax])
        ts(ohg[:, :], lg[:, :], gmax[:, 0:1], None, ALU.is_equal, None, [lg, gmax], [ohg])
        ts(ngmax[:, :], gmax[:, :], -1.0, None, ALU.mult, None, [gmax], [ngmax])
        o("act", lambda e: e.activation(out=eg[:, :], in_=lg[:, :], func=AF.Exp, bias=ngmax[:, 0:1], accum_out=sume[:, 0:1]), reads=[lg, ngmax], writes=[eg, sume])
        o("dve", lambda e: e.reciprocal(out=pgrp[:, :], in_=sume[:, :]), reads=[sume], writes=[pgrp])
        ts(pen[:, :], ohg[:, :], 1.0, 1e30, ALU.subtract, ALU.mult, [ohg], [pen])
        tt(lem[:, :], pL[:, 4:4 + NE], brt[:, 4:4 + NE], ALU.add, [pL, brt], [lem])
        tt(lem[:, :].rearrange("p (g e) -> p g e", g=NG), lem[:, :].rearrange("p (g e) -> p g e", g=NG), pen[:, :].unsqueeze(2).to_broadcast([128, NG, EPG]), ALU.add, [lem, pen], [lem])
        red(m1[:, :], lem[:, :], ALU.max, [lem], [m1])
        ts(oh1[:, :], lem[:, :], m1[:, 0:1], None, ALU.is_equal, None, [lem, m1], [oh1])
        o("dve", lambda e: e.scalar_tensor_tensor(out=lem2[:, :], in0=oh1[:, :], scalar=-1e30, in1=lem[:, :], op0=ALU.mult, op1=ALU.add), reads=[oh1, lem], writes=[lem2])
        red(m2[:, :], lem2[:, :], ALU.max, [lem2], [m2])
        ts(oh2[:, :], lem2[:, :], m2[:, 0:1], None, ALU.is_equal, None, [lem2, m2], [oh2])
        tt(dd[:, :], m2[:, :], m1[:, :], ALU.subtract, [m1, m2], [dd])
        o("act", lambda e: e.activation(out=ed[:, :], in_=dd[:, :], func=AF.Exp), reads=[dd], writes=[ed])
        ts(den[:, :], ed[:, :], 1.0, None, ALU.add, None, [ed], [den])
        o("dve", lambda e: e.reciprocal(out=rr[:, :], in_=den[:, :]), reads=[den], writes=[rr])
        tt(self.gw_all[:, c, 0:1], rr[:, :], pgrp[:, :], ALU.mult, [rr, pgrp], [gwb])
        tt(self.gw_all[:, c, 1:2], pgrp[:, :], self.gw_all[:, c, 0:1], ALU.subtract, [pgrp, gwb], [gwb])
        tt(mm[:, :], oh1[:, :], oh2[:, :], ALU.add, [oh1, oh2], [mm])
        o("dve", lambda e: e.tensor_copy(out=mbf[:, :], in_=mm[:, :]), reads=[mm], writes=[mbf])
        pP = pf.next()
        o("pe", lambda e: e.matmul(out=pP[:, 0:NE], lhsT=tri[:, :], rhs=mbf[:, :], start=True, stop=False), reads=[tri, mbf], writes=[pP])
        o("pe", lambda e: e.matmul(out=pP[:, 0:NE], lhsT=ones_b[:, :], rhs=msum[:, :], start=False, stop=True), reads=[ones_b, msum], writes=[pP])
        o("dve", lambda e: e.tensor_copy(out=pfx[:, :], in_=pP[:, 0:NE]), reads=[pP], writes=[pfx])
        for k, oh in ((0, oh1), (1, oh2)):
            tt(prod[:, :], oh[:, :], pfx[:, :], ALU.mult, [oh, pfx], [prod])
            red(rank[:, k:k + 1], prod[:, :], ALU.add, [prod], [rank])
            tt(prod[:, :], oh[:, :], iota_e[:, :], ALU.mult, [oh, iota_e], [prod])
            red(eidx[:, k:k + 1], prod[:, :], ALU.add, [prod], [eidx])
        ts(ovf[:, :], rank[:, :], float(C), float(ROWS), ALU.is_ge, ALU.mult, [rank], [ovf])
        o("dve", lambda e: e.scalar_tensor_tensor(out=destf[:, :], in0=eidx[:, :], scalar=float(C), in1=rank[:, :], op0=ALU.mult, op1=ALU.add), reads=[eidx, rank], writes=[destf])
        tt(destf[:, :], destf[:, :], ovf[:, :], ALU.add, [destf, ovf], [destf])
        ts(destf[:, :], destf[:, :], float(ROWS + 7), None, ALU.min, None, [destf], [destf])
        sidx = sidx_r.next()
        o("dve", lambda e: e.tensor_copy(out=sidx[:, :], in_=destf[:, :]), reads=[destf], writes=[sidx])
        o("dve", lambda e: e.tensor_copy(out=self.destf_all[:, c, :], in_=destf[:, :]), reads=[destf], writes=[self.rt_bufs[c]])
        o("dve", lambda e: e.tensor_copy(out=self.rank_all[:, c, :], in_=rank[:, :]), reads=[rank], writes=[self.rt_bufs[c]])
        o("dve", lambda e: e.tensor_copy(out=self.eidx_all[:, c, :], in_=eidx[:, :]), reads=[eidx], writes=[self.rt_bufs[c]])
        tt(msum[:, :], msum[:, :], mm[:, :], ALU.add, [msum, mm], [msum])
        for k in range(2):
            self.dma("pool", (lambda k: lambda e: e.indirect_dma_start(out=d["xs_scr"][:, :], out_offset=bass.IndirectOffsetOnAxis(ap=sidx[:, k:k + 1], axis=0),
                                                                      in_=xn2bf[:, :], in_offset=None, bounds_check=self.bound_reg(e, ROWS - 1), oob_is_err=False))(k),
                     reads=[xn2bf, sidx], writes=[self.dbuf["xs_scr"]], key=("st", id(xn2bf.b)))
        if self.debug and c == cfg.nmain - 1:
            self.dma("sp", lambda e: e.dma_start(out=d["dbg_gw"][:, :, :], in_=self.gw_all[:, :, :]), reads=self.gw_bufs, key=("st", "dbggw"))
            self.dma("sp", lambda e: e.dma_start(out=d["dbg_state"][:, :, :], in_=self.state[:, :, :]), reads=[self.state], key=("st", "dbgstate"))

    def overflow_dispatch(self, sb, pf, ones_b, msum, iota_e, x1t_r, xn2bf_r, sidx_r):
        d, o, cfg = self.dram, self.op, self.cfg
        NE, C, NS, NM, ROWS = cfg.ne, cfg.cap, cfg.ns, cfg.nmain, cfg.rows
        OOB = float(ROWS + 7)
        HALF = C / 2.0 - 0.5
        cnt = sb("od_cnt", [128, NE]); t1 = sb("od_t1", [128, NE]); ni = sb("od_ni", [128, NE], I32)
        nov = sb("od_nov", [128, NE]); ca = sb("od_ca", [128, NE]); cb = sb("od_cb", [128, NE]); obase = sb("od_obase", [128, NE])
        esf = sb("od_esf", [128, NS]); tmpe = sb("od_tmpe", [128, NE])
        pC = pf.next()
        o("pe", lambda e: e.matmul(out=pC[:, 0:NE], lhsT=ones_b[:, :], rhs=msum[:, :], start=True, stop=True), reads=[ones_b, msum], writes=[pC])
        o("dve", lambda e: e.tensor_copy(out=cnt[:, :], in_=pC[:, 0:NE]), reads=[pC], writes=[cnt])
        o("dve", lambda e: e.tensor_scalar(out=t1[:, :], in0=cnt[:, :], scalar1=float(-C), scalar2=0.0, op0=ALU.add, op1=ALU.max), reads=[cnt], writes=[t1])
        o("dve", lambda e: e.tensor_scalar(out=t1[:, :], in0=t1[:, :], scalar1=HALF, scalar2=1.0 / C, op0=ALU.add, op1=ALU.mult), reads=[t1], writes=[t1])
        o("dve", lambda e: e.tensor_copy(out=ni[:, :], in_=t1[:, :]), reads=[t1], writes=[ni])
        o("dve", lambda e: e.tensor_copy(out=nov[:, :], in_=ni[:, :]), reads=[ni], writes=[nov])
        cur, nxt = nov, ca
        sh = 1
        while sh < NE:
            o("dve", (lambda cur, nxt, sh: lambda e: e.tensor_copy(out=nxt[:, 0:sh], in_=cur[:, 0:sh]))(cur, nxt, sh), reads=[cur], writes=[nxt])
            o("dve", (lambda cur, nxt, sh: lambda e: e.tensor_tensor(out=nxt[:, sh:NE], in0=cur[:, sh:NE], in1=cur[:, 0:NE - sh], op=ALU.add))(cur, nxt, sh), reads=[cur], writes=[nxt])
            cur, nxt = nxt, (cb if nxt is ca else ca)
            sh *= 2
        oincl = cur
        o("dve", lambda e: e.tensor_tensor(out=obase[:, :], in0=oincl[:, :], in1=nov[:, :], op=ALU.subtract), reads=[oincl, nov], writes=[obase])
        for s_ in range(NS):
            o("dve", (lambda s_: lambda e: e.tensor_scalar(out=tmpe[:, :], in0=oincl[:, :], scalar1=float(s_), scalar2=None, op0=ALU.is_le))(s_), reads=[oincl], writes=[tmpe])
            o("dve", (lambda s_: lambda e: e.tensor_reduce(out=esf[:, s_:s_ + 1], in_=tmpe[:, :], axis=AX.X, op=ALU.add))(s_), reads=[tmpe], writes=[esf])
        iop = sb("od_iop", [128, 1]); esc = sb("od_esc", [128, NS]); esc4 = sb("od_esc4", [128, NS * 4])
        self.load("sp", iop, iop[:, :], d["iota_p"][:, :])
        o("dve", lambda e: e.tensor_scalar(out=esc[:, :], in0=esf[:, :], scalar1=512.0, scalar2=iop[:, 0:1], op0=ALU.mult, op1=ALU.add), reads=[esf, iop], writes=[esc])
        for u in range(4):
            o("dve", (lambda u: lambda e: e.tensor_scalar(out=esc4[:, :].rearrange("p (s u) -> p s u", u=4)[:, :, u], in0=esc[:, :], scalar1=float(u * 128), scalar2=None, op0=ALU.add))(u), reads=[esc], writes=[esc4])
        o("dve", lambda e: e.tensor_copy(out=self.slot_idx[:, 0:4 * NS], in_=esc4[:, :]), reads=[esc4], writes=[self.slot_idx])
        sm = {n: sb("od_" + n, [128, w]) for n, w in (("ov", 1), ("q", 1), ("qf", 1), ("oh", NE), ("ob", 1), ("a", 1), ("b", 1), ("fl", 1), ("dl", 2), ("sx", 2), ("df", 2))}
        qi = sb("od_qi", [128, 1], I32)

        def chunk(c):
            x1t = x1t_r.next(); xn2bf = xn2bf_r.next(); sidx = sidx_r.next()
            self.load("sp", x1t, x1t[:, :], d["x1_scr"][c * 128:(c + 1) * 128, :], reads=[self.x1_bufs[c]])
            o("dve", lambda e: e.tensor_scalar(out=x1t[:, :], in0=x1t[:, :], scalar1=self.rstd2_all[:, c:c + 1], scalar2=None, op0=ALU.mult), reads=[x1t, self.rt_bufs[c]], writes=[x1t])
            o("act", lambda e: e.activation(out=xn2bf[:, :], in_=x1t[:, :], func=AF.Copy), reads=[x1t], writes=[xn2bf])
            ov, q, qf, oh, ob, a, b, fl, dl, sx, df = (sm[n] for n in ("ov", "q", "qf", "oh", "ob", "a", "b", "fl", "dl", "sx", "df"))
            def per_k(k):
                rk = self.rank_all[:, c, k:k + 1]; ek = self.eidx_all[:, c, k:k + 1]
                o("dve", lambda e: e.tensor_scalar(out=ov[:, :], in0=rk, scalar1=float(-C), scalar2=None, op0=ALU.add), reads=[self.rt_bufs[c]], writes=[ov])
                o("dve", lambda e: e.tensor_scalar(out=q[:, :], in0=ov[:, :], scalar1=-HALF, scalar2=1.0 / C, op0=ALU.add, op1=ALU.mult), reads=[ov], writes=[q])
                o("dve", lambda e: e.tensor_copy(out=qi[:, :], in_=q[:, :]), reads=[q], writes=[qi])
                o("dve", lambda e: e.tensor_copy(out=qf[:, :], in_=qi[:, :]), reads=[qi], writes=[qf])
                o("dve", lambda e: e.tensor_scalar(out=oh[:, :], in0=iota_e[:, :], scalar1=ek, scalar2=None, op0=ALU.is_equal), reads=[iota_e, self.rt_bufs[c]], writes=[oh])
                o("dve", lambda e: e.tensor_tensor(out=oh[:, :], in0=oh[:, :], in1=obase[:, :], op=ALU.mult), reads=[oh, obase], writes=[oh])
                o("dve", lambda e: e.tensor_reduce(out=ob[:, :], in_=oh[:, :], axis=AX.X, op=ALU.add), reads=[oh], writes=[ob])
                o("dve", lambda e: e.scalar_tensor_tensor(out=a[:, :], in0=ob[:, :], scalar=float(NE), in1=qf[:, :], op0=ALU.add, op1=ALU.add), reads=[ob, qf], writes=[a])
                o("dve", lambda e: e.scalar_tensor_tensor(out=b[:, :], in0=qf[:, :], scalar=float(-C), in1=ov[:, :], op0=ALU.mult, op1=ALU.add), reads=[qf, ov], writes=[b])
                o("dve", lambda e: e.scalar_tensor_tensor(out=a[:, :], in0=a[:, :], scalar=float(C), in1=b[:, :], op0=ALU.mult, op1=ALU.add), reads=[a, b], writes=[a])
                o("dve", lambda e: e.tensor_scalar(out=fl[:, :], in0=ov[:, :], scalar1=0.0, scalar2=None, op0=ALU.is_ge), reads=[ov], writes=[fl])
                o("dve", lambda e: e.scalar_tensor_tensor(out=dl[:, k:k + 1], in0=a[:, :], scalar=-OOB, in1=fl[:, :], op0=ALU.add, op1=ALU.mult), reads=[a, fl], writes=[dl])
            per_k(0)
            per_k(1)
            o("dve", lambda e: e.tensor_scalar(out=sx[:, :], in0=dl[:, :], scalar1=OOB, scalar2=None, op0=ALU.add), reads=[dl], writes=[sx])
            o("dve", lambda e: e.tensor_copy(out=sidx[:, :], in_=sx[:, :]), reads=[sx], writes=[sidx])
            o("dve", lambda e: e.tensor_tensor(out=df[:, :], in0=self.destf_all[:, c, :], in1=dl[:, :], op=ALU.add), reads=[self.rt_bufs[c], dl], writes=[df])
            o("dve", lambda e: e.tensor_copy(out=self.dest_all[:, c, :], in_=df[:, :]), reads=[df], writes=[self.dest_bufs[c]])
            for k in range(2):
                self.dma("pool", (lambda k: lambda e: e.indirect_dma_start(out=d["xs_scr"][:, :], out_offset=bass.IndirectOffsetOnAxis(ap=sidx[:, k:k + 1], axis=0),
                                                                          in_=xn2bf[:, :], in_offset=None, bounds_check=self.bound_reg(e, ROWS - 1), oob_is_err=False))(k),
                         reads=[xn2bf, sidx], writes=[self.dbuf["xs_scr"]], key=("st", id(xn2bf.b)))

        for c in range(NM):
            chunk(c)
        if self.debug:
            self.dma("sp", lambda e: e.dma_start(out=d["dbg_dest"][:, :, :], in_=self.dest_all[:, :, :]), reads=self.dest_bufs, key=("st", "dbgdest"))

    def phase3(self):
        nc, d, o, cfg = self.nc, self.dram, self.op, self.cfg
        NE, C, NB = cfg.ne, cfg.cap, cfg.nb
        with ExitStack() as pes:
            sb = lambda n, sh, dt=F32: T(pes.enter_context(nc.sbuf_tensor("p3_" + n, list(sh), dt)), n)
            ps = lambda n, sh, dt=F32: T(pes.enter_context(nc.psum_tensor("p3_" + n, list(sh), dt)), n)
            xst_r = Ring([sb("xst%d" % i, [128, D], BF16) for i in range(3)])
            xbT_r = Ring([sb("xbT%d" % i, [128, KC, cfg.wcap], BF16) for i in range(2 * cfg.nwin)])
            hidT_r = Ring([sb("hidT%d" % i, [128, FC, cfg.wcap], BF16) for i in range(2 * cfg.nwin)])
            w13_r = Ring([sb("w13_%d" % i, [128, KC, 256], BF16) for i in range(4)])
            w2_r = Ring([sb("w2_%d" % i, [128, FC, 512], BF16) for i in range(3)])
            st_r = Ring([sb("silu%d" % i, [128, cfg.wcap]) for i in range(2)])
            yt_r = Ring([sb("yt%d" % i, [128, 512]) for i in range(4)])
            ptrs = [ps("ptr%d" % i, [128, 1024], BF16) for i in range(2)]
            pf = Ring([ps("pf%d" % i, [128, 512]) for i in range(6)])
            NW, WC = cfg.nwin, cfg.wcap

            wstg_r = Ring([sb("wstg%d" % i, [128, 4096]) for i in range(2)])

            def wdma(dst, name, ex, u):
                dst2 = dst[:, :, :].rearrange("p k n -> p (k n)")
                if isinstance(ex, tuple):
                    j = ex[1] * 4 + u
                    stg = wstg_r.next()
                    self.dma("pool", lambda e: e.indirect_dma_start(out=stg[:, :], out_offset=None, in_=d[name][:, :],
                                                                   in_offset=bass.IndirectOffsetOnAxis(ap=self.slot_idx[:, j:j + 1], axis=0),
                                                                   bounds_check=self.bound_reg(e, NE * 512 - 1), oob_is_err=False),
                             reads=[self.slot_idx], writes=[stg], key=stg)
                    return o("pool", lambda e: e.tensor_copy(out=dst2, in_=stg[:, :]), reads=[stg], writes=[dst])
                return self.load("pool", dst, dst2, d[name][(ex * 4 + u) * 128:(ex * 4 + u + 1) * 128, :])

            def expert_body(ex, rbase=None):
                if rbase is not None:
                    ex = ("dyn", ex)
                else:
                    rbase = ex * C
                xbTs = [xbT_r.next() for _ in range(NW)]
                hidTs = [hidT_r.next() for _ in range(NW)]
                for w in range(NW):
                    for b in range(NB):
                        xst = xst_r.next()
                        r0 = rbase + w * WC + b * 128
                        self.load("sp", xst, xst[:, :], d["xs_scr"][r0:r0 + 128, :], reads=[self.dbuf["xs_scr"]])
                        for kc in range(KC):
                            pt = ptrs[kc // 8]
                            o("pe", (lambda kc, pt, xst: lambda e: e.transpose(out=pt[:, (kc % 8) * 128:(kc % 8 + 1) * 128], in_=xst[:, kc * 128:(kc + 1) * 128], identity=self.ident_b[:, :]))(kc, pt, xst),
                              reads=[xst, self.ident_b], writes=[pt])
                        self.evac_mod(ptrs, xbTs[w], b * 128, self.g2eff, self.shift2)
                for fu in range(4):
                    w1u = w13_r.next(); w3u = w13_r.next()
                    wdma(w1u, "w1", ex, fu)
                    wdma(w3u, "w3", ex, fu)
                    for w in range(NW):
                        xbT = xbTs[w]; hidT = hidTs[w]
                        for fh in range(2):
                            fc = fu * 2 + fh
                            p1 = pf.next(); p3 = pf.next()
                            for wu, pp in ((w1u, p1), (w3u, p3)):
                                for kc in range(KC):
                                    o("pe", (lambda wu, pp, kc, fh, xbT: lambda e: e.matmul(out=pp[:, 0:WC], lhsT=wu[:, kc, fh * 128:(fh + 1) * 128], rhs=xbT[:, kc, :], start=(kc == 0), stop=(kc == KC - 1)))(wu, pp, kc, fh, xbT),
                                      reads=[wu, xbT], writes=[pp])
                            st = st_r.next()
                            o("act", (lambda p1, st: lambda e: e.activation(out=st[:, :], in_=p1[:, 0:WC], func=AF.Silu))(p1, st), reads=[p1], writes=[st])
                            o("dve", (lambda p3, st, fc, hidT: lambda e: e.tensor_tensor(out=hidT[:, fc, :], in0=p3[:, 0:WC], in1=st[:, :], op=ALU.mult))(p3, st, fc, hidT), reads=[p3, st], writes=[hidT])
                for nu in range(4):
                    w2u = w2_r.next()
                    wdma(w2u, "w2", ex, nu)
                    for w in range(NW):
                        hidT = hidTs[w]
                        for b in range(NB):
                            py = pf.next()
                            for fc in range(FC):
                                o("pe", (lambda py, fc, b, w2u, hidT: lambda e: e.matmul(out=py[:, :], lhsT=hidT[:, fc, b * 128:(b + 1) * 128], rhs=w2u[:, fc, :], start=(fc == 0), stop=(fc == FC - 1)))(py, fc, b, w2u, hidT),
                                  reads=[hidT, w2u], writes=[py])
                            yt = yt_r.next()
                            if (nu * NB + b) % 2 == 0:
                                o("act", (lambda py, yt: lambda e: e.activation(out=yt[:, :], in_=py[:, :], func=AF.Copy))(py, yt), reads=[py], writes=[yt])
                            else:
                                o("dve", (lambda py, yt: lambda e: e.tensor_copy(out=yt[:, :], in_=py[:, :]))(py, yt), reads=[py], writes=[yt])
                            r0 = rbase + w * WC + b * 128
                            self.store("sp", yt, d["ys_scr"][r0:r0 + 128, nu * 512:(nu + 1) * 512], yt[:, :], writes=[self.dbuf["ys_scr"]])

            for ex in range(NE):
                expert_body(ex)
            for s_ in range(cfg.ns):
                expert_body(s_, rbase=(NE + s_) * C)
            self.P.emit()

    def phase4(self):
        nc, d, o, cfg = self.nc, self.dram, self.op, self.cfg
        NM, NE, C = cfg.nmain, cfg.ne, cfg.cap
        with ExitStack() as pes:
            sb = lambda n, sh, dt=F32: T(pes.enter_context(nc.sbuf_tensor("p4_" + n, list(sh), dt)), n)
            gate2 = sb("gate2", [128, D]); fgain = sb("fgain", [128, D])
            self.load("sp", gate2, gate2[:, :], d["gate2_scr"][:, :], reads=[self.dbuf["gate2_scr"]], group="c4")
            self.load("sp", fgain, fgain[:, :], d["fgain_bc"][:, :], group="c4")
            x1_r = Ring([sb("x1_%d" % i, [128, D]) for i in range(2)])
            y0_r = Ring([sb("y0_%d" % i, [128, D]) for i in range(2)])
            y1_r = Ring([sb("y1_%d" % i, [128, D]) for i in range(2)])
            junk = sb("junk", [128, D], BF16)
            smalls = Ring([(sb("ssq%d" % i, [128, 1]), sb("rt%d" % i, [128, 1]), sb("rstd%d" % i, [128, 1])) for i in range(2)])
            for c in range(NM):
                x1 = x1_r.next(); y0 = y0_r.next(); y1 = y1_r.next(); ssq, rt, rstd = smalls.next()
                self.load("sp", x1, x1[:, :], d["x1_scr"][c * 128:(c + 1) * 128, :], reads=[self.x1_bufs[c]])
                for k, y in ((0, y0), (1, y1)):
                    o("pool", (lambda y: lambda e: e.memset(y[:, :], 0.0))(y), writes=[y])
                    self.dma("pool", (lambda k, y, c: lambda e: e.indirect_dma_start(out=y[:, :], out_offset=None, in_=d["ys_scr"][:, :],
                                                                                   in_offset=bass.IndirectOffsetOnAxis(ap=self.dest_all[:, c, k:k + 1], axis=0),
                                                                                   bounds_check=self.bound_reg(e, cfg.rows - 1), oob_is_err=False))(k, y, c),
                             reads=[self.dbuf["ys_scr"], self.dest_bufs[c]], writes=[y], key=y)
                o("dve", (lambda y0, c: lambda e: e.tensor_scalar(out=y0[:, :], in0=y0[:, :], scalar1=self.gw_all[:, c, 0:1], scalar2=None, op0=ALU.mult))(y0, c), reads=[y0, self.gw_bufs[c]], writes=[y0])
                o("dve", (lambda y0, y1, c: lambda e: e.scalar_tensor_tensor(out=y1[:, :], in0=y1[:, :], scalar=self.gw_all[:, c, 1:2], in1=y0[:, :], op0=ALU.mult, op1=ALU.add))(y0, y1, c),
                  reads=[y0, y1, self.gw_bufs[c]], writes=[y1])
                o("pool", (lambda y1: lambda e: e.tensor_tensor(out=y1[:, :], in0=y1[:, :], in1=gate2[:, :], op=ALU.mult))(y1), reads=[y1, gate2], writes=[y1])
                o("dve", (lambda y1, x1: lambda e: e.tensor_tensor(out=x1[:, :], in0=y1[:, :], in1=x1[:, :], op=ALU.add))(y1, x1), reads=[y1, x1], writes=[x1])
                o("act", (lambda x1, ssq: lambda e: e.activation(out=junk[:, :], in_=x1[:, :], func=AF.Square, accum_out=ssq[:, 0:1]))(x1, ssq), reads=[x1], writes=[junk, ssq])
                self.rstd_from_ssq(ssq[:, 0:1], ssq, rt, rstd, 1.0 / D)
                o("dve", (lambda x1, y0, rstd: lambda e: e.scalar_tensor_tensor(out=y0[:, :], in0=x1[:, :], scalar=rstd[:, 0:1], in1=fgain[:, :], op0=ALU.mult, op1=ALU.mult))(x1, y0, rstd),
                  reads=[x1, rstd, fgain], writes=[y0])
                self.store("sp", y0, d["out"][c * 128:(c + 1) * 128, :], y0[:, :])
            self.P.emit()


def make_consts(cfg, first_seg):
    gam = np.array([1.0 - 2.0 ** (-5.0 - h) for h in range(H)], np.float64)
    idx = np.arange(128, dtype=np.float64)
    c = {}
    c["ident_f"] = np.eye(128, dtype=np.float32)
    bands = np.zeros((128, 16, 128), np.float64)
    for g, w in enumerate(POOL_WINDOWS):
        cur = np.zeros((128, 128)); prv = np.zeros((128, 128)); cur0 = np.zeros((128, 128))
        for t in range(128):
            for j in range(w):
                tp = t - j
                if tp >= 0:
                    cur[tp, t] += 1.0 / w
                else:
                    prv[128 + tp, t] += 1.0 / w
            cnt = min(t + 1, w)
            for j in range(cnt):
                cur0[t - j, t] += 1.0 / cnt
            cur[t, t] -= 1.0
            cur0[t, t] -= 1.0
        bands[:, g * 2, :] = cur
        bands[:, g * 2 + 1, :] = prv
        if first_seg:
            bands[:, 8 + g * 2, :] = cur0
        else:
            bands[:, 8 + g * 2, :] = cur
            bands[:, 8 + g * 2 + 1, :] = prv
    c["bands"] = bands.astype(np.float32)
    c["causalT"] = (idx[None, :] >= idx[:, None]).astype(np.float32)
    qs = gam[:, None] ** (idx[None, :] + 1.0)
    ku = gam[:, None] ** (-(idx[None, :] + 1.0)) * (128.0 ** -0.5)
    kd = gam[:, None] ** (127.0 - idx[None, :]) * (128.0 ** -0.5)
    c["qscaleT"] = np.broadcast_to(qs[None], (128, H, 128)).astype(np.float32).copy()
    c["kuscaleT"] = np.broadcast_to(ku[None], (128, H, 128)).astype(np.float32).copy()
    c["kdscale"] = kd.T.astype(np.float32).copy()
    c["tri_u"] = (idx[:, None] < idx[None, :]).astype(np.float32)
    c["iota_p"] = idx.astype(np.float32).reshape(128, 1).copy()
    c["iota_e"] = np.broadcast_to(np.arange(cfg.ne, dtype=np.float32)[None], (128, cfg.ne)).copy()
    invf = (10000.0 ** (-np.arange(64, dtype=np.float32) / np.float32(64))).astype(np.float32)
    c["invf_bc"] = np.broadcast_to(invf[None], (128, 64)).copy()
    return c


def shared_inputs(inp, cfg):
    f = lambda a: np.ascontiguousarray(np.asarray(a), dtype=np.float32)
    col = lambda v: np.ascontiguousarray(f(v).reshape(-1, 128).T)
    bc = lambda v: np.ascontiguousarray(np.broadcast_to(f(v).reshape(1, -1), (128, f(v).size)))
    s = {}
    s["b_ada_bc"] = bc(inp["b_ada"][0])
    s["n1g_fm"] = col(inp["norm1_gain"][0])
    s["n2g_fm"] = col(inp["norm2_gain"][0])
    s["fgain_bc"] = bc(inp["final_gain"])
    s["pscale_fm"] = col(inp["pool_scale"][0])
    s["brt_bc"] = bc(np.concatenate([f(inp["b_group"][0]), f(inp["b_router"][0])]))
    s["w_ada"] = f(inp["w_ada"][0]); s["w_in"] = f(inp["w_in"][0]); s["w_pool"] = f(inp["w_pool"][0])
    s["w_bp"] = f(inp["w_branch_pool"][0]); s["w_br"] = f(inp["w_branch_ret"][0]); s["w_out"] = f(inp["w_out"][0])
    s["w_rt"] = np.ascontiguousarray(np.concatenate([f(inp["w_group"][0]), f(inp["w_router"][0])], axis=1))
    ne = np.asarray(inp["w1"]).shape[1]
    relay = lambda w, k, n: np.ascontiguousarray(f(w).reshape(ne, k, 128, 4, n).transpose(0, 3, 2, 1, 4)).reshape(ne * 512, k * n)
    s["w1"] = relay(inp["w1"][0], KC, 256); s["w3"] = relay(inp["w3"][0], KC, 256); s["w2"] = relay(inp["w2"][0], FC, 512)
    return s


def core_inputs(inp, shared, cfg, b, start):
    NM, NP_ = cfg.nmain, cfg.npre
    x = np.asarray(inp["x"]); pos = np.asarray(inp["positions"])
    m = dict(shared)
    m.update(make_consts(cfg, start == 0))
    m["x_main"] = np.ascontiguousarray(x[b, start:start + NM * 128], dtype=np.float32)
    m["pos_main"] = np.ascontiguousarray(pos[b, start:start + NM * 128].reshape(NM, 128).T.astype(np.int32))
    npre_tok = NP_ * 128
    xp = np.zeros((max(npre_tok, 128), D), np.float32)
    pp = np.zeros((max(npre_tok, 128),), np.int32)
    fl = np.zeros((max(NP_, 1),), np.float32)
    lo = start - npre_tok
    for p in range(NP_):
        t0 = lo + p * 128
        if t0 >= 0:
            xp[p * 128:(p + 1) * 128] = x[b, t0:t0 + 128]
            pp[p * 128:(p + 1) * 128] = pos[b, t0:t0 + 128]
            fl[p] = 1.0
    m["x_pre"] = xp[:max(npre_tok, 128)]
    m["pos_pre"] = np.ascontiguousarray(pp.reshape(-1, 128).T)
    m["flags_pre"] = np.ascontiguousarray(np.broadcast_to(fl[None], (128, fl.size)))
    m["c_col"] = np.ascontiguousarray(np.asarray(inp["c"], dtype=np.float32)[b].reshape(-1, 128).T)
    return m


_NC_CACHE = {}


def get_nc(cfg_key, debug=False):
    key = (cfg_key, debug)
    if key not in _NC_CACHE:
        cfg = Cfg(*cfg_key)
        _NC_CACHE[key] = K(cfg, debug=debug).build()
    return _NC_CACHE[key]


def kernel(**inputs):
    cfg_key = (32, 96, 2, 8, 3, 1)
    cfg = Cfg(*cfg_key)
    nc = get_nc(cfg_key)
    shared = shared_inputs(inputs, cfg)
    B, S = 2, 16384
    seg = cfg.nmain * 128
    in_maps = []
    for core in range(8):
        b, sg = core // 4, core % 4
        in_maps.append(core_inputs(inputs, shared, cfg, b, sg * seg))
    res = run_bass_kernel_spmd(nc, in_maps, core_ids=list(range(8)))
    out = np.empty((B, S, D), np.float32)
    for core in range(8):
        b, sg = core // 4, core % 4
        out[b, sg * seg:(sg + 1) * seg] = np.asarray(res.results[core]["out"])
    return out
```

```python
import math
from contextlib import ExitStack

import numpy as np
import concourse.bass as bass
import concourse.mybir as mybir
from concourse.bass_utils import run_bass_kernel_spmd

F32 = mybir.dt.float32
BF16 = mybir.dt.bfloat16
I32 = mybir.dt.int32
AF = mybir.ActivationFunctionType
ALU = mybir.AluOpType
AX = mybir.AxisListType

ENGS = ("pe", "act", "dve", "pool", "sp")


class Buf:
    __slots__ = ("name", "writers", "readers", "prev_readers")

    def __init__(self, name):
        self.name = name
        self.writers = []
        self.readers = []
        self.prev_readers = []


class Op:
    __slots__ = ("eng", "fn", "deps", "is_dma", "sem", "val", "sig", "sigidx", "group", "done", "pos")

    def __init__(self, eng, fn, is_dma):
        self.eng = eng
        self.fn = fn
        self.deps = {}
        self.is_dma = is_dma
        self.sem = None
        self.val = 0
        self.sig = False
        self.sigidx = 0
        self.group = None
        self.done = False
        self.pos = 0


class Prog:
    def __init__(self, nc, es):
        self.nc = nc
        self.es = es
        self.streams = {e: [] for e in ENGS}
        self.eng_sem = {e: es.enter_context(nc.semaphore("prog_" + e)) for e in ("pe", "act", "dve", "pool")}
        self.eng_cnt = {e: 0 for e in ("pe", "act", "dve", "pool")}
        self.done_sem = es.enter_context(nc.semaphore("phase_done"))
        self.done_cnt = 0
        self.dma_pool = {"sp": [], "pool": []}
        self.dma_keys = {}
        self.dma_used = {"sp": 0, "pool": 0}
        self.group_ops = {}

    def _track(self, op, reads, writes):
        for b in reads:
            for w in b.writers:
                op.deps[w] = True
            b.readers.append(op)
        for b in writes:
            for r in b.readers:
                if r is not op:
                    op.deps.setdefault(r, False)
            for r in b.prev_readers:
                if r is not op:
                    op.deps.setdefault(r, False)
            for w in b.writers:
                op.deps.setdefault(w, False)
            if b.readers:
                b.prev_readers = b.readers
                b.readers = []
                b.writers = [op]
            else:
                b.writers.append(op)

    def op(self, eng, fn, reads=(), writes=()):
        o = Op(eng, fn, False)
        self._track(o, reads, writes)
        o.pos = len(self.streams[eng])
        self.streams[eng].append(o)
        return o

    def _dma_sem(self, key, queue):
        k = (queue, key)
        if k not in self.dma_keys:
            idx = self.dma_used[queue]
            self.dma_used[queue] += 1
            pool = self.dma_pool[queue]
            if idx >= len(pool):
                s = self.es.enter_context(self.nc.semaphore("dma_%s%d" % (queue, idx)))
                pool.append([s, 0])
            self.dma_keys[k] = pool[idx]
        return self.dma_keys[k]

    def dma(self, queue, fn, reads=(), writes=(), key=None, group=None):
        o = Op(queue, fn, True)
        self._track(o, reads, writes)
        ent = self._dma_sem(("g", group) if group is not None else key, queue)
        ent[1] += 1
        o.sem = ent[0]
        o.val = 16 * ent[1]
        if group is not None:
            o.group = group
            self.group_ops.setdefault(group, []).append(o)
        self.streams[queue].append(o)
        return o

    def emit(self):
        nc = self.nc
        for ops in self.group_ops.values():
            final = max(o.val for o in ops)
            for o in ops:
                o.val = final
        def needed(o, d, is_raw):
            if d.done:
                return False
            if d.group is not None and d.group == o.group:
                return False
            if d.is_dma or o.is_dma:
                return True
            if d.eng != o.eng:
                return True
            return d.eng != "pe"
        for e in ENGS:
            for o in self.streams[e]:
                latest = {}
                for d, is_raw in o.deps.items():
                    if not d.is_dma and needed(o, d, is_raw):
                        if d.eng not in latest or latest[d.eng].pos < d.pos:
                            latest[d.eng] = d
                for d in latest.values():
                    d.sig = True
        last = {}
        for e in ("pe", "act", "dve", "pool"):
            for o in reversed(self.streams[e]):
                if not o.is_dma:
                    o.sig = True
                    last[e] = o
                    break
        for e in ("pe", "act", "dve", "pool"):
            c = self.eng_cnt[e]
            for o in self.streams[e]:
                if not o.is_dma and o.sig:
                    c += 1
                    o.sigidx = c
                    o.sem = self.eng_sem[e]
                    o.val = c
            self.eng_cnt[e] = c
        self.done_cnt += 1
        done_val = self.done_cnt
        final_dma = [(ent[0], 16 * ent[1]) for q in ("sp", "pool") for ent in self.dma_pool[q] if ent[1] > 0]
        final_eng = [(self.eng_sem[e], self.eng_cnt[e]) for e in ("pe", "act", "dve", "pool") if self.eng_cnt[e] > 0]
        streams = self.streams
        done_sem = self.done_sem

        def run(eng_name, eng):
            waited = {}
            for o in streams[eng_name]:
                req = {}
                latest = {}
                for d, is_raw in o.deps.items():
                    if not needed(o, d, is_raw):
                        continue
                    if not d.is_dma:
                        if d.eng not in latest or latest[d.eng].pos < d.pos:
                            latest[d.eng] = d
                        continue
                    k = id(d.sem)
                    if k not in req or req[k][1] < d.val:
                        req[k] = (d.sem, d.val)
                for d in latest.values():
                    k = id(d.sem)
                    if k not in req or req[k][1] < d.val:
                        req[k] = (d.sem, d.val)
                for k, (sem, val) in req.items():
                    if waited.get(k, -1) < val:
                        eng.wait_ge(sem, val)
                        waited[k] = val
                ins = o.fn(eng)
                if o.is_dma:
                    ins.then_inc(o.sem, 16)
                elif o.sig:
                    ins.then_inc(o.sem, 1)
            if eng_name == "sp":
                for s, v in final_eng + final_dma:
                    eng.wait_ge(s, v)
                eng.sem_inc(done_sem, 1)
            else:
                eng.wait_ge(done_sem, done_val)

        with nc.Block() as block:
            @block.tensor
            def _(eng):
                run("pe", eng)

            @block.scalar
            def _(eng):
                run("act", eng)

            @block.vector
            def _(eng):
                run("dve", eng)

            @block.gpsimd
            def _(eng):
                run("pool", eng)

            @block.sync
            def _(eng):
                run("sp", eng)
        for e in ENGS:
            for o in self.streams[e]:
                o.done = True
                o.fn = None
        self.streams = {e: [] for e in ENGS}
        self.dma_keys = {}
        self.dma_used = {"sp": 0, "pool": 0}
        self.group_ops = {}


D = 2048
KC = 16
H = 8
NG = 4
F = 1024
FC = 8
IN_W = 9216
OFF_A, OFF_Q, OFF_K, OFF_V, OFF_RG, OFF_GA, OFF_GB = 0, 1024, 2048, 3072, 4096, 5120, 7168
EPS = 1e-6
PI_LO = 3.141592
TWO_PI = 2.0 * math.pi
CW1 = 6.28125
CW2 = TWO_PI - CW1
POOL_WINDOWS = (2, 4, 8, 16)


class Cfg:
    def __init__(self, nmain=32, npre=96, tch=4, epg=8, cap_blocks=3, nwin=1):
        self.nmain, self.npre, self.tch, self.epg, self.nb, self.nwin = nmain, npre, tch, epg, cap_blocks, nwin
        self.ne = NG * epg
        self.wcap = cap_blocks * 128
        self.cap = cap_blocks * 128 * nwin
        assert nwin == 1
        self.ns = (2 * 128 * nmain) // self.cap
        self.rows = (self.ne + self.ns) * self.cap
        self.nrt = NG + self.ne
        assert nmain % tch == 0


class T:
    __slots__ = ("h", "b")

    def __init__(self, h, name):
        self.h = h
        self.b = Buf(name)

    def __getitem__(self, k):
        return self.h[k]


class Ring:
    def __init__(self, items):
        self.items = items
        self.i = 0

    def next(self):
        t = self.items[self.i % len(self.items)]
        self.i += 1
        return t


def _bufs(lst):
    return [x.b if isinstance(x, T) else x for x in lst]


class K:
    def __init__(self, cfg, debug=False, stop_after=9):
        self.cfg = cfg
        self.debug = debug
        self.stop_after = stop_after
        self.nc = bass.Bass("TRN2", target_bir_lowering=False)
        self.dram = {}
        self.dbuf = {}

    def din(self, name, shape, dt=F32):
        self.dram[name] = self.nc.dram_tensor(name, list(shape), dt, kind="ExternalInput").ap()
        return self.dram[name]

    def dout(self, name, shape, dt=F32):
        self.dram[name] = self.nc.dram_tensor(name, list(shape), dt, kind="ExternalOutput").ap()
        return self.dram[name]

    def dscr(self, name, shape, dt):
        self.dram[name] = self.nc.dram_tensor(name, list(shape), dt, kind="Internal").ap()
        return self.dram[name]

    def op(self, eng, fn, reads=(), writes=()):
        return self.P.op(eng, fn, _bufs(reads), _bufs(writes))

    def dma(self, q, fn, reads=(), writes=(), key=None, group=None):
        if isinstance(key, T):
            key = key.b
        return self.P.dma(q, fn, _bufs(reads), _bufs(writes), key=key, group=group)

    def load(self, q, dst, dst_ap, src_ap, reads=(), group=None):
        return self.dma(q, lambda e: e.dma_start(out=dst_ap, in_=src_ap), reads=reads, writes=[dst], key=dst, group=group)

    def store(self, q, src, dst_ap, src_ap, writes=()):
        return self.dma(q, lambda e: e.dma_start(out=dst_ap, in_=src_ap), reads=[src], writes=writes, key=("st", id(src.b)))

    def build(self):
        cfg, nc = self.cfg, self.nc
        NM, NP_, NE, C, NRT = cfg.nmain, cfg.npre, cfg.ne, cfg.cap, cfg.nrt
        d = self.dram
        self.din("x_main", [NM * 128, D])
        self.din("x_pre", [NP_ * 128, D])
        self.din("pos_main", [128, NM], I32)
        self.din("pos_pre", [128, NP_], I32)
        self.din("flags_pre", [128, NP_])
        self.din("c_col", [128, KC])
        self.din("b_ada_bc", [128, 6 * D])
        self.din("n1g_fm", [128, KC])
        self.din("n2g_fm", [128, KC])
        self.din("fgain_bc", [128, D])
        self.din("pscale_fm", [128, 8])
        self.din("brt_bc", [128, NRT])
        self.din("w_ada", [D, 6 * D])
        self.din("w_in", [D, IN_W])
        self.din("w_pool", [4, 256, 256])
        self.din("w_bp", [1024, D])
        self.din("w_br", [1024, D])
        self.din("w_out", [D, D])
        self.din("w_rt", [D, NRT])
        self.din("w1", [NE * 512, KC * 256])
        self.din("w3", [NE * 512, KC * 256])
        self.din("w2", [NE * 512, FC * 512])
        self.din("iota_p", [128, 1])
        self.din("ident_f", [128, 128])
        self.din("bands", [128, 16, 128])
        self.din("causalT", [128, 128])
        self.din("qscaleT", [128, H, 128])
        self.din("kuscaleT", [128, H, 128])
        self.din("kdscale", [128, H])
        self.din("tri_u", [128, 128])
        self.din("iota_e", [128, NE])
        self.din("invf_bc", [128, 64])
        self.dout("out", [NM * 128, D])
        self.dscr("w_out_g", [D, D], BF16)
        self.dscr("w_in_b", [D, IN_W], BF16)
        self.dscr("w_bp_b", [1024, D], BF16)
        self.dscr("w_br_b", [1024, D], BF16)
        self.dscr("gate2_scr", [128, D], F32)
        self.dscr("x1_scr", [NM * 128, D], F32)
        self.dscr("xs_scr", [cfg.rows, D], BF16)
        self.dscr("ys_scr", [cfg.rows, D], F32)
        if self.debug:
            self.dout("dbg_x1", [NM * 128, D])
            self.dout("dbg_dest", [128, NM, 2], I32)
            self.dout("dbg_gw", [128, NM, 2])
            self.dout("dbg_mod", [128, 4, KC])
            self.dout("dbg_state", [128, H, 128])
        for n in ("w_out_g", "gate2_scr", "xs_scr", "ys_scr", "w_in_b", "w_bp_b", "w_br_b"):
            self.dbuf[n] = Buf(n)
        self.x1_bufs = [Buf("x1scr%d" % c) for c in range(NM)]
        self.dest_bufs = [Buf("dest%d" % c) for c in range(NM)]
        self.gw_bufs = [Buf("gw%d" % c) for c in range(NM)]
        self.rt_bufs = [Buf("rt%d" % c) for c in range(NM)]
        gam = [1.0 - 2.0 ** (-5.0 - h) for h in range(H)]
        self.cd = [float(np.float32(g ** 128)) for g in gam]

        with ExitStack() as es:
            self.es = es
            self.P = Prog(nc, es)
            P = self.P
            sbp = lambda n, sh, dt=F32: T(es.enter_context(nc.sbuf_tensor(n, list(sh), dt)), n)
            self.g1eff = sbp("g1eff", [128, KC]); self.shift1 = sbp("shift1", [128, KC])
            self.g2eff = sbp("g2eff", [128, KC]); self.shift2 = sbp("shift2", [128, KC])
            self.ident_f = sbp("ident_f_sb", [128, 128]); self.ident_b = sbp("ident_b_sb", [128, 128], BF16)
            self.epsb = sbp("epsb", [128, 1])
            self.state = sbp("state", [128, H, 128]); self.state_bf = sbp("state_bf", [128, H, 128], BF16)
            self.a_prev0 = sbp("a_prev0", [128, 1024], BF16)
            self.dest_all = sbp("dest_all", [128, NM, 2], I32); self.gw_all = sbp("gw_all", [128, NM, 2])
            self.destf_all = sbp("destf_all", [128, NM, 2]); self.rank_all = sbp("rank_all", [128, NM, 2]); self.eidx_all = sbp("eidx_all", [128, NM, 2])
            self.rstd2_all = sbp("rstd2_all", [128, NM]); self.slot_idx = sbp("slot_idx", [128, 4 * max(cfg.ns, 1)], I32)
            self.load("sp", self.ident_f, self.ident_f[:, :], d["ident_f"][:, :], group="c0")
            self.load("pool", self.ident_b, self.ident_b[:, :], d["ident_f"][:, :], group="c0p")
            self.op("dve", lambda e: e.memset(self.epsb[:, :], EPS), writes=[self.epsb])
            self.op("dve", lambda e: e.memset(self.state[:, :, :], 0.0), writes=[self.state])
            for i, ph in enumerate((self.phase0, self.phase1, self.phase2, self.phase3, self.phase4)):
                if i <= self.stop_after:
                    ph()
        return nc

    def bound_reg(self, e, val):
        key = (self.P.done_cnt, val)
        if getattr(self, "_breg_key", None) != key:
            r = e.alloc_register("idma_bound%d" % self.P.done_cnt)
            e.reg_mov(r, val)
            self._breg_key = key
            self._breg = r
        return self._breg

    def rstd_from_ssq(self, ssq_ap, ssq_t, rt, rstd, scale):
        self.op("act", lambda e: e.activation(out=rt[:, :], in_=ssq_ap, func=AF.Sqrt, scale=scale, bias=self.epsb[:, 0:1]),
                reads=[ssq_t, self.epsb], writes=[rt])
        self.op("dve", lambda e: e.reciprocal(out=rstd[:, :], in_=rt[:, :]), reads=[rt], writes=[rstd])

    def norm_transpose(self, x_ap, xt, junk, small, xnb, ptrs, hT, col0, q="sp"):
        ssq, rt, rstd = small
        self.load(q, xt, xt[:, :], x_ap)
        self.op("act", lambda e: e.activation(out=junk[:, :], in_=xt[:, :], func=AF.Square, accum_out=ssq[:, 0:1]),
                reads=[xt], writes=[junk, ssq])
        self.rstd_from_ssq(ssq[:, 0:1], ssq, rt, rstd, 1.0 / D)
        self.op("dve", lambda e: e.tensor_scalar(out=xnb[:, :], in0=xt[:, :], scalar1=rstd[:, 0:1], scalar2=None, op0=ALU.mult),
                reads=[xt, rstd], writes=[xnb])
        for kc in range(KC):
            pt = ptrs[kc // 8]
            self.op("pe", (lambda kc, pt: lambda e: e.transpose(out=pt[:, (kc % 8) * 128:(kc % 8 + 1) * 128], in_=xnb[:, kc * 128:(kc + 1) * 128], identity=self.ident_b[:, :]))(kc, pt),
                    reads=[xnb, self.ident_b], writes=[pt])
        self.evac_mod(ptrs, hT, col0, self.g1eff, self.shift1)

    def evac_mod(self, ptrs, dst, col0, geff, shift):
        for kc in range(KC):
            pt = ptrs[kc // 8]
            src = pt[:, (kc % 8) * 128:(kc % 8 + 1) * 128]
            out = dst[:, kc, col0:col0 + 128]
            if kc % 2 == 0:
                self.op("act", (lambda out, src, kc: lambda e: e.activation(out=out, in_=src, func=AF.Identity, scale=geff[:, kc:kc + 1], bias=shift[:, kc:kc + 1]))(out, src, kc),
                        reads=[pt, geff, shift], writes=[dst])
            else:
                self.op("dve", (lambda out, src, kc: lambda e: e.tensor_scalar(out=out, in0=src, scalar1=geff[:, kc:kc + 1], scalar2=shift[:, kc:kc + 1], op0=ALU.mult, op1=ALU.add))(out, src, kc),
                        reads=[pt, geff, shift], writes=[dst])

    def trig(self, pos_f, col, tw, cos_t, sin_t):
        invf = self.invf
        ang, y, ki, kf, ra, r1, rs, c1, tt, c2 = (tw[n] for n in ("ang", "y", "ki", "kf", "ra", "r1", "rs", "c1", "tt", "c2"))
        o = self.op
        o("dve", lambda e: e.tensor_scalar(out=ang[:, :], in0=invf[:, :], scalar1=pos_f[:, col:col + 1], scalar2=None, op0=ALU.mult), reads=[invf, pos_f], writes=[ang])
        o("dve", lambda e: e.tensor_scalar(out=y[:, :], in0=ang[:, :], scalar1=1.0 / TWO_PI, scalar2=None, op0=ALU.mult), reads=[ang], writes=[y])
        o("dve", lambda e: e.tensor_copy(out=ki[:, :], in_=y[:, :]), reads=[y], writes=[ki])
        o("dve", lambda e: e.tensor_copy(out=kf[:, :], in_=ki[:, :]), reads=[ki], writes=[kf])
        o("dve", lambda e: e.scalar_tensor_tensor(out=ra[:, :], in0=kf[:, :], scalar=-CW1, in1=ang[:, :], op0=ALU.mult, op1=ALU.add), reads=[kf, ang], writes=[ra])
        o("dve", lambda e: e.scalar_tensor_tensor(out=r1[:, :], in0=kf[:, :], scalar=-CW2, in1=ra[:, :], op0=ALU.mult, op1=ALU.add), reads=[kf, ra], writes=[r1])
        o("dve", lambda e: e.tensor_scalar(out=rs[:, :], in0=r1[:, :], scalar1=PI_LO, scalar2=-PI_LO, op0=ALU.min, op1=ALU.max), reads=[r1], writes=[rs])
        o("act", lambda e: e.activation(out=sin_t[:, :], in_=rs[:, :], func=AF.Sin), reads=[rs], writes=[sin_t])
        o("dve", lambda e: e.tensor_scalar(out=c1[:, :], in0=r1[:, :], scalar1=math.pi / 2, scalar2=None, op0=ALU.add), reads=[r1], writes=[c1])
        o("dve", lambda e: e.tensor_scalar(out=tt[:, :], in0=c1[:, :], scalar1=math.pi, scalar2=-TWO_PI, op0=ALU.is_gt, op1=ALU.mult), reads=[c1], writes=[tt])
        o("dve", lambda e: e.tensor_tensor(out=c2[:, :], in0=c1[:, :], in1=tt[:, :], op=ALU.add), reads=[c1, tt], writes=[c2])
        o("dve", lambda e: e.tensor_scalar(out=c2[:, :], in0=c2[:, :], scalar1=PI_LO, scalar2=-PI_LO, op0=ALU.min, op1=ALU.max), reads=[c2], writes=[c2])
        o("act", lambda e: e.activation(out=cos_t[:, :], in_=c2[:, :], func=AF.Sin), reads=[c2], writes=[cos_t])

    def rotary(self, pt, nh, cos_t, sin_t, tring, dst, dcol0):
        pv = pt[:, 0:nh * 128].rearrange("p (h t f) -> p h t f", h=nh, t=2)
        dv = dst[:, dcol0:dcol0 + nh * 128].rearrange("p (h t f) -> p h t f", h=nh, t=2)
        cb = cos_t[:, :].unsqueeze(1).to_broadcast([128, nh, 64])
        sb_ = sin_t[:, :].unsqueeze(1).to_broadcast([128, nh, 64])
        o = self.op
        for half in (0, 1):
            tA = tring.next()
            tB = tring.next()
            a3 = tA[:, 0:nh * 64].rearrange("p (h f) -> p h f", h=nh)
            b3 = tB[:, 0:nh * 64].rearrange("p (h f) -> p h f", h=nh)
            o("dve", (lambda half, a3: lambda e: e.tensor_tensor(out=a3, in0=pv[:, :, half, :], in1=cb, op=ALU.mult))(half, a3), reads=[pt, cos_t], writes=[tA])
            o("dve", (lambda half, b3: lambda e: e.tensor_tensor(out=b3, in0=pv[:, :, 1 - half, :], in1=sb_, op=ALU.mult))(half, b3), reads=[pt, sin_t], writes=[tB])
            opc = ALU.subtract if half == 0 else ALU.add
            o("pool", (lambda half, opc, a3, b3: lambda e: e.tensor_tensor(out=dv[:, :, half, :], in0=a3, in1=b3, op=opc))(half, opc, a3, b3), reads=[tA, tB], writes=[dst])

    def phase0(self):
        nc, d, o = self.nc, self.dram, self.op
        with ExitStack() as pes:
            sb = lambda n, sh, dt=F32: T(pes.enter_context(nc.sbuf_tensor("p0_" + n, list(sh), dt)), n)
            ps = lambda n, sh, dt=F32: T(pes.enter_context(nc.psum_tensor("p0_" + n, list(sh), dt)), n)
            c_col = sb("c_col", [128, KC]); c_act = sb("c_act", [128, KC])
            ones_f = sb("ones_f", [128, 128]); cbc = sb("cbc", [128, KC, 128])
            mod_bc = sb("mod_bc", [128, 6 * D])
            n1g = sb("n1g", [128, KC]); n2g = sb("n2g", [128, KC])
            self.load("sp", c_col, c_col[:, :], d["c_col"][:, :])
            self.load("sp", n1g, n1g[:, :], d["n1g_fm"][:, :])
            self.load("sp", n2g, n2g[:, :], d["n2g_fm"][:, :])
            o("act", lambda e: e.activation(out=c_act[:, :], in_=c_col[:, :], func=AF.Silu), reads=[c_col], writes=[c_act])
            o("pool", lambda e: e.memset(ones_f[:, :], 1.0), writes=[ones_f])
            for kc in range(KC):
                o("dve", (lambda kc: lambda e: e.tensor_scalar(out=cbc[:, kc, :], in0=ones_f[:, :], scalar1=c_act[:, kc:kc + 1], scalar2=None, op0=ALU.mult))(kc),
                  reads=[ones_f, c_act], writes=[cbc])
            wring = Ring([sb("wada%d" % i, [128, KC, 256]) for i in range(2)])
            bring = Ring([sb("bada%d" % i, [128, 256]) for i in range(2)])
            pring = Ring([ps("pada%d" % i, [128, 512]) for i in range(2)])
            for j in range(6 * D // 256):
                w = wring.next(); b = bring.next(); pt = pring.next()
                self.load("sp", w, w[:, :, :], d["w_ada"][:, j * 256:(j + 1) * 256].rearrange("(k p) n -> p k n", p=128))
                self.load("sp", b, b[:, :], d["b_ada_bc"][:, j * 256:(j + 1) * 256])
                for kc in range(KC):
                    o("pe", (lambda kc, w, pt: lambda e: e.matmul(out=pt[:, 0:256], lhsT=cbc[:, kc, :], rhs=w[:, kc, :], start=(kc == 0), stop=(kc == KC - 1)))(kc, w, pt),
                      reads=[cbc, w], writes=[pt])
                o("dve", (lambda j, pt, b: lambda e: e.tensor_tensor(out=mod_bc[:, j * 256:(j + 1) * 256], in0=pt[:, 0:256], in1=b[:, :], op=ALU.add))(j, pt, b),
                  reads=[pt, b], writes=[mod_bc])
            tmp = sb("diag_tmp", [128, KC, 128])
            fm = [sb("fm%d" % i, [128, KC]) for i in range(6)]
            identb3 = self.ident_f[:, :].unsqueeze(1).to_broadcast([128, KC, 128])
            for i in (0, 1, 3, 4):
                o("dve", (lambda i: lambda e: e.tensor_tensor(out=tmp[:, :, :], in0=mod_bc[:, i * D:(i + 1) * D].rearrange("p (k n) -> p k n", k=KC), in1=identb3, op=ALU.mult))(i),
                  reads=[mod_bc, self.ident_f], writes=[tmp])
                o("dve", (lambda i: lambda e: e.tensor_reduce(out=fm[i][:, :], in_=tmp[:, :, :], axis=AX.X, op=ALU.add))(i), reads=[tmp], writes=[fm[i]])
            o("dve", lambda e: e.tensor_copy(out=self.shift1[:, :], in_=fm[0][:, :]), reads=[fm[0]], writes=[self.shift1])
            o("dve", lambda e: e.scalar_tensor_tensor(out=self.g1eff[:, :], in0=fm[1][:, :], scalar=1.0, in1=n1g[:, :], op0=ALU.add, op1=ALU.mult), reads=[fm[1], n1g], writes=[self.g1eff])
            o("dve", lambda e: e.tensor_copy(out=self.shift2[:, :], in_=fm[3][:, :]), reads=[fm[3]], writes=[self.shift2])
            o("dve", lambda e: e.scalar_tensor_tensor(out=self.g2eff[:, :], in0=fm[4][:, :], scalar=1.0, in1=n2g[:, :], op0=ALU.add, op1=ALU.mult), reads=[fm[4], n2g], writes=[self.g2eff])
            if self.debug:
                for i, t in enumerate((self.g1eff, self.shift1, self.g2eff, self.shift2)):
                    self.store("sp", t, d["dbg_mod"][:, i, :], t[:, :])
            self.dma("sp", lambda e: e.dma_start(out=d["gate2_scr"][:, :], in_=mod_bc[:, 5 * D:6 * D]), reads=[mod_bc], writes=[self.dbuf["gate2_scr"]], key=("st", "g2"))
            woring = Ring([sb("wo%d" % i, [128, D]) for i in range(2)])
            wgring = Ring([sb("wg%d" % i, [128, D], BF16) for i in range(2)])
            for kc in range(KC):
                wo = woring.next(); wg = wgring.next()
                self.load("sp", wo, wo[:, :], d["w_out"][kc * 128:(kc + 1) * 128, :])
                o("dve", (lambda wo, wg: lambda e: e.tensor_tensor(out=wg[:, :], in0=wo[:, :], in1=mod_bc[:, 2 * D:3 * D], op=ALU.mult))(wo, wg), reads=[wo, mod_bc], writes=[wg])
                self.store("sp", wg, d["w_out_g"][kc * 128:(kc + 1) * 128, :], wg[:, :], writes=[self.dbuf["w_out_g"]])
            self.P.emit()

    def load_consts_ret(self, sb, need_q):
        d = self.dram
        self.invf = sb("invf", [128, 64])
        self.load("sp", self.invf, self.invf[:, :], d["invf_bc"][:, :], group="c1")
        self.kdscale = sb("kdscale", [128, H])
        self.load("sp", self.kdscale, self.kdscale[:, :], d["kdscale"][:, :], group="c1")

    def make_trig_work(self, sb, tag):
        tw = {}
        for n in ("ang", "y", "kf", "ra", "r1", "rs", "c1", "tt", "c2"):
            tw[n] = sb(tag + n, [128, 64])
        tw["ki"] = sb(tag + "ki", [128, 64], I32)
        return tw

    def phase1(self):
        nc, d, o, cfg = self.nc, self.dram, self.op, self.cfg
        NP_ = cfg.npre
        if NP_ == 0:
            return
        with ExitStack() as pes:
            sb = lambda n, sh, dt=F32: T(pes.enter_context(nc.sbuf_tensor("p1_" + n, list(sh), dt)), n)
            ps = lambda n, sh, dt=F32: T(pes.enter_context(nc.psum_tensor("p1_" + n, list(sh), dt)), n)
            self.load_consts_ret(sb, False)
            wkv = sb("wkv", [128, KC, 2048], BF16)
            wa = sb("wa", [128, KC, 1024], BF16)
            for j in range(4):
                self.load("pool", wkv, wkv[:, :, j * 512:(j + 1) * 512], d["w_in"][:, OFF_K + j * 512:OFF_K + (j + 1) * 512].rearrange("(k p) n -> p k n", p=128), group="c1p")
            for j in range(2):
                self.load("pool", wa, wa[:, :, j * 512:(j + 1) * 512], d["w_in"][:, OFF_A + j * 512:OFF_A + (j + 1) * 512].rearrange("(k p) n -> p k n", p=128), group="c1p")
            pos_i = sb("pos_i", [128, NP_], I32); pos_f = sb("pos_f", [128, NP_]); flags = sb("flags", [128, NP_])
            self.load("sp", pos_i, pos_i[:, :], d["pos_pre"][:, :], group="c1")
            self.load("sp", flags, flags[:, :], d["flags_pre"][:, :], group="c1")
            o("dve", lambda e: e.tensor_copy(out=pos_f[:, :], in_=pos_i[:, :]), reads=[pos_i], writes=[pos_f])
            xring = Ring([sb("x%d" % i, [128, D]) for i in range(2)])
            smalls = Ring([(sb("ssq%d" % i, [128, 1]), sb("rt%d" % i, [128, 1]), sb("rstd%d" % i, [128, 1])) for i in range(2)])
            xnbs = Ring([sb("xnb%d" % i, [128, D], BF16) for i in range(2)])
            hTs = Ring([sb("hT%d" % i, [128, KC, 128], BF16) for i in range(2)])
            ptrs = [ps("ptr%d" % i, [128, 1024], BF16) for i in range(2)]
            pfr = Ring([ps("pf%d" % i, [128, 512]) for i in range(6)])
            tws = Ring([self.make_trig_work(sb, "tw%d" % i) for i in range(2)])
            coss = Ring([sb("cos%d" % i, [128, 64]) for i in range(2)])
            sins = Ring([sb("sin%d" % i, [128, 64]) for i in range(2)])
            tring = Ring([sb("rt_tmp%d" % i, [128, 256]) for i in range(8)])
            krots = Ring([sb("krot%d" % i, [128, 1024], BF16) for i in range(2)])
            kds = Ring([sb("kd%d" % i, [128, 1024], BF16) for i in range(2)])
            vbs = Ring([sb("vb%d" % i, [128, 1024], BF16) for i in range(2)])
            kdb = self.kdscale[:, :].unsqueeze(2).to_broadcast([128, H, 128])
            cvr = Ring([sb("cv%d" % i, [128, KC, 256], BF16) for i in range(2)])
            zt = sb("zeros", [128, D], BF16)
            o("pool", lambda e: e.memset(zt[:, :], 0.0), writes=[zt])
            jobs = []

            def conv_w_in(j):
                cv = cvr.next()
                self.load("pool", cv, cv[:, :, :], d["w_in"][:, j * 256:(j + 1) * 256].rearrange("(k p) n -> p k n", p=128))
                self.store("sp", cv, d["w_in_b"][:, j * 256:(j + 1) * 256].rearrange("(k p) n -> p k n", p=128), cv[:, :, :], writes=[self.dbuf["w_in_b"]])

            def conv_w_b(nm, j):
                cv = cvr.next()
                cvv = cv[:, :, :].rearrange("p k n -> p (k n)").rearrange("p (k n) -> p k n", k=8)
                self.load("pool", cv, cvv, d[nm][:, j * 512:(j + 1) * 512].rearrange("(k p) n -> p k n", p=128))
                self.store("sp", cv, d[nm + "_b"][:, j * 512:(j + 1) * 512].rearrange("(k p) n -> p k n", p=128), cvv, writes=[self.dbuf[nm + "_b"]])

            def zero_rows(r0, nr):
                for q4 in range(nr // 128):
                    self.dma("sp", (lambda r: lambda e: e.dma_start(out=d["xs_scr"][r:r + 128, :], in_=zt[:, :]))(r0 + q4 * 128), reads=[zt], writes=[self.dbuf["xs_scr"]], group="zero")

            for j in range(IN_W // 256):
                jobs.append((conv_w_in, (j,)))
            for nm in ("w_bp", "w_br"):
                for j in range(4):
                    jobs.append((conv_w_b, (nm, j)))
            for r0 in range(0, cfg.rows, 512):
                jobs.append((zero_rows, (r0, min(512, cfg.rows - r0))))
            per_chunk = -(-len(jobs) // NP_)

            def pre_chunk(p):
                for _ in range(per_chunk):
                    if jobs:
                        fn, args = jobs.pop(0)
                        fn(*args)
                xt = xring.next(); small = smalls.next(); xnb = xnbs.next(); hT = hTs.next()
                self.norm_transpose(d["x_pre"][p * 128:(p + 1) * 128, :], xt, xnb, small, xnb, ptrs, hT, 0)
                cos_t = coss.next(); sin_t = sins.next()
                self.trig(pos_f, p, tws.next(), cos_t, sin_t)
                krot = krots.next(); kd = kds.next(); vb = vbs.next()
                for j in range(2):
                    pk = pfr.next()
                    for kc in range(KC):
                        o("pe", (lambda j, kc, pk: lambda e: e.matmul(out=pk[:, :], lhsT=hT[:, kc, :], rhs=wkv[:, kc, j * 512:(j + 1) * 512], start=(kc == 0), stop=(kc == KC - 1)))(j, kc, pk),
                          reads=[hT, wkv], writes=[pk])
                    self.rotary(pk, 4, cos_t, sin_t, tring, krot, j * 512)
                for j in range(2):
                    pv = pfr.next()
                    for kc in range(KC):
                        o("pe", (lambda j, kc, pv: lambda e: e.matmul(out=pv[:, :], lhsT=hT[:, kc, :], rhs=wkv[:, kc, 1024 + j * 512:1024 + (j + 1) * 512], start=(kc == 0), stop=(kc == KC - 1)))(j, kc, pv),
                          reads=[hT, wkv], writes=[pv])
                    o("act", (lambda j, pv: lambda e: e.activation(out=vb[:, j * 512:(j + 1) * 512], in_=pv[:, :], func=AF.Copy, scale=flags[:, p:p + 1]))(j, pv),
                      reads=[pv, flags], writes=[vb])
                o("pool", lambda e: e.tensor_tensor(out=kd[:, :].rearrange("p (h f) -> p h f", h=H), in0=krot[:, :].rearrange("p (h f) -> p h f", h=H), in1=kdb, op=ALU.mult),
                  reads=[krot, self.kdscale], writes=[kd])
                self.state_update(kd, vb, [pfr.next(), pfr.next()])
                if p == NP_ - 1:
                    for j in range(2):
                        pa = pfr.next()
                        for kc in range(KC):
                            o("pe", (lambda j, kc, pa: lambda e: e.matmul(out=pa[:, :], lhsT=hT[:, kc, :], rhs=wa[:, kc, j * 512:(j + 1) * 512], start=(kc == 0), stop=(kc == KC - 1)))(j, kc, pa),
                              reads=[hT, wa], writes=[pa])
                        o("act", (lambda j, pa: lambda e: e.activation(out=self.a_prev0[:, j * 512:(j + 1) * 512], in_=pa[:, :], func=AF.Copy))(j, pa),
                          reads=[pa], writes=[self.a_prev0])

            for p in range(NP_):
                pre_chunk(p)
            if self.debug:
                self.dma("sp", lambda e: e.dma_start(out=d["dbg_state"][:, :, :], in_=self.state[:, :, :]), reads=[self.state], key=("st", "dbgstate1"))
            self.P.emit()

    def state_update(self, kd, vb, pst):
        o = self.op
        for h in range(H):
            pt = pst[h // 4]
            o("pe", (lambda h, pt: lambda e: e.matmul(out=pt[:, (h % 4) * 128:(h % 4 + 1) * 128], lhsT=kd[:, h * 128:(h + 1) * 128], rhs=vb[:, h * 128:(h + 1) * 128], start=True, stop=True))(h, pt),
              reads=[kd, vb], writes=[pt])
        for h in range(H):
            pt = pst[h // 4]
            o("dve", (lambda h, pt: lambda e: e.scalar_tensor_tensor(out=self.state[:, h, :], in0=self.state[:, h, :], scalar=self.cd[h], in1=pt[:, (h % 4) * 128:(h % 4 + 1) * 128], op0=ALU.mult, op1=ALU.add))(h, pt),
              reads=[self.state, pt], writes=[self.state])

    def phase2(self):
        nc, d, o, cfg = self.nc, self.dram, self.op, self.cfg
        NM, TCH, NE, C, NRT = cfg.nmain, cfg.tch, cfg.ne, cfg.cap, cfg.nrt
        TT = TCH * 128
        with ExitStack() as pes:
            sb = lambda n, sh, dt=F32: T(pes.enter_context(nc.sbuf_tensor("p2_" + n, list(sh), dt)), n)
            ps = lambda n, sh, dt=F32: T(pes.enter_context(nc.psum_tensor("p2_" + n, list(sh), dt)), n)
            self.load_consts_ret(sb, True)
            bands = sb("bands", [128, 16, 128], BF16)
            self.load("pool", bands, bands[:, :, :], d["bands"][:, :, :], group="c2p")
            causal = sb("causal", [128, 128]); qsc = sb("qsc", [128, H, 128]); kusc = sb("kusc", [128, H, 128])
            tri = sb("tri", [128, 128], BF16); ones_b = sb("ones_b", [128, 128], BF16)
            iota_e = sb("iota_e", [128, NE]); brt = sb("brt", [128, NRT]); pscale = sb("pscale", [128, 8])
            wrt = sb("wrt", [128, KC, NRT]); wpool = sb("wpool", [128, 8, 256], BF16)
            pos_i = sb("pos_i", [128, NM], I32); pos_f = sb("pos_f", [128, NM])
            for t, src in ((causal, d["causalT"][:, :]), (qsc, d["qscaleT"][:, :, :]), (kusc, d["kuscaleT"][:, :, :]), (iota_e, d["iota_e"][:, :]),
                           (brt, d["brt_bc"][:, :]), (pscale, d["pscale_fm"][:, :]), (pos_i, d["pos_main"][:, :]),
                           (wrt, d["w_rt"][:, :].rearrange("(k p) n -> p k n", p=128))):
                self.load("sp", t, t[(slice(None),) * len(src.shape)], src, group="c2")
            self.load("pool", tri, tri[:, :], d["tri_u"][:, :], group="c2p")
            self.load("pool", wpool, wpool[:, :, :], d["w_pool"][:, :, :].rearrange("g (hh p) n -> p (g hh) n", p=128), group="c2p")
            o("pool", lambda e: e.memset(ones_b[:, :], 1.0), writes=[ones_b])
            o("dve", lambda e: e.tensor_copy(out=pos_f[:, :], in_=pos_i[:, :]), reads=[pos_i], writes=[pos_f])
            o("pool", lambda e: e.tensor_copy(out=self.state_bf[:, :, :], in_=self.state[:, :, :]), reads=[self.state], writes=[self.state_bf])
            msum = sb("msum", [128, NE], BF16)
            o("dve", lambda e: e.memset(msum[:, :], 0.0), writes=[msum])
            xring = Ring([sb("x%d" % i, [128, D]) for i in range(2)])
            smalls = Ring([(sb("ssq%d" % i, [128, 1]), sb("rt%d" % i, [128, 1]), sb("rstd%d" % i, [128, 1])) for i in range(2)])
            xnbs = Ring([sb("xnb%d" % i, [128, D], BF16) for i in range(2)])
            hT = sb("hT", [128, KC, TT], BF16)
            ptrs = [ps("ptr%d" % i, [128, 1024], BF16) for i in range(2)]
            pf = Ring([ps("pf%d" % i, [128, 512]) for i in range(6)])
            wr = Ring([sb("wslot%d" % i, [128, 4096], BF16) for i in range(3)])
            tws = Ring([self.make_trig_work(sb, "tw%d" % i) for i in range(1)])
            cos_l = [sb("cos%d" % i, [128, 64]) for i in range(TCH)]
            sin_l = [sb("sin%d" % i, [128, 64]) for i in range(TCH)]
            tring = Ring([sb("rt_tmp%d" % i, [128, 128]) for i in range(8)])
            a_ring = Ring([sb("a_tok%d" % i, [128, 1024], BF16) for i in range(TCH + 1)])
            bufA = sb("bufA", [128, 8, TT], BF16)
            bufB = sb("bufB", [128, 8, TT], BF16)
            mgT = sb("mgT", [128, KC, TT], BF16)
            sgt_r = Ring([sb("sgt%d" % i, [128, TT]) for i in range(2)])
            tmp2_r = Ring([sb("tmp2_%d" % i, [128, TT], BF16) for i in range(2)])
            qrot = [sb("qrot%d" % i, [128, 1024], BF16) for i in range(TCH)]
            krot = [sb("krot%d" % i, [128, 1024], BF16) for i in range(TCH)]
            kd_l = [sb("kd%d" % i, [128, 1024], BF16) for i in range(TCH)]
            v_l = [sb("v%d" % i, [128, 1024], BF16) for i in range(TCH)]
            srg_l = [sb("srg%d" % i, [128, 1024], BF16) for i in range(TCH)]
            qdT = sb("qdT", [128, H, TT], BF16); kuT = sb("kuT", [128, H, TT], BF16); retT = sb("retT", [128, H, TT], BF16)
            sbf = sb("sbf", [128, H, 128], BF16)
            sqj = sb("sqj", [128, 128], BF16); ont = sb("ont", [128, 1024]); ret_tok = sb("ret_tok", [128, 1024], BF16)
            ssqh = sb("ssqh", [128, H]); rth = sb("rth", [128, H]); rstdh = sb("rstdh", [128, H])
            xp_r = Ring([sb("xp%d" % i, [128, 256]) for i in range(4)])
            xnp_r = Ring([sb("xnp%d" % i, [128, 256]) for i in range(4)])
            junk2 = sb("junk2", [128, 256], BF16)
            ssq2 = [sb("ssq2_%d" % i, [128, 8]) for i in range(TCH)]
            x1t_r = xring
            xn2bf_r = xnbs
            h2T_r = Ring([sb("h2T%d" % i, [128, 4, 128]) for i in range(2)])
            rsm = {n: sb("rs_" + n, [128, w]) for n, w in (("ssum", 1), ("rt2", 1), ("rstd2", 1), ("lg", 4), ("gmax", 1), ("ngmax", 1), ("ohg", 4), ("eg", 4), ("sume", 1), ("pgrp", 1),
                                                               ("pen", 4), ("lem", NE), ("m1", 1), ("oh1", NE), ("lem2", NE), ("m2", 1), ("oh2", NE), ("dd", 1), ("ed", 1), ("den", 1), ("rr", 1),
                                                               ("mm", NE), ("prod", NE), ("rank", 2), ("eidx", 2), ("ovf", 2), ("destf", 2), ("pfx", NE))}
            mbf = sb("mbf", [128, NE], BF16)
            sidx_r = Ring([sb("sidx%d" % i, [128, 2], I32) for i in range(4)])
            kdb = self.kdscale[:, :].unsqueeze(2).to_broadcast([128, H, 128])
            self.p2_sbuf_left = nc.sbuf_bytes_remaining

            def wload(q, src_ap, k, reads=()):
                slot = wr.next()
                view = slot[:, :].rearrange("p (k n) -> p k n", k=k)
                self.load(q, slot, view, src_ap, reads=reads)
                return slot, view

            def tile_body(ti, a_prev):
                gch = [ti * TCH + lc for lc in range(TCH)]
                for lc, c in enumerate(gch):
                    xnb_ = xnbs.next()
                    self.norm_transpose(d["x_main"][c * 128:(c + 1) * 128, :], xring.next(), xnb_, smalls.next(), xnb_, ptrs, hT, lc * 128)
                    self.trig(pos_f, c, tws.next(), cos_l[lc], sin_l[lc])
                a_l = [a_ring.next() for _ in range(TCH)]
                for u in range(4):
                    slot, wv = wload("sp", d["w_in_b"][:, OFF_A + u * 256:OFF_A + (u + 1) * 256].rearrange("(k p) n -> p k n", p=128), KC, reads=[self.dbuf["w_in_b"]])
                    for lc in range(TCH):
                        pt = pf.next()
                        for kc in range(KC):
                            o("pe", (lambda kc, pt, wv, lc: lambda e: e.matmul(out=pt[:, 0:256], lhsT=hT[:, kc, lc * 128:(lc + 1) * 128], rhs=wv[:, kc, :], start=(kc == 0), stop=(kc == KC - 1)))(kc, pt, wv, lc),
                              reads=[hT, slot], writes=[pt])
                        o("act", (lambda pt, lc, u: lambda e: e.activation(out=a_l[lc][:, u * 256:(u + 1) * 256], in_=pt[:, 0:256], func=AF.Copy))(pt, lc, u), reads=[pt], writes=[a_l[lc]])
                for lc, c in enumerate(gch):
                    var = 8 if c == 0 else 0
                    acur = a_l[lc]; aprv = a_prev if lc == 0 else a_l[lc - 1]
                    for jb in range(2):
                        pt = pf.next()
                        for q4 in range(4):
                            j = jb * 4 + q4; g = j // 2
                            o("pe", (lambda pt, q4, j, g, acur, var: lambda e: e.matmul(out=pt[:, q4 * 128:(q4 + 1) * 128], lhsT=acur[:, j * 128:(j + 1) * 128], rhs=bands[:, var + g * 2, :], start=True, stop=False))(pt, q4, j, g, acur, var),
                              reads=[acur, bands], writes=[pt])
                            o("pe", (lambda pt, q4, j, g, aprv, var: lambda e: e.matmul(out=pt[:, q4 * 128:(q4 + 1) * 128], lhsT=aprv[:, j * 128:(j + 1) * 128], rhs=bands[:, var + g * 2 + 1, :], start=False, stop=True))(pt, q4, j, g, aprv, var),
                              reads=[aprv, bands], writes=[pt])
                        o("dve", (lambda pt, jb, lc: lambda e: e.tensor_copy(out=bufA[:, jb * 4:(jb + 1) * 4, lc * 128:(lc + 1) * 128], in_=pt[:, :].rearrange("p (j t) -> p j t", j=4)))(pt, jb, lc),
                          reads=[pt], writes=[bufA])
                a_prev_next = a_l[TCH - 1]
                for jo in range(8):
                    g, dh = jo // 2, jo % 2
                    pt = pf.next()
                    for hh in range(2):
                        o("pe", (lambda pt, g, dh, hh: lambda e: e.matmul(out=pt[:, 0:TT], lhsT=wpool[:, g * 2 + hh, dh * 128:(dh + 1) * 128], rhs=bufA[:, g * 2 + hh, :], start=(hh == 0), stop=(hh == 1)))(pt, g, dh, hh),
                          reads=[wpool, bufA], writes=[pt])
                    o("act", (lambda pt, jo: lambda e: e.activation(out=bufB[:, jo, :], in_=pt[:, 0:TT], func=AF.Copy, scale=pscale[:, jo:jo + 1]))(pt, jo), reads=[pt, pscale], writes=[bufB])
                self.gated_branch(OFF_GA, "w_bp", bufB, mgT, True, wload, pf, hT, sgt_r, tmp2_r, TT)
                for which, off, rot_l in (("q", OFF_Q, qrot), ("k", OFF_K, krot)):
                    for u in range(4):
                        slot, wv = wload("sp", d["w_in_b"][:, off + u * 256:off + (u + 1) * 256].rearrange("(k p) n -> p k n", p=128), KC, reads=[self.dbuf["w_in_b"]])
                        for lc in range(TCH):
                            pt = pf.next()
                            for kc in range(KC):
                                o("pe", (lambda kc, pt, wv, lc: lambda e: e.matmul(out=pt[:, 0:256], lhsT=hT[:, kc, lc * 128:(lc + 1) * 128], rhs=wv[:, kc, :], start=(kc == 0), stop=(kc == KC - 1)))(kc, pt, wv, lc),
                                  reads=[hT, slot], writes=[pt])
                            self.rotary(pt, 2, cos_l[lc], sin_l[lc], tring, rot_l[lc], u * 256)
                for lc in range(TCH):
                    for rot_l, dstT, sc in ((qrot, qdT, qsc), (krot, kuT, kusc)):
                        pt = ptrs[0] if rot_l is qrot else ptrs[1]
                        for h in range(H):
                            o("pe", (lambda pt, h, r: lambda e: e.transpose(out=pt[:, h * 128:(h + 1) * 128], in_=r[:, h * 128:(h + 1) * 128], identity=self.ident_b[:, :]))(pt, h, rot_l[lc]),
                              reads=[rot_l[lc], self.ident_b], writes=[pt])
                        o("dve", (lambda pt, dstT, sc, lc: lambda e: e.tensor_tensor(out=dstT[:, :, lc * 128:(lc + 1) * 128], in0=pt[:, :].rearrange("p (h t) -> p h t", h=H), in1=sc[:, :, :], op=ALU.mult))(pt, dstT, sc, lc),
                          reads=[pt, sc], writes=[dstT])
                    o("pool", (lambda lc: lambda e: e.tensor_tensor(out=kd_l[lc][:, :].rearrange("p (h f) -> p h f", h=H), in0=krot[lc][:, :].rearrange("p (h f) -> p h f", h=H), in1=kdb, op=ALU.mult))(lc),
                      reads=[krot[lc], self.kdscale], writes=[kd_l[lc]])
                for off, dst_l, fn in ((OFF_V, v_l, AF.Copy), (OFF_RG, srg_l, AF.Silu)):
                    for u in range(4):
                        slot, wv = wload("sp", d["w_in_b"][:, off + u * 256:off + (u + 1) * 256].rearrange("(k p) n -> p k n", p=128), KC, reads=[self.dbuf["w_in_b"]])
                        for lc in range(TCH):
                            pt = pf.next()
                            for kc in range(KC):
                                o("pe", (lambda kc, pt, wv, lc: lambda e: e.matmul(out=pt[:, 0:256], lhsT=hT[:, kc, lc * 128:(lc + 1) * 128], rhs=wv[:, kc, :], start=(kc == 0), stop=(kc == KC - 1)))(kc, pt, wv, lc),
                                  reads=[hT, slot], writes=[pt])
                            o("act", (lambda pt, lc, u, dst_l, fn: lambda e: e.activation(out=dst_l[lc][:, u * 256:(u + 1) * 256], in_=pt[:, 0:256], func=fn))(pt, lc, u, dst_l, fn), reads=[pt], writes=[dst_l[lc]])
                def ret_chunk(lc):
                    cs = slice(lc * 128, (lc + 1) * 128)
                    pS = [pf.next(), pf.next()]
                    for h in range(H):
                        o("pe", (lambda h, cs: lambda e: e.matmul(out=pS[h // 4][:, (h % 4) * 128:(h % 4 + 1) * 128], lhsT=kuT[:, h, cs], rhs=qdT[:, h, cs], start=True, stop=True))(h, cs),
                          reads=[kuT, qdT], writes=[pS[h // 4]])
                    for jb in range(2):
                        o("dve", (lambda jb: lambda e: e.tensor_tensor(out=sbf[:, jb * 4:(jb + 1) * 4, :], in0=pS[jb][:, :].rearrange("p (h t) -> p h t", h=4), in1=causal[:, :].unsqueeze(1).to_broadcast([128, 4, 128]), op=ALU.mult))(jb),
                          reads=[pS[jb], causal], writes=[sbf])
                    pO = [pf.next(), pf.next()]
                    for h in range(H):
                        o("pe", (lambda h: lambda e: e.matmul(out=pO[h // 4][:, (h % 4) * 128:(h % 4 + 1) * 128], lhsT=sbf[:, h, :], rhs=v_l[lc][:, h * 128:(h + 1) * 128], start=True, stop=False))(h),
                          reads=[sbf, v_l[lc]], writes=[pO[h // 4]])
                        o("pe", (lambda h, cs: lambda e: e.matmul(out=pO[h // 4][:, (h % 4) * 128:(h % 4 + 1) * 128], lhsT=qdT[:, h, cs], rhs=self.state_bf[:, h, :], start=False, stop=True))(h, cs),
                          reads=[qdT, self.state_bf], writes=[pO[h // 4]])
                    for h in range(H):
                        o("act", (lambda h: lambda e: e.activation(out=sqj[:, :], in_=pO[h // 4][:, (h % 4) * 128:(h % 4 + 1) * 128], func=AF.Square, accum_out=ssqh[:, h:h + 1]))(h),
                          reads=[pO[h // 4]], writes=[sqj, ssqh])
                    self.rstd_from_ssq(ssqh[:, :], ssqh, rth, rstdh, 1.0 / 128)
                    for jb in range(2):
                        o("dve", (lambda jb: lambda e: e.tensor_tensor(out=ont[:, jb * 512:(jb + 1) * 512].rearrange("p (h f) -> p h f", h=4), in0=pO[jb][:, :].rearrange("p (h f) -> p h f", h=4),
                                                                      in1=rstdh[:, jb * 4:(jb + 1) * 4].unsqueeze(2).to_broadcast([128, 4, 128]), op=ALU.mult))(jb),
                          reads=[pO[jb], rstdh], writes=[ont])
                    o("pool", (lambda lc: lambda e: e.tensor_tensor(out=ret_tok[:, :], in0=ont[:, :], in1=srg_l[lc][:, :], op=ALU.mult))(lc), reads=[ont, srg_l[lc]], writes=[ret_tok])
                    pt = ptrs[0]
                    for h in range(H):
                        o("pe", (lambda pt, h: lambda e: e.transpose(out=pt[:, h * 128:(h + 1) * 128], in_=ret_tok[:, h * 128:(h + 1) * 128], identity=self.ident_b[:, :]))(pt, h),
                          reads=[ret_tok, self.ident_b], writes=[pt])
                    o("act", (lambda pt, cs: lambda e: e.activation(out=retT[:, :, cs], in_=pt[:, :].rearrange("p (h t) -> p h t", h=H), func=AF.Copy))(pt, cs), reads=[pt], writes=[retT])
                    pU = [pf.next(), pf.next()]
                    self.state_update(kd_l[lc], v_l[lc], pU)
                    o("pool", lambda e: e.tensor_copy(out=self.state_bf[:, :, :], in_=self.state[:, :, :]), reads=[self.state], writes=[self.state_bf])
                for lc in range(TCH):
                    ret_chunk(lc)
                self.gated_branch(OFF_GB, "w_br", retT, mgT, False, wload, pf, hT, sgt_r, tmp2_r, TT)
                for u in range(8):
                    slot, wv = wload("sp", d["w_out_g"][:, u * 256:(u + 1) * 256].rearrange("(k p) n -> p k n", p=128), KC, reads=[self.dbuf["w_out_g"]])
                    for lc, c in enumerate(gch):
                        pt = pf.next()
                        for kc in range(KC):
                            o("pe", (lambda kc, pt, wv, lc: lambda e: e.matmul(out=pt[:, 0:256], lhsT=mgT[:, kc, lc * 128:(lc + 1) * 128], rhs=wv[:, kc, :], start=(kc == 0), stop=(kc == KC - 1)))(kc, pt, wv, lc),
                              reads=[mgT, slot], writes=[pt])
                        xp = xp_r.next(); xnp = xnp_r.next()
                        self.load("sp", xp, xp[:, :], d["x_main"][c * 128:(c + 1) * 128, u * 256:(u + 1) * 256])
                        o("dve", (lambda pt, xp, xnp: lambda e: e.tensor_tensor(out=xnp[:, :], in0=pt[:, 0:256], in1=xp[:, :], op=ALU.add))(pt, xp, xnp), reads=[pt, xp], writes=[xnp])
                        o("act", (lambda xnp, lc, u: lambda e: e.activation(out=junk2[:, :], in_=xnp[:, :], func=AF.Square, accum_out=ssq2[lc][:, u:u + 1]))(xnp, lc, u), reads=[xnp], writes=[junk2, ssq2[lc]])
                        self.store("sp", xnp, d["x1_scr"][c * 128:(c + 1) * 128, u * 256:(u + 1) * 256], xnp[:, :], writes=[self.x1_bufs[c]])
                        if self.debug:
                            self.dma("sp", (lambda xnp, c, u: lambda e: e.dma_start(out=d["dbg_x1"][c * 128:(c + 1) * 128, u * 256:(u + 1) * 256], in_=xnp[:, :]))(xnp, c, u), reads=[xnp], key=("stdbg", id(xnp.b)))
                for lc, c in enumerate(gch):
                    self.route_chunk(lc, c, ssq2[lc], rsm, x1t_r.next(), xn2bf_r.next(), h2T_r, pf, wrt, brt, iota_e, tri, ones_b, msum, mbf, sidx_r)
                return a_prev_next

            a_prev = self.a_prev0
            for ti in range(NM // TCH):
                a_prev = tile_body(ti, a_prev)
            self.overflow_dispatch(sb, pf, ones_b, msum, iota_e, x1t_r, xn2bf_r, sidx_r)
            self.P.emit()

    def gated_branch(self, off, wname, rhsT, mgT, first, wload, pf, hT, sgt_r, tmp2_r, TT):
        d, o = self.dram, self.op
        gslot = gv = bslot = bv = None
        for nch in range(KC):
            if nch % 2 == 0:
                gslot, gv = wload("sp", d["w_in_b"][:, off + (nch // 2) * 256:off + (nch // 2 + 1) * 256].rearrange("(k p) n -> p k n", p=128), KC, reads=[self.dbuf["w_in_b"]])
            if nch % 4 == 0:
                bslot, bv = wload("sp", d[wname + "_b"][:, (nch // 4) * 512:(nch // 4 + 1) * 512].rearrange("(k p) n -> p k n", p=128), 8, reads=[self.dbuf[wname + "_b"]])
            pA = pf.next()
            for kc in range(KC):
                o("pe", (lambda kc, pA, gv, nch: lambda e: e.matmul(out=pA[:, 0:TT], lhsT=gv[:, kc, (nch % 2) * 128:(nch % 2 + 1) * 128], rhs=hT[:, kc, :], start=(kc == 0), stop=(kc == KC - 1)))(kc, pA, gv, nch),
                  reads=[gslot, hT], writes=[pA])
            pB = pf.next()
            for kc in range(8):
                o("pe", (lambda kc, pB, bv, nch: lambda e: e.matmul(out=pB[:, 0:TT], lhsT=bv[:, kc, (nch % 4) * 128:(nch % 4 + 1) * 128], rhs=rhsT[:, kc, :], start=(kc == 0), stop=(kc == 7)))(kc, pB, bv, nch),
                  reads=[bslot, rhsT], writes=[pB])
            sgt = sgt_r.next()
            o("act", (lambda pA, sgt: lambda e: e.activation(out=sgt[:, :], in_=pA[:, 0:TT], func=AF.Sigmoid))(pA, sgt), reads=[pA], writes=[sgt])
            if first:
                o("dve", (lambda pB, sgt, nch: lambda e: e.tensor_tensor(out=mgT[:, nch, :], in0=pB[:, 0:TT], in1=sgt[:, :], op=ALU.mult))(pB, sgt, nch), reads=[pB, sgt], writes=[mgT])
            else:
                tmp2 = tmp2_r.next()
                o("dve", (lambda pB, sgt, tmp2: lambda e: e.tensor_tensor(out=tmp2[:, :], in0=pB[:, 0:TT], in1=sgt[:, :], op=ALU.mult))(pB, sgt, tmp2), reads=[pB, sgt], writes=[tmp2])
                o("pool", (lambda tmp2, nch: lambda e: e.tensor_tensor(out=mgT[:, nch, :], in0=mgT[:, nch, :], in1=tmp2[:, :], op=ALU.add))(tmp2, nch), reads=[mgT, tmp2], writes=[mgT])

    def route_chunk(self, lc, c, ssq2_t, R, x1t, xn2bf, h2T_r, pf, wrt, brt, iota_e, tri, ones_b, msum, mbf, sidx_r):
        d, o, cfg = self.dram, self.op, self.cfg
        NE, C, NRT, EPG = cfg.ne, cfg.cap, cfg.nrt, cfg.epg
        ROWS = cfg.rows
        o("dve", lambda e: e.tensor_reduce(out=R["ssum"][:, :], in_=ssq2_t[:, :], axis=AX.X, op=ALU.add), reads=[ssq2_t], writes=[R["ssum"]])
        self.rstd_from_ssq(R["ssum"][:, :], R["ssum"], R["rt2"], R["rstd2"], 1.0 / D)
        o("dve", lambda e: e.tensor_copy(out=self.rstd2_all[:, c:c + 1], in_=R["rstd2"][:, :]), reads=[R["rstd2"]], writes=[self.rt_bufs[c]])
        self.load("sp", x1t, x1t[:, :], d["x1_scr"][c * 128:(c + 1) * 128, :], reads=[self.x1_bufs[c]])
        o("dve", lambda e: e.tensor_scalar(out=x1t[:, :], in0=x1t[:, :], scalar1=R["rstd2"][:, 0:1], scalar2=None, op0=ALU.mult), reads=[x1t, R["rstd2"]], writes=[x1t])
        o("act", lambda e: e.activation(out=xn2bf[:, :], in_=x1t[:, :], func=AF.Copy), reads=[x1t], writes=[xn2bf])
        pL = pf.next()
        for grp in range(4):
            pt = pf.next()
            for q4 in range(4):
                kc = grp * 4 + q4
                o("pe", (lambda pt, q4, kc: lambda e: e.transpose(out=pt[:, q4 * 128:(q4 + 1) * 128], in_=x1t[:, kc * 128:(kc + 1) * 128], identity=self.ident_f[:, :]))(pt, q4, kc),
                  reads=[x1t, self.ident_f], writes=[pt])
            h2 = h2T_r.next()
            for q4 in range(4):
                kc = grp * 4 + q4
                if q4 % 2 == 0:
                    o("act", (lambda pt, q4, kc, h2: lambda e: e.activation(out=h2[:, q4, :], in_=pt[:, q4 * 128:(q4 + 1) * 128], func=AF.Identity, scale=self.g2eff[:, kc:kc + 1], bias=self.shift2[:, kc:kc + 1]))(pt, q4, kc, h2),
                      reads=[pt, self.g2eff, self.shift2], writes=[h2])
                else:
                    o("dve", (lambda pt, q4, kc, h2: lambda e: e.tensor_scalar(out=h2[:, q4, :], in0=pt[:, q4 * 128:(q4 + 1) * 128], scalar1=self.g2eff[:, kc:kc + 1], scalar2=self.shift2[:, kc:kc + 1], op0=ALU.mult, op1=ALU.add))(pt, q4, kc, h2),
                      reads=[pt, self.g2eff, self.shift2], writes=[h2])
            for q4 in range(4):
                kc = grp * 4 + q4
                o("pe", (lambda q4, kc, h2: lambda e: e.matmul(out=pL[:, 0:NRT], lhsT=h2[:, q4, :], rhs=wrt[:, kc, :], start=(kc == 0), stop=(kc == KC - 1)))(q4, kc, h2),
                  reads=[h2, wrt], writes=[pL])
        def ts(out, in0, s1, s2, op0, op1=None, reads=(), writes=()):
            if op1 is None:
                o("dve", lambda e: e.tensor_scalar(out=out, in0=in0, scalar1=s1, scalar2=None, op0=op0), reads=reads, writes=writes)
            else:
                o("dve", lambda e: e.tensor_scalar(out=out, in0=in0, scalar1=s1, scalar2=s2, op0=op0, op1=op1), reads=reads, writes=writes)

        def tt(out, in0, in1, op, reads=(), writes=()):
            o("dve", lambda e: e.tensor_tensor(out=out, in0=in0, in1=in1, op=op), reads=reads, writes=writes)

        def red(out, in_, op, reads=(), writes=()):
            o("dve", lambda e: e.tensor_reduce(out=out, in_=in_, axis=AX.X, op=op), reads=reads, writes=writes)
        lg, gmax, ngmax, ohg, eg, sume, pgrp, pen = (R[n] for n in ("lg", "gmax", "ngmax", "ohg", "eg", "sume", "pgrp", "pen"))
        lem, m1, oh1, lem2, m2, oh2, dd, ed, den, rr = (R[n] for n in ("lem", "m1", "oh1", "lem2", "m2", "oh2", "dd", "ed", "den", "rr"))
        mm, prod, rank, eidx, ovf, destf, pfx = (R[n] for n in ("mm", "prod", "rank", "eidx", "ovf", "destf", "pfx"))
        gwb, dsb = self.gw_bufs[c], self.dest_bufs[c]
        tt(lg[:, :], pL[:, 0:4], brt[:, 0:4], ALU.add, [pL, brt], [lg])
        red(gmax[:, :], lg[:, :], ALU.max, [lg], [gmax])
        ts(ohg[:, :], lg[:, :], gmax[:, 0:1], None, ALU.is_equal, None, [lg, gmax], [ohg])
        ts(ngmax[:, :], gmax[:, :], -1.0, None, ALU.mult, None, [gmax], [ngmax])
        o("act", lambda e: e.activation(out=eg[:, :], in_=lg[:, :], func=AF.Exp, bias=ngmax[:, 0:1], accum_out=sume[:, 0:1]), reads=[lg, ngmax], writes=[eg, sume])
        o("dve", lambda e: e.reciprocal(out=pgrp[:, :], in_=sume[:, :]), reads=[sume], writes=[pgrp])
        ts(pen[:, :], ohg[:, :], 1.0, 1e30, ALU.subtract, ALU.mult, [ohg], [pen])
        tt(lem[:, :], pL[:, 4:4 + NE], brt[:, 4:4 + NE], ALU.add, [pL, brt], [lem])
        tt(lem[:, :].rearrange("p (g e) -> p g e", g=NG), lem[:, :].rearrange("p (g e) -> p g e", g=NG), pen[:, :].unsqueeze(2).to_broadcast([128, NG, EPG]), ALU.add, [lem, pen], [lem])
        red(m1[:, :], lem[:, :], ALU.max, [lem], [m1])
        ts(oh1[:, :], lem[:, :], m1[:, 0:1], None, ALU.is_equal, None, [lem, m1], [oh1])
        o("dve", lambda e: e.scalar_tensor_tensor(out=lem2[:, :], in0=oh1[:, :], scalar=-1e30, in1=lem[:, :], op0=ALU.mult, op1=ALU.add), reads=[oh1, lem], writes=[lem2])
        red(m2[:, :], lem2[:, :], ALU.max, [lem2], [m2])
        ts(oh2[:, :], lem2[:, :], m2[:, 0:1], None, ALU.is_equal, None, [lem2, m2], [oh2])
        tt(dd[:, :], m2[:, :], m1[:, :], ALU.subtract, [m1, m2], [dd])
        o("act", lambda e: e.activation(out=ed[:, :], in_=dd[:, :], func=AF.Exp), reads=[dd], writes=[ed])
        ts(den[:, :], ed[:, :], 1.0, None, ALU.add, None, [ed], [den])
        o("dve", lambda e: e.reciprocal(out=rr[:, :], in_=den[:, :]), reads=[den], writes=[rr])
        tt(self.gw_all[:, c, 0:1], rr[:, :], pgrp[:, :], ALU.mult, [rr, pgrp], [gwb])
        tt(self.gw_all[:, c, 1:2], pgrp[:, :], self.gw_all[:, c, 0:1], ALU.subtract, [pgrp, gwb], [gwb])
        tt(mm[:, :], oh1[:, :], oh2[:, :], ALU.add, [oh1, oh2], [mm])
        o("dve", lambda e: e.tensor_copy(out=mbf[:, :], in_=mm[:, :]), reads=[mm], writes=[mbf])
        pP = pf.next()
        o("pe", lambda e: e.matmul(out=pP[:, 0:NE], lhsT=tri[:, :], rhs=mbf[:, :], start=True, stop=False), reads=[tri, mbf], writes=[pP])
        o("pe", lambda e: e.matmul(out=pP[:, 0:NE], lhsT=ones_b[:, :], rhs=msum[:, :], start=False, stop=True), reads=[ones_b, msum], writes=[pP])
        o("dve", lambda e: e.tensor_copy(out=pfx[:, :], in_=pP[:, 0:NE]), reads=[pP], writes=[pfx])
        for k, oh in ((0, oh1), (1, oh2)):
            tt(prod[:, :], oh[:, :], pfx[:, :], ALU.mult, [oh, pfx], [prod])
            red(rank[:, k:k + 1], prod[:, :], ALU.add, [prod], [rank])
            tt(prod[:, :], oh[:, :], iota_e[:, :], ALU.mult, [oh, iota_e], [prod])
            red(eidx[:, k:k + 1], prod[:, :], ALU.add, [prod], [eidx])
        ts(ovf[:, :], rank[:, :], float(C), float(ROWS), ALU.is_ge, ALU.mult, [rank], [ovf])
        o("dve", lambda e: e.scalar_tensor_tensor(out=destf[:, :], in0=eidx[:, :], scalar=float(C), in1=rank[:, :], op0=ALU.mult, op1=ALU.add), reads=[eidx, rank], writes=[destf])
        tt(destf[:, :], destf[:, :], ovf[:, :], ALU.add, [destf, ovf], [destf])
        ts(destf[:, :], destf[:, :], float(ROWS + 7), None, ALU.min, None, [destf], [destf])
        sidx = sidx_r.next()
        o("dve", lambda e: e.tensor_copy(out=sidx[:, :], in_=destf[:, :]), reads=[destf], writes=[sidx])
        o("dve", lambda e: e.tensor_copy(out=self.destf_all[:, c, :], in_=destf[:, :]), reads=[destf], writes=[self.rt_bufs[c]])
        o("dve", lambda e: e.tensor_copy(out=self.rank_all[:, c, :], in_=rank[:, :]), reads=[rank], writes=[self.rt_bufs[c]])
        o("dve", lambda e: e.tensor_copy(out=self.eidx_all[:, c, :], in_=eidx[:, :]), reads=[eidx], writes=[self.rt_bufs[c]])
        tt(msum[:, :], msum[:, :], mm[:, :], ALU.add, [msum, mm], [msum])
        for k in range(2):
            self.dma("pool", (lambda k: lambda e: e.indirect_dma_start(out=d["xs_scr"][:, :], out_offset=bass.IndirectOffsetOnAxis(ap=sidx[:, k:k + 1], axis=0),
                                                                      in_=xn2bf[:, :], in_offset=None, bounds_check=self.bound_reg(e, ROWS - 1), oob_is_err=False))(k),
                     reads=[xn2bf, sidx], writes=[self.dbuf["xs_scr"]], key=("st", id(xn2bf.b)))
        if self.debug and c == cfg.nmain - 1:
            self.dma("sp", lambda e: e.dma_start(out=d["dbg_gw"][:, :, :], in_=self.gw_all[:, :, :]), reads=self.gw_bufs, key=("st", "dbggw"))
            self.dma("sp", lambda e: e.dma_start(out=d["dbg_state"][:, :, :], in_=self.state[:, :, :]), reads=[self.state], key=("st", "dbgstate"))

    def overflow_dispatch(self, sb, pf, ones_b, msum, iota_e, x1t_r, xn2bf_r, sidx_r):
        d, o, cfg = self.dram, self.op, self.cfg
        NE, C, NS, NM, ROWS = cfg.ne, cfg.cap, cfg.ns, cfg.nmain, cfg.rows
        OOB = float(ROWS + 7)
        HALF = C / 2.0 - 0.5
        cnt = sb("od_cnt", [128, NE]); t1 = sb("od_t1", [128, NE]); ni = sb("od_ni", [128, NE], I32)
        nov = sb("od_nov", [128, NE]); ca = sb("od_ca", [128, NE]); cb = sb("od_cb", [128, NE]); obase = sb("od_obase", [128, NE])
        esf = sb("od_esf", [128, NS]); tmpe = sb("od_tmpe", [128, NE])
        pC = pf.next()
        o("pe", lambda e: e.matmul(out=pC[:, 0:NE], lhsT=ones_b[:, :], rhs=msum[:, :], start=True, stop=True), reads=[ones_b, msum], writes=[pC])
        o("dve", lambda e: e.tensor_copy(out=cnt[:, :], in_=pC[:, 0:NE]), reads=[pC], writes=[cnt])
        o("dve", lambda e: e.tensor_scalar(out=t1[:, :], in0=cnt[:, :], scalar1=float(-C), scalar2=0.0, op0=ALU.add, op1=ALU.max), reads=[cnt], writes=[t1])
        o("dve", lambda e: e.tensor_scalar(out=t1[:, :], in0=t1[:, :], scalar1=HALF, scalar2=1.0 / C, op0=ALU.add, op1=ALU.mult), reads=[t1], writes=[t1])
        o("dve", lambda e: e.tensor_copy(out=ni[:, :], in_=t1[:, :]), reads=[t1], writes=[ni])
        o("dve", lambda e: e.tensor_copy(out=nov[:, :], in_=ni[:, :]), reads=[ni], writes=[nov])
        cur, nxt = nov, ca
        sh = 1
        while sh < NE:
            o("dve", (lambda cur, nxt, sh: lambda e: e.tensor_copy(out=nxt[:, 0:sh], in_=cur[:, 0:sh]))(cur, nxt, sh), reads=[cur], writes=[nxt])
            o("dve", (lambda cur, nxt, sh: lambda e: e.tensor_tensor(out=nxt[:, sh:NE], in0=cur[:, sh:NE], in1=cur[:, 0:NE - sh], op=ALU.add))(cur, nxt, sh), reads=[cur], writes=[nxt])
            cur, nxt = nxt, (cb if nxt is ca else ca)
            sh *= 2
        oincl = cur
        o("dve", lambda e: e.tensor_tensor(out=obase[:, :], in0=oincl[:, :], in1=nov[:, :], op=ALU.subtract), reads=[oincl, nov], writes=[obase])
        for s_ in range(NS):
            o("dve", (lambda s_: lambda e: e.tensor_scalar(out=tmpe[:, :], in0=oincl[:, :], scalar1=float(s_), scalar2=None, op0=ALU.is_le))(s_), reads=[oincl], writes=[tmpe])
            o("dve", (lambda s_: lambda e: e.tensor_reduce(out=esf[:, s_:s_ + 1], in_=tmpe[:, :], axis=AX.X, op=ALU.add))(s_), reads=[tmpe], writes=[esf])
        iop = sb("od_iop", [128, 1]); esc = sb("od_esc", [128, NS]); esc4 = sb("od_esc4", [128, NS * 4])
        self.load("sp", iop, iop[:, :], d["iota_p"][:, :])
        o("dve", lambda e: e.tensor_scalar(out=esc[:, :], in0=esf[:, :], scalar1=512.0, scalar2=iop[:, 0:1], op0=ALU.mult, op1=ALU.add), reads=[esf, iop], writes=[esc])
        for u in range(4):
            o("dve", (lambda u: lambda e: e.tensor_scalar(out=esc4[:, :].rearrange("p (s u) -> p s u", u=4)[:, :, u], in0=esc[:, :], scalar1=float(u * 128), scalar2=None, op0=ALU.add))(u), reads=[esc], writes=[esc4])
        o("dve", lambda e: e.tensor_copy(out=self.slot_idx[:, 0:4 * NS], in_=esc4[:, :]), reads=[esc4], writes=[self.slot_idx])
        sm = {n: sb("od_" + n, [128, w]) for n, w in (("ov", 1), ("q", 1), ("qf", 1), ("oh", NE), ("ob", 1), ("a", 1), ("b", 1), ("fl", 1), ("dl", 2), ("sx", 2), ("df", 2))}
        qi = sb("od_qi", [128, 1], I32)

        def chunk(c):
            x1t = x1t_r.next(); xn2bf = xn2bf_r.next(); sidx = sidx_r.next()
            self.load("sp", x1t, x1t[:, :], d["x1_scr"][c * 128:(c + 1) * 128, :], reads=[self.x1_bufs[c]])
            o("dve", lambda e: e.tensor_scalar(out=x1t[:, :], in0=x1t[:, :], scalar1=self.rstd2_all[:, c:c + 1], scalar2=None, op0=ALU.mult), reads=[x1t, self.rt_bufs[c]], writes=[x1t])
            o("act", lambda e: e.activation(out=xn2bf[:, :], in_=x1t[:, :], func=AF.Copy), reads=[x1t], writes=[xn2bf])
            ov, q, qf, oh, ob, a, b, fl, dl, sx, df = (sm[n] for n in ("ov", "q", "qf", "oh", "ob", "a", "b", "fl", "dl", "sx", "df"))
            def per_k(k):
                rk = self.rank_all[:, c, k:k + 1]; ek = self.eidx_all[:, c, k:k + 1]
                o("dve", lambda e: e.tensor_scalar(out=ov[:, :], in0=rk, scalar1=float(-C), scalar2=None, op0=ALU.add), reads=[self.rt_bufs[c]], writes=[ov])
                o("dve", lambda e: e.tensor_scalar(out=q[:, :], in0=ov[:, :], scalar1=-HALF, scalar2=1.0 / C, op0=ALU.add, op1=ALU.mult), reads=[ov], writes=[q])
                o("dve", lambda e: e.tensor_copy(out=qi[:, :], in_=q[:, :]), reads=[q], writes=[qi])
                o("dve", lambda e: e.tensor_copy(out=qf[:, :], in_=qi[:, :]), reads=[qi], writes=[qf])
                o("dve", lambda e: e.tensor_scalar(out=oh[:, :], in0=iota_e[:, :], scalar1=ek, scalar2=None, op0=ALU.is_equal), reads=[iota_e, self.rt_bufs[c]], writes=[oh])
                o("dve", lambda e: e.tensor_tensor(out=oh[:, :], in0=oh[:, :], in1=obase[:, :], op=ALU.mult), reads=[oh, obase], writes=[oh])
                o("dve", lambda e: e.tensor_reduce(out=ob[:, :], in_=oh[:, :], axis=AX.X, op=ALU.add), reads=[oh], writes=[ob])
                o("dve", lambda e: e.scalar_tensor_tensor(out=a[:, :], in0=ob[:, :], scalar=float(NE), in1=qf[:, :], op0=ALU.add, op1=ALU.add), reads=[ob, qf], writes=[a])
                o("dve", lambda e: e.scalar_tensor_tensor(out=b[:, :], in0=qf[:, :], scalar=float(-C), in1=ov[:, :], op0=ALU.mult, op1=ALU.add), reads=[qf, ov], writes=[b])
                o("dve", lambda e: e.scalar_tensor_tensor(out=a[:, :], in0=a[:, :], scalar=float(C), in1=b[:, :], op0=ALU.mult, op1=ALU.add), reads=[a, b], writes=[a])
                o("dve", lambda e: e.tensor_scalar(out=fl[:, :], in0=ov[:, :], scalar1=0.0, scalar2=None, op0=ALU.is_ge), reads=[ov], writes=[fl])
                o("dve", lambda e: e.scalar_tensor_tensor(out=dl[:, k:k + 1], in0=a[:, :], scalar=-OOB, in1=fl[:, :], op0=ALU.add, op1=ALU.mult), reads=[a, fl], writes=[dl])
            per_k(0)
            per_k(1)
            o("dve", lambda e: e.tensor_scalar(out=sx[:, :], in0=dl[:, :], scalar1=OOB, scalar2=None, op0=ALU.add), reads=[dl], writes=[sx])
            o("dve", lambda e: e.tensor_copy(out=sidx[:, :], in_=sx[:, :]), reads=[sx], writes=[sidx])
            o("dve", lambda e: e.tensor_tensor(out=df[:, :], in0=self.destf_all[:, c, :], in1=dl[:, :], op=ALU.add), reads=[self.rt_bufs[c], dl], writes=[df])
            o("dve", lambda e: e.tensor_copy(out=self.dest_all[:, c, :], in_=df[:, :]), reads=[df], writes=[self.dest_bufs[c]])
            for k in range(2):
                self.dma("pool", (lambda k: lambda e: e.indirect_dma_start(out=d["xs_scr"][:, :], out_offset=bass.IndirectOffsetOnAxis(ap=sidx[:, k:k + 1], axis=0),
                                                                          in_=xn2bf[:, :], in_offset=None, bounds_check=self.bound_reg(e, ROWS - 1), oob_is_err=False))(k),
                         reads=[xn2bf, sidx], writes=[self.dbuf["xs_scr"]], key=("st", id(xn2bf.b)))

        for c in range(NM):
            chunk(c)
        if self.debug:
            self.dma("sp", lambda e: e.dma_start(out=d["dbg_dest"][:, :, :], in_=self.dest_all[:, :, :]), reads=self.dest_bufs, key=("st", "dbgdest"))

    def phase3(self):
        nc, d, o, cfg = self.nc, self.dram, self.op, self.cfg
        NE, C, NB = cfg.ne, cfg.cap, cfg.nb
        with ExitStack() as pes:
            sb = lambda n, sh, dt=F32: T(pes.enter_context(nc.sbuf_tensor("p3_" + n, list(sh), dt)), n)
            ps = lambda n, sh, dt=F32: T(pes.enter_context(nc.psum_tensor("p3_" + n, list(sh), dt)), n)
            xst_r = Ring([sb("xst%d" % i, [128, D], BF16) for i in range(3)])
            xbT_r = Ring([sb("xbT%d" % i, [128, KC, cfg.wcap], BF16) for i in range(2 * cfg.nwin)])
            hidT_r = Ring([sb("hidT%d" % i, [128, FC, cfg.wcap], BF16) for i in range(2 * cfg.nwin)])
            w13_r = Ring([sb("w13_%d" % i, [128, KC, 256], BF16) for i in range(4)])
            w2_r = Ring([sb("w2_%d" % i, [128, FC, 512], BF16) for i in range(3)])
            st_r = Ring([sb("silu%d" % i, [128, cfg.wcap]) for i in range(2)])
            yt_r = Ring([sb("yt%d" % i, [128, 512]) for i in range(4)])
            ptrs = [ps("ptr%d" % i, [128, 1024], BF16) for i in range(2)]
            pf = Ring([ps("pf%d" % i, [128, 512]) for i in range(6)])
            NW, WC = cfg.nwin, cfg.wcap

            wstg_r = Ring([sb("wstg%d" % i, [128, 4096]) for i in range(3)])

            def wdma(dst, name, ex, u):
                dst2 = dst[:, :, :].rearrange("p k n -> p (k n)")
                if isinstance(ex, tuple):
                    j = ex[1] * 4 + u
                    stg = wstg_r.next()
                    self.dma("pool", lambda e: e.indirect_dma_start(out=stg[:, :], out_offset=None, in_=d[name][:, :],
                                                                   in_offset=bass.IndirectOffsetOnAxis(ap=self.slot_idx[:, j:j + 1], axis=0),
                                                                   bounds_check=self.bound_reg(e, NE * 512 - 1), oob_is_err=False),
                             reads=[self.slot_idx], writes=[stg], key=stg)
                    self._cast_i = getattr(self, "_cast_i", 0) + 1
                    if self._cast_i % 2 == 0:
                        return o("act", lambda e: e.activation(out=dst2, in_=stg[:, :], func=AF.Copy), reads=[stg], writes=[dst])
                    return o("dve", lambda e: e.tensor_copy(out=dst2, in_=stg[:, :]), reads=[stg], writes=[dst])
                return self.load("pool", dst, dst2, d[name][(ex * 4 + u) * 128:(ex * 4 + u + 1) * 128, :])

            def expert_body(ex, rbase=None):
                if rbase is not None:
                    ex = ("dyn", ex)
                else:
                    rbase = ex * C
                xbTs = [xbT_r.next() for _ in range(NW)]
                hidTs = [hidT_r.next() for _ in range(NW)]
                for w in range(NW):
                    for b in range(NB):
                        xst = xst_r.next()
                        r0 = rbase + w * WC + b * 128
                        self.load("sp", xst, xst[:, :], d["xs_scr"][r0:r0 + 128, :], reads=[self.dbuf["xs_scr"]])
                        for kc in range(KC):
                            pt = ptrs[kc // 8]
                            o("pe", (lambda kc, pt, xst: lambda e: e.transpose(out=pt[:, (kc % 8) * 128:(kc % 8 + 1) * 128], in_=xst[:, kc * 128:(kc + 1) * 128], identity=self.ident_b[:, :]))(kc, pt, xst),
                              reads=[xst, self.ident_b], writes=[pt])
                        self.evac_mod(ptrs, xbTs[w], b * 128, self.g2eff, self.shift2)
                def fetch13(fu):
                    w1u = w13_r.next(); w3u = w13_r.next()
                    wdma(w1u, "w1", ex, fu)
                    wdma(w3u, "w3", ex, fu)
                    return w1u, w3u

                def fetch2(nu):
                    w2u = w2_r.next()
                    wdma(w2u, "w2", ex, nu)
                    return w2u

                nxt13 = fetch13(0)
                nxt2 = None
                for fu in range(4):
                    w1u, w3u = nxt13
                    if fu + 1 < 4:
                        nxt13 = fetch13(fu + 1)
                    else:
                        nxt2 = fetch2(0)
                    for w in range(NW):
                        xbT = xbTs[w]; hidT = hidTs[w]
                        for fh in range(2):
                            fc = fu * 2 + fh
                            p1 = pf.next(); p3 = pf.next()
                            for wu, pp in ((w1u, p1), (w3u, p3)):
                                for kc in range(KC):
                                    o("pe", (lambda wu, pp, kc, fh, xbT: lambda e: e.matmul(out=pp[:, 0:WC], lhsT=wu[:, kc, fh * 128:(fh + 1) * 128], rhs=xbT[:, kc, :], start=(kc == 0), stop=(kc == KC - 1)))(wu, pp, kc, fh, xbT),
                                      reads=[wu, xbT], writes=[pp])
                            st = st_r.next()
                            o("act", (lambda p1, st: lambda e: e.activation(out=st[:, :], in_=p1[:, 0:WC], func=AF.Silu))(p1, st), reads=[p1], writes=[st])
                            o("dve", (lambda p3, st, fc, hidT: lambda e: e.tensor_tensor(out=hidT[:, fc, :], in0=p3[:, 0:WC], in1=st[:, :], op=ALU.mult))(p3, st, fc, hidT), reads=[p3, st], writes=[hidT])
                for nu in range(4):
                    w2u = nxt2
                    if nu + 1 < 4:
                        nxt2 = fetch2(nu + 1)
                    for w in range(NW):
                        hidT = hidTs[w]
                        for b in range(NB):
                            py = pf.next()
                            for fc in range(FC):
                                o("pe", (lambda py, fc, b, w2u, hidT: lambda e: e.matmul(out=py[:, :], lhsT=hidT[:, fc, b * 128:(b + 1) * 128], rhs=w2u[:, fc, :], start=(fc == 0), stop=(fc == FC - 1)))(py, fc, b, w2u, hidT),
                                  reads=[hidT, w2u], writes=[py])
                            yt = yt_r.next()
                            if (nu * NB + b) % 2 == 0:
                                o("act", (lambda py, yt: lambda e: e.activation(out=yt[:, :], in_=py[:, :], func=AF.Copy))(py, yt), reads=[py], writes=[yt])
                            else:
                                o("dve", (lambda py, yt: lambda e: e.tensor_copy(out=yt[:, :], in_=py[:, :]))(py, yt), reads=[py], writes=[yt])
                            r0 = rbase + w * WC + b * 128
                            self.store("sp", yt, d["ys_scr"][r0:r0 + 128, nu * 512:(nu + 1) * 512], yt[:, :], writes=[self.dbuf["ys_scr"]])

            for ex in range(NE):
                expert_body(ex)
            for s_ in range(cfg.ns):
                expert_body(s_, rbase=(NE + s_) * C)
            self.P.emit()

    def phase4(self):
        nc, d, o, cfg = self.nc, self.dram, self.op, self.cfg
        NM, NE, C = cfg.nmain, cfg.ne, cfg.cap
        with ExitStack() as pes:
            sb = lambda n, sh, dt=F32: T(pes.enter_context(nc.sbuf_tensor("p4_" + n, list(sh), dt)), n)
            gate2 = sb("gate2", [128, D]); fgain = sb("fgain", [128, D])
            self.load("sp", gate2, gate2[:, :], d["gate2_scr"][:, :], reads=[self.dbuf["gate2_scr"]], group="c4")
            self.load("sp", fgain, fgain[:, :], d["fgain_bc"][:, :], group="c4")
            x1_r = Ring([sb("x1_%d" % i, [128, D]) for i in range(2)])
            y0_r = Ring([sb("y0_%d" % i, [128, D]) for i in range(2)])
            y1_r = Ring([sb("y1_%d" % i, [128, D]) for i in range(2)])
            junk = sb("junk", [128, D], BF16)
            smalls = Ring([(sb("ssq%d" % i, [128, 1]), sb("rt%d" % i, [128, 1]), sb("rstd%d" % i, [128, 1])) for i in range(2)])
            for c in range(NM):
                x1 = x1_r.next(); y0 = y0_r.next(); y1 = y1_r.next(); ssq, rt, rstd = smalls.next()
                self.load("sp", x1, x1[:, :], d["x1_scr"][c * 128:(c + 1) * 128, :], reads=[self.x1_bufs[c]])
                for k, y in ((0, y0), (1, y1)):
                    o("pool", (lambda y: lambda e: e.memset(y[:, :], 0.0))(y), writes=[y])
                    self.dma("pool", (lambda k, y, c: lambda e: e.indirect_dma_start(out=y[:, :], out_offset=None, in_=d["ys_scr"][:, :],
                                                                                   in_offset=bass.IndirectOffsetOnAxis(ap=self.dest_all[:, c, k:k + 1], axis=0),
                                                                                   bounds_check=self.bound_reg(e, cfg.rows - 1), oob_is_err=False))(k, y, c),
                             reads=[self.dbuf["ys_scr"], self.dest_bufs[c]], writes=[y], key=y)
                o("dve", (lambda y0, c: lambda e: e.tensor_scalar(out=y0[:, :], in0=y0[:, :], scalar1=self.gw_all[:, c, 0:1], scalar2=None, op0=ALU.mult))(y0, c), reads=[y0, self.gw_bufs[c]], writes=[y0])
                o("dve", (lambda y0, y1, c: lambda e: e.scalar_tensor_tensor(out=y1[:, :], in0=y1[:, :], scalar=self.gw_all[:, c, 1:2], in1=y0[:, :], op0=ALU.mult, op1=ALU.add))(y0, y1, c),
                  reads=[y0, y1, self.gw_bufs[c]], writes=[y1])
                o("pool", (lambda y1: lambda e: e.tensor_tensor(out=y1[:, :], in0=y1[:, :], in1=gate2[:, :], op=ALU.mult))(y1), reads=[y1, gate2], writes=[y1])
                o("dve", (lambda y1, x1: lambda e: e.tensor_tensor(out=x1[:, :], in0=y1[:, :], in1=x1[:, :], op=ALU.add))(y1, x1), reads=[y1, x1], writes=[x1])
                o("act", (lambda x1, ssq: lambda e: e.activation(out=junk[:, :], in_=x1[:, :], func=AF.Square, accum_out=ssq[:, 0:1]))(x1, ssq), reads=[x1], writes=[junk, ssq])
                self.rstd_from_ssq(ssq[:, 0:1], ssq, rt, rstd, 1.0 / D)
                o("dve", (lambda x1, y0, rstd: lambda e: e.scalar_tensor_tensor(out=y0[:, :], in0=x1[:, :], scalar=rstd[:, 0:1], in1=fgain[:, :], op0=ALU.mult, op1=ALU.mult))(x1, y0, rstd),
                  reads=[x1, rstd, fgain], writes=[y0])
                self.store("sp", y0, d["out"][c * 128:(c + 1) * 128, :], y0[:, :])
            self.P.emit()


def make_consts(cfg, first_seg):
    gam = np.array([1.0 - 2.0 ** (-5.0 - h) for h in range(H)], np.float64)
    idx = np.arange(128, dtype=np.float64)
    c = {}
    c["ident_f"] = np.eye(128, dtype=np.float32)
    bands = np.zeros((128, 16, 128), np.float64)
    for g, w in enumerate(POOL_WINDOWS):
        cur = np.zeros((128, 128)); prv = np.zeros((128, 128)); cur0 = np.zeros((128, 128))
        for t in range(128):
            for j in range(w):
                tp = t - j
                if tp >= 0:
                    cur[tp, t] += 1.0 / w
                else:
                    prv[128 + tp, t] += 1.0 / w
            cnt = min(t + 1, w)
            for j in range(cnt):
                cur0[t - j, t] += 1.0 / cnt
            cur[t, t] -= 1.0
            cur0[t, t] -= 1.0
        bands[:, g * 2, :] = cur
        bands[:, g * 2 + 1, :] = prv
        if first_seg:
            bands[:, 8 + g * 2, :] = cur0
        else:
            bands[:, 8 + g * 2, :] = cur
            bands[:, 8 + g * 2 + 1, :] = prv
    c["bands"] = bands.astype(np.float32)
    c["causalT"] = (idx[None, :] >= idx[:, None]).astype(np.float32)
    qs = gam[:, None] ** (idx[None, :] + 1.0)
    ku = gam[:, None] ** (-(idx[None, :] + 1.0)) * (128.0 ** -0.5)
    kd = gam[:, None] ** (127.0 - idx[None, :]) * (128.0 ** -0.5)
    c["qscaleT"] = np.broadcast_to(qs[None], (128, H, 128)).astype(np.float32).copy()
    c["kuscaleT"] = np.broadcast_to(ku[None], (128, H, 128)).astype(np.float32).copy()
    c["kdscale"] = kd.T.astype(np.float32).copy()
    c["tri_u"] = (idx[:, None] < idx[None, :]).astype(np.float32)
    c["iota_p"] = idx.astype(np.float32).reshape(128, 1).copy()
    c["iota_e"] = np.broadcast_to(np.arange(cfg.ne, dtype=np.float32)[None], (128, cfg.ne)).copy()
    invf = (10000.0 ** (-np.arange(64, dtype=np.float32) / np.float32(64))).astype(np.float32)
    c["invf_bc"] = np.broadcast_to(invf[None], (128, 64)).copy()
    return c


def shared_inputs(inp, cfg):
    f = lambda a: np.ascontiguousarray(np.asarray(a), dtype=np.float32)
    col = lambda v: np.ascontiguousarray(f(v).reshape(-1, 128).T)
    bc = lambda v: np.ascontiguousarray(np.broadcast_to(f(v).reshape(1, -1), (128, f(v).size)))
    s = {}
    s["b_ada_bc"] = bc(inp["b_ada"][0])
    s["n1g_fm"] = col(inp["norm1_gain"][0])
    s["n2g_fm"] = col(inp["norm2_gain"][0])
    s["fgain_bc"] = bc(inp["final_gain"])
    s["pscale_fm"] = col(inp["pool_scale"][0])
    s["brt_bc"] = bc(np.concatenate([f(inp["b_group"][0]), f(inp["b_router"][0])]))
    s["w_ada"] = f(inp["w_ada"][0]); s["w_in"] = f(inp["w_in"][0]); s["w_pool"] = f(inp["w_pool"][0])
    s["w_bp"] = f(inp["w_branch_pool"][0]); s["w_br"] = f(inp["w_branch_ret"][0]); s["w_out"] = f(inp["w_out"][0])
    s["w_rt"] = np.ascontiguousarray(np.concatenate([f(inp["w_group"][0]), f(inp["w_router"][0])], axis=1))
    ne = np.asarray(inp["w1"]).shape[1]
    relay = lambda w, k, n: np.ascontiguousarray(f(w).reshape(ne, k, 128, 4, n).transpose(0, 3, 2, 1, 4)).reshape(ne * 512, k * n)
    s["w1"] = relay(inp["w1"][0], KC, 256); s["w3"] = relay(inp["w3"][0], KC, 256); s["w2"] = relay(inp["w2"][0], FC, 512)
    return s


def core_inputs(inp, shared, cfg, b, start):
    NM, NP_ = cfg.nmain, cfg.npre
    x = np.asarray(inp["x"]); pos = np.asarray(inp["positions"])
    m = dict(shared)
    m.update(make_consts(cfg, start == 0))
    m["x_main"] = np.ascontiguousarray(x[b, start:start + NM * 128], dtype=np.float32)
    m["pos_main"] = np.ascontiguousarray(pos[b, start:start + NM * 128].reshape(NM, 128).T.astype(np.int32))
    npre_tok = NP_ * 128
    xp = np.zeros((max(npre_tok, 128), D), np.float32)
    pp = np.zeros((max(npre_tok, 128),), np.int32)
    fl = np.zeros((max(NP_, 1),), np.float32)
    lo = start - npre_tok
    for p in range(NP_):
        t0 = lo + p * 128
        if t0 >= 0:
            xp[p * 128:(p + 1) * 128] = x[b, t0:t0 + 128]
            pp[p * 128:(p + 1) * 128] = pos[b, t0:t0 + 128]
            fl[p] = 1.0
    m["x_pre"] = xp[:max(npre_tok, 128)]
    m["pos_pre"] = np.ascontiguousarray(pp.reshape(-1, 128).T)
    m["flags_pre"] = np.ascontiguousarray(np.broadcast_to(fl[None], (128, fl.size)))
    m["c_col"] = np.ascontiguousarray(np.asarray(inp["c"], dtype=np.float32)[b].reshape(-1, 128).T)
    return m


_NC_CACHE = {}


def get_nc(cfg_key, debug=False):
    key = (cfg_key, debug)
    if key not in _NC_CACHE:
        cfg = Cfg(*cfg_key)
        _NC_CACHE[key] = K(cfg, debug=debug).build()
    return _NC_CACHE[key]


def kernel(**inputs):
    cfg_key = (32, 96, 2, 8, 3, 1)
    cfg = Cfg(*cfg_key)
    nc = get_nc(cfg_key)
    shared = shared_inputs(inputs, cfg)
    B, S = 2, 16384
    seg = cfg.nmain * 128
    in_maps = []
    for core in range(8):
        b, sg = core // 4, core % 4
        in_maps.append(core_inputs(inputs, shared, cfg, b, sg * seg))
    res = run_bass_kernel_spmd(nc, in_maps, core_ids=list(range(8)))
    out = np.empty((B, S, D), np.float32)
    for core in range(8):
        b, sg = core // 4, core % 4
        out[b, sg * seg:(sg + 1) * seg] = np.asarray(res.results[core]["out"])
    return out
```

```python
import math
from contextlib import ExitStack

import numpy as np
import concourse.bass as bass
import concourse.mybir as mybir
from concourse.bass_utils import run_bass_kernel_spmd

F32 = mybir.dt.float32
BF16 = mybir.dt.bfloat16
I32 = mybir.dt.int32
AF = mybir.ActivationFunctionType
ALU = mybir.AluOpType
AX = mybir.AxisListType

ENGS = ("pe", "act", "dve", "pool", "sp")


class Buf:
    __slots__ = ("name", "writers", "readers", "prev_readers")

    def __init__(self, name):
        self.name = name
        self.writers = []
        self.readers = []
        self.prev_readers = []


class Op:
    __slots__ = ("eng", "fn", "deps", "is_dma", "sem", "val", "sig", "sigidx", "group", "done", "pos")

    def __init__(self, eng, fn, is_dma):
        self.eng = eng
        self.fn = fn
        self.deps = {}
        self.is_dma = is_dma
        self.sem = None
        self.val = 0
        self.sig = False
        self.sigidx = 0
        self.group = None
        self.done = False
        self.pos = 0


class Prog:
    def __init__(self, nc, es):
        self.nc = nc
        self.es = es
        self.streams = {e: [] for e in ENGS}
        self.eng_sem = {e: es.enter_context(nc.semaphore("prog_" + e)) for e in ("pe", "act", "dve", "pool")}
        self.eng_cnt = {e: 0 for e in ("pe", "act", "dve", "pool")}
        self.done_sem = es.enter_context(nc.semaphore("phase_done"))
        self.done_cnt = 0
        self.dma_pool = {"sp": [], "pool": []}
        self.dma_keys = {}
        self.dma_used = {"sp": 0, "pool": 0}
        self.group_ops = {}

    def _track(self, op, reads, writes):
        for b in reads:
            for w in b.writers:
                op.deps[w] = True
            b.readers.append(op)
        for b in writes:
            for r in b.readers:
                if r is not op:
                    op.deps.setdefault(r, False)
            for r in b.prev_readers:
                if r is not op:
                    op.deps.setdefault(r, False)
            for w in b.writers:
                op.deps.setdefault(w, False)
            if b.readers:
                b.prev_readers = b.readers
                b.readers = []
                b.writers = [op]
            else:
                b.writers.append(op)

    def op(self, eng, fn, reads=(), writes=()):
        o = Op(eng, fn, False)
        self._track(o, reads, writes)
        o.pos = len(self.streams[eng])
        self.streams[eng].append(o)
        return o

    def _dma_sem(self, key, queue):
        k = (queue, key)
        if k not in self.dma_keys:
            idx = self.dma_used[queue]
            self.dma_used[queue] += 1
            pool = self.dma_pool[queue]
            if idx >= len(pool):
                s = self.es.enter_context(self.nc.semaphore("dma_%s%d" % (queue, idx)))
                pool.append([s, 0])
            self.dma_keys[k] = pool[idx]
        return self.dma_keys[k]

    def dma(self, queue, fn, reads=(), writes=(), key=None, group=None):
        o = Op(queue, fn, True)
        self._track(o, reads, writes)
        ent = self._dma_sem(("g", group) if group is not None else key, queue)
        ent[1] += 1
        o.sem = ent[0]
        o.val = 16 * ent[1]
        if group is not None:
            o.group = group
            self.group_ops.setdefault(group, []).append(o)
        self.streams[queue].append(o)
        return o

    def emit(self):
        nc = self.nc
        for ops in self.group_ops.values():
            final = max(o.val for o in ops)
            for o in ops:
                o.val = final
        def needed(o, d, is_raw):
            if d.done:
                return False
            if d.group is not None and d.group == o.group:
                return False
            if d.is_dma or o.is_dma:
                return True
            if d.eng != o.eng:
                return True
            return d.eng != "pe"
        for e in ENGS:
            for o in self.streams[e]:
                latest = {}
                for d, is_raw in o.deps.items():
                    if not d.is_dma and needed(o, d, is_raw):
                        if d.eng not in latest or latest[d.eng].pos < d.pos:
                            latest[d.eng] = d
                for d in latest.values():
                    d.sig = True
        last = {}
        for e in ("pe", "act", "dve", "pool"):
            for o in reversed(self.streams[e]):
                if not o.is_dma:
                    o.sig = True
                    last[e] = o
                    break
        for e in ("pe", "act", "dve", "pool"):
            c = self.eng_cnt[e]
            for o in self.streams[e]:
                if not o.is_dma and o.sig:
                    c += 1
                    o.sigidx = c
                    o.sem = self.eng_sem[e]
                    o.val = c
            self.eng_cnt[e] = c
        self.done_cnt += 1
        done_val = self.done_cnt
        final_dma = [(ent[0], 16 * ent[1]) for q in ("sp", "pool") for ent in self.dma_pool[q] if ent[1] > 0]
        final_eng = [(self.eng_sem[e], self.eng_cnt[e]) for e in ("pe", "act", "dve", "pool") if self.eng_cnt[e] > 0]
        streams = self.streams
        done_sem = self.done_sem

        def run(eng_name, eng):
            waited = {}
            for o in streams[eng_name]:
                req = {}
                latest = {}
                for d, is_raw in o.deps.items():
                    if not needed(o, d, is_raw):
                        continue
                    if not d.is_dma:
                        if d.eng not in latest or latest[d.eng].pos < d.pos:
                            latest[d.eng] = d
                        continue
                    k = id(d.sem)
                    if k not in req or req[k][1] < d.val:
                        req[k] = (d.sem, d.val)
                for d in latest.values():
                    k = id(d.sem)
                    if k not in req or req[k][1] < d.val:
                        req[k] = (d.sem, d.val)
                for k, (sem, val) in req.items():
                    if waited.get(k, -1) < val:
                        eng.wait_ge(sem, val)
                        waited[k] = val
                ins = o.fn(eng)
                if o.is_dma:
                    ins.then_inc(o.sem, 16)
                elif o.sig:
                    ins.then_inc(o.sem, 1)
            if eng_name == "sp":
                for s, v in final_eng + final_dma:
                    eng.wait_ge(s, v)
                eng.sem_inc(done_sem, 1)
            else:
                eng.wait_ge(done_sem, done_val)

        with nc.Block() as block:
            @block.tensor
            def _(eng):
                run("pe", eng)

            @block.scalar
            def _(eng):
                run("act", eng)

            @block.vector
            def _(eng):
                run("dve", eng)

            @block.gpsimd
            def _(eng):
                run("pool", eng)

            @block.sync
            def _(eng):
                run("sp", eng)
        for e in ENGS:
            for o in self.streams[e]:
                o.done = True
                o.fn = None
        self.streams = {e: [] for e in ENGS}
        self.dma_keys = {}
        self.dma_used = {"sp": 0, "pool": 0}
        self.group_ops = {}


D = 2048
KC = 16
H = 8
NG = 4
F = 1024
FC = 8
IN_W = 9216
OFF_A, OFF_Q, OFF_K, OFF_V, OFF_RG, OFF_GA, OFF_GB = 0, 1024, 2048, 3072, 4096, 5120, 7168
EPS = 1e-6
PI_LO = 3.141592
TWO_PI = 2.0 * math.pi
CW1 = 6.28125
CW2 = TWO_PI - CW1
POOL_WINDOWS = (2, 4, 8, 16)


class Cfg:
    def __init__(self, nmain=32, npre=96, tch=4, epg=8, cap_blocks=3, nwin=1):
        self.nmain, self.npre, self.tch, self.epg, self.nb, self.nwin = nmain, npre, tch, epg, cap_blocks, nwin
        self.ne = NG * epg
        self.wcap = cap_blocks * 128
        self.cap = cap_blocks * 128 * nwin
        assert nwin == 1
        self.ns = (2 * 128 * nmain) // self.cap
        self.rows = (self.ne + self.ns) * self.cap
        self.nrt = NG + self.ne
        assert nmain % tch == 0


class T:
    __slots__ = ("h", "b")

    def __init__(self, h, name):
        self.h = h
        self.b = Buf(name)

    def __getitem__(self, k):
        return self.h[k]


class Ring:
    def __init__(self, items):
        self.items = items
        self.i = 0

    def next(self):
        t = self.items[self.i % len(self.items)]
        self.i += 1
        return t


def _bufs(lst):
    return [x.b if isinstance(x, T) else x for x in lst]


class K:
    def __init__(self, cfg, debug=False, stop_after=9):
        self.cfg = cfg
        self.debug = debug
        self.stop_after = stop_after
        self.nc = bass.Bass("TRN2", target_bir_lowering=False)
        self.dram = {}
        self.dbuf = {}

    def din(self, name, shape, dt=F32):
        self.dram[name] = self.nc.dram_tensor(name, list(shape), dt, kind="ExternalInput").ap()
        return self.dram[name]

    def dout(self, name, shape, dt=F32):
        self.dram[name] = self.nc.dram_tensor(name, list(shape), dt, kind="ExternalOutput").ap()
        return self.dram[name]

    def dscr(self, name, shape, dt):
        self.dram[name] = self.nc.dram_tensor(name, list(shape), dt, kind="Internal").ap()
        return self.dram[name]

    def op(self, eng, fn, reads=(), writes=()):
        return self.P.op(eng, fn, _bufs(reads), _bufs(writes))

    def dma(self, q, fn, reads=(), writes=(), key=None, group=None):
        if isinstance(key, T):
            key = key.b
        return self.P.dma(q, fn, _bufs(reads), _bufs(writes), key=key, group=group)

    def load(self, q, dst, dst_ap, src_ap, reads=(), group=None):
        return self.dma(q, lambda e: e.dma_start(out=dst_ap, in_=src_ap), reads=reads, writes=[dst], key=dst, group=group)

    def store(self, q, src, dst_ap, src_ap, writes=()):
        return self.dma(q, lambda e: e.dma_start(out=dst_ap, in_=src_ap), reads=[src], writes=writes, key=("st", id(src.b)))

    def build(self):
        cfg, nc = self.cfg, self.nc
        NM, NP_, NE, C, NRT = cfg.nmain, cfg.npre, cfg.ne, cfg.cap, cfg.nrt
        d = self.dram
        self.din("x_main", [NM * 128, D])
        self.din("x_pre", [NP_ * 128, D])
        self.din("pos_main", [128, NM], I32)
        self.din("pos_pre", [128, NP_], I32)
        self.din("flags_pre", [128, NP_])
        self.din("c_col", [128, KC])
        self.din("b_ada_bc", [128, 6 * D])
        self.din("n1g_fm", [128, KC])
        self.din("n2g_fm", [128, KC])
        self.din("fgain_bc", [128, D])
        self.din("pscale_fm", [128, 8])
        self.din("brt_bc", [128, NRT])
        self.din("w_ada", [D, 6 * D])
        self.din("w_in", [D, IN_W])
        self.din("w_pool", [4, 256, 256])
        self.din("w_bp", [1024, D])
        self.din("w_br", [1024, D])
        self.din("w_out", [D, D])
        self.din("w_rt", [D, NRT])
        self.din("w1", [NE * 512, KC * 256])
        self.din("w3", [NE * 512, KC * 256])
        self.din("w2", [NE * 512, FC * 512])
        self.din("iota_p", [128, 1])
        self.din("ident_f", [128, 128])
        self.din("bands", [128, 16, 128])
        self.din("causalT", [128, 128])
        self.din("qscaleT", [128, H, 128])
        self.din("kuscaleT", [128, H, 128])
        self.din("kdscale", [128, H])
        self.din("tri_u", [128, 128])
        self.din("iota_e", [128, NE])
        self.din("invf_bc", [128, 64])
        self.dout("out", [NM * 128, D])
        self.dscr("w_out_g", [8, 128, 4096], BF16)
        self.dscr("w_in_b", [IN_W // 256, 128, 4096], BF16)
        self.dscr("w_bp_b", [4, 128, 4096], BF16)
        self.dscr("w_br_b", [4, 128, 4096], BF16)
        self.dscr("gate2_scr", [128, D], F32)
        self.dscr("x1_scr", [NM * 128, D], F32)
        self.dscr("xs_scr", [cfg.rows, D], BF16)
        self.dscr("ys_scr", [cfg.rows, D], F32)
        if self.debug:
            self.dout("dbg_x1", [NM * 128, D])
            self.dout("dbg_dest", [128, NM, 2], I32)
            self.dout("dbg_gw", [128, NM, 2])
            self.dout("dbg_mod", [128, 4, KC])
            self.dout("dbg_state", [128, H, 128])
        for n in ("w_out_g", "gate2_scr", "xs_scr", "ys_scr", "w_in_b", "w_bp_b", "w_br_b"):
            self.dbuf[n] = Buf(n)
        self.x1_bufs = [Buf("x1scr%d" % c) for c in range(NM)]
        self.dest_bufs = [Buf("dest%d" % c) for c in range(NM)]
        self.gw_bufs = [Buf("gw%d" % c) for c in range(NM)]
        self.rt_bufs = [Buf("rt%d" % c) for c in range(NM)]
        gam = [1.0 - 2.0 ** (-5.0 - h) for h in range(H)]
        self.cd = [float(np.float32(g ** 128)) for g in gam]

        with ExitStack() as es:
            self.es = es
            self.P = Prog(nc, es)
            P = self.P
            sbp = lambda n, sh, dt=F32: T(es.enter_context(nc.sbuf_tensor(n, list(sh), dt)), n)
            self.g1eff = sbp("g1eff", [128, KC]); self.shift1 = sbp("shift1", [128, KC])
            self.g2eff = sbp("g2eff", [128, KC]); self.shift2 = sbp("shift2", [128, KC])
            self.ident_f = sbp("ident_f_sb", [128, 128]); self.ident_b = sbp("ident_b_sb", [128, 128], BF16)
            self.epsb = sbp("epsb", [128, 1])
            self.state = sbp("state", [128, H, 128]); self.state_bf = sbp("state_bf", [128, H, 128], BF16)
            self.a_prev0 = sbp("a_prev0", [128, 1024], BF16)
            self.dest_all = sbp("dest_all", [128, NM, 2], I32); self.gw_all = sbp("gw_all", [128, NM, 2])
            self.destf_all = sbp("destf_all", [128, NM, 2]); self.rank_all = sbp("rank_all", [128, NM, 2]); self.eidx_all = sbp("eidx_all", [128, NM, 2])
            self.rstd2_all = sbp("rstd2_all", [128, NM]); self.slot_idx = sbp("slot_idx", [128, 4 * max(cfg.ns, 1)], I32)
            self.load("sp", self.ident_f, self.ident_f[:, :], d["ident_f"][:, :], group="c0")
            self.load("pool", self.ident_b, self.ident_b[:, :], d["ident_f"][:, :], group="c0p")
            self.op("dve", lambda e: e.memset(self.epsb[:, :], EPS), writes=[self.epsb])
            self.op("dve", lambda e: e.memset(self.state[:, :, :], 0.0), writes=[self.state])
            for i, ph in enumerate((self.phase0, self.phase1, self.phase2, self.phase3, self.phase4)):
                if i <= self.stop_after:
                    ph()
        return nc

    def bound_reg(self, e, val):
        key = (self.P.done_cnt, val)
        if getattr(self, "_breg_key", None) != key:
            r = e.alloc_register("idma_bound%d" % self.P.done_cnt)
            e.reg_mov(r, val)
            self._breg_key = key
            self._breg = r
        return self._breg

    def rstd_from_ssq(self, ssq_ap, ssq_t, rt, rstd, scale):
        self.op("act", lambda e: e.activation(out=rt[:, :], in_=ssq_ap, func=AF.Sqrt, scale=scale, bias=self.epsb[:, 0:1]),
                reads=[ssq_t, self.epsb], writes=[rt])
        self.op("dve", lambda e: e.reciprocal(out=rstd[:, :], in_=rt[:, :]), reads=[rt], writes=[rstd])

    def norm_transpose(self, x_ap, xt, junk, small, xnb, ptrs, hT, col0, q="sp"):
        ssq, rt, rstd = small
        self.load(q, xt, xt[:, :], x_ap)
        self.op("act", lambda e: e.activation(out=junk[:, :], in_=xt[:, :], func=AF.Square, accum_out=ssq[:, 0:1]),
                reads=[xt], writes=[junk, ssq])
        self.rstd_from_ssq(ssq[:, 0:1], ssq, rt, rstd, 1.0 / D)
        self.op("dve", lambda e: e.tensor_scalar(out=xnb[:, :], in0=xt[:, :], scalar1=rstd[:, 0:1], scalar2=None, op0=ALU.mult),
                reads=[xt, rstd], writes=[xnb])
        for kc in range(KC):
            pt = ptrs[kc // 8]
            self.op("pe", (lambda kc, pt: lambda e: e.transpose(out=pt[:, (kc % 8) * 128:(kc % 8 + 1) * 128], in_=xnb[:, kc * 128:(kc + 1) * 128], identity=self.ident_b[:, :]))(kc, pt),
                    reads=[xnb, self.ident_b], writes=[pt])
        self.evac_mod(ptrs, hT, col0, self.g1eff, self.shift1)

    def evac_mod(self, ptrs, dst, col0, geff, shift):
        for kc in range(KC):
            pt = ptrs[kc // 8]
            src = pt[:, (kc % 8) * 128:(kc % 8 + 1) * 128]
            out = dst[:, kc, col0:col0 + 128]
            if kc % 2 == 0:
                self.op("act", (lambda out, src, kc: lambda e: e.activation(out=out, in_=src, func=AF.Identity, scale=geff[:, kc:kc + 1], bias=shift[:, kc:kc + 1]))(out, src, kc),
                        reads=[pt, geff, shift], writes=[dst])
            else:
                self.op("dve", (lambda out, src, kc: lambda e: e.tensor_scalar(out=out, in0=src, scalar1=geff[:, kc:kc + 1], scalar2=shift[:, kc:kc + 1], op0=ALU.mult, op1=ALU.add))(out, src, kc),
                        reads=[pt, geff, shift], writes=[dst])

    def trig(self, pos_f, col, tw, cos_t, sin_t):
        invf = self.invf
        ang, y, ki, kf, ra, r1, rs, c1, tt, c2 = (tw[n] for n in ("ang", "y", "ki", "kf", "ra", "r1", "rs", "c1", "tt", "c2"))
        o = self.op
        o("dve", lambda e: e.tensor_scalar(out=ang[:, :], in0=invf[:, :], scalar1=pos_f[:, col:col + 1], scalar2=None, op0=ALU.mult), reads=[invf, pos_f], writes=[ang])
        o("dve", lambda e: e.tensor_scalar(out=y[:, :], in0=ang[:, :], scalar1=1.0 / TWO_PI, scalar2=None, op0=ALU.mult), reads=[ang], writes=[y])
        o("dve", lambda e: e.tensor_copy(out=ki[:, :], in_=y[:, :]), reads=[y], writes=[ki])
        o("dve", lambda e: e.tensor_copy(out=kf[:, :], in_=ki[:, :]), reads=[ki], writes=[kf])
        o("dve", lambda e: e.scalar_tensor_tensor(out=ra[:, :], in0=kf[:, :], scalar=-CW1, in1=ang[:, :], op0=ALU.mult, op1=ALU.add), reads=[kf, ang], writes=[ra])
        o("dve", lambda e: e.scalar_tensor_tensor(out=r1[:, :], in0=kf[:, :], scalar=-CW2, in1=ra[:, :], op0=ALU.mult, op1=ALU.add), reads=[kf, ra], writes=[r1])
        o("dve", lambda e: e.tensor_scalar(out=rs[:, :], in0=r1[:, :], scalar1=PI_LO, scalar2=-PI_LO, op0=ALU.min, op1=ALU.max), reads=[r1], writes=[rs])
        o("act", lambda e: e.activation(out=sin_t[:, :], in_=rs[:, :], func=AF.Sin), reads=[rs], writes=[sin_t])
        o("dve", lambda e: e.tensor_scalar(out=c1[:, :], in0=r1[:, :], scalar1=math.pi / 2, scalar2=None, op0=ALU.add), reads=[r1], writes=[c1])
        o("dve", lambda e: e.tensor_scalar(out=tt[:, :], in0=c1[:, :], scalar1=math.pi, scalar2=-TWO_PI, op0=ALU.is_gt, op1=ALU.mult), reads=[c1], writes=[tt])
        o("dve", lambda e: e.tensor_tensor(out=c2[:, :], in0=c1[:, :], in1=tt[:, :], op=ALU.add), reads=[c1, tt], writes=[c2])
        o("dve", lambda e: e.tensor_scalar(out=c2[:, :], in0=c2[:, :], scalar1=PI_LO, scalar2=-PI_LO, op0=ALU.min, op1=ALU.max), reads=[c2], writes=[c2])
        o("act", lambda e: e.activation(out=cos_t[:, :], in_=c2[:, :], func=AF.Sin), reads=[c2], writes=[cos_t])

    def rotary(self, pt, nh, cos_t, sin_t, tring, dst, dcol0):
        pv = pt[:, 0:nh * 128].rearrange("p (h t f) -> p h t f", h=nh, t=2)
        dv = dst[:, dcol0:dcol0 + nh * 128].rearrange("p (h t f) -> p h t f", h=nh, t=2)
        cb = cos_t[:, :].unsqueeze(1).to_broadcast([128, nh, 64])
        sb_ = sin_t[:, :].unsqueeze(1).to_broadcast([128, nh, 64])
        o = self.op
        for half in (0, 1):
            tA = tring.next()
            tB = tring.next()
            a3 = tA[:, 0:nh * 64].rearrange("p (h f) -> p h f", h=nh)
            b3 = tB[:, 0:nh * 64].rearrange("p (h f) -> p h f", h=nh)
            o("dve", (lambda half, a3: lambda e: e.tensor_tensor(out=a3, in0=pv[:, :, half, :], in1=cb, op=ALU.mult))(half, a3), reads=[pt, cos_t], writes=[tA])
            o("dve", (lambda half, b3: lambda e: e.tensor_tensor(out=b3, in0=pv[:, :, 1 - half, :], in1=sb_, op=ALU.mult))(half, b3), reads=[pt, sin_t], writes=[tB])
            opc = ALU.subtract if half == 0 else ALU.add
            o("pool", (lambda half, opc, a3, b3: lambda e: e.tensor_tensor(out=dv[:, :, half, :], in0=a3, in1=b3, op=opc))(half, opc, a3, b3), reads=[tA, tB], writes=[dst])

    def phase0(self):
        nc, d, o = self.nc, self.dram, self.op
        with ExitStack() as pes:
            sb = lambda n, sh, dt=F32: T(pes.enter_context(nc.sbuf_tensor("p0_" + n, list(sh), dt)), n)
            ps = lambda n, sh, dt=F32: T(pes.enter_context(nc.psum_tensor("p0_" + n, list(sh), dt)), n)
            c_col = sb("c_col", [128, KC]); c_act = sb("c_act", [128, KC])
            ones_f = sb("ones_f", [128, 128]); cbc = sb("cbc", [128, KC, 128])
            mod_bc = sb("mod_bc", [128, 6 * D])
            n1g = sb("n1g", [128, KC]); n2g = sb("n2g", [128, KC])
            self.load("sp", c_col, c_col[:, :], d["c_col"][:, :])
            self.load("sp", n1g, n1g[:, :], d["n1g_fm"][:, :])
            self.load("sp", n2g, n2g[:, :], d["n2g_fm"][:, :])
            o("act", lambda e: e.activation(out=c_act[:, :], in_=c_col[:, :], func=AF.Silu), reads=[c_col], writes=[c_act])
            o("pool", lambda e: e.memset(ones_f[:, :], 1.0), writes=[ones_f])
            for kc in range(KC):
                o("dve", (lambda kc: lambda e: e.tensor_scalar(out=cbc[:, kc, :], in0=ones_f[:, :], scalar1=c_act[:, kc:kc + 1], scalar2=None, op0=ALU.mult))(kc),
                  reads=[ones_f, c_act], writes=[cbc])
            wring = Ring([sb("wada%d" % i, [128, KC, 256]) for i in range(2)])
            bring = Ring([sb("bada%d" % i, [128, 256]) for i in range(2)])
            pring = Ring([ps("pada%d" % i, [128, 512]) for i in range(2)])
            for j in range(6 * D // 256):
                w = wring.next(); b = bring.next(); pt = pring.next()
                self.load("sp", w, w[:, :, :], d["w_ada"][:, j * 256:(j + 1) * 256].rearrange("(k p) n -> p k n", p=128))
                self.load("sp", b, b[:, :], d["b_ada_bc"][:, j * 256:(j + 1) * 256])
                for kc in range(KC):
                    o("pe", (lambda kc, w, pt: lambda e: e.matmul(out=pt[:, 0:256], lhsT=cbc[:, kc, :], rhs=w[:, kc, :], start=(kc == 0), stop=(kc == KC - 1)))(kc, w, pt),
                      reads=[cbc, w], writes=[pt])
                o("dve", (lambda j, pt, b: lambda e: e.tensor_tensor(out=mod_bc[:, j * 256:(j + 1) * 256], in0=pt[:, 0:256], in1=b[:, :], op=ALU.add))(j, pt, b),
                  reads=[pt, b], writes=[mod_bc])
            tmp = sb("diag_tmp", [128, KC, 128])
            fm = [sb("fm%d" % i, [128, KC]) for i in range(6)]
            identb3 = self.ident_f[:, :].unsqueeze(1).to_broadcast([128, KC, 128])
            for i in (0, 1, 3, 4):
                o("dve", (lambda i: lambda e: e.tensor_tensor(out=tmp[:, :, :], in0=mod_bc[:, i * D:(i + 1) * D].rearrange("p (k n) -> p k n", k=KC), in1=identb3, op=ALU.mult))(i),
                  reads=[mod_bc, self.ident_f], writes=[tmp])
                o("dve", (lambda i: lambda e: e.tensor_reduce(out=fm[i][:, :], in_=tmp[:, :, :], axis=AX.X, op=ALU.add))(i), reads=[tmp], writes=[fm[i]])
            o("dve", lambda e: e.tensor_copy(out=self.shift1[:, :], in_=fm[0][:, :]), reads=[fm[0]], writes=[self.shift1])
            o("dve", lambda e: e.scalar_tensor_tensor(out=self.g1eff[:, :], in0=fm[1][:, :], scalar=1.0, in1=n1g[:, :], op0=ALU.add, op1=ALU.mult), reads=[fm[1], n1g], writes=[self.g1eff])
            o("dve", lambda e: e.tensor_copy(out=self.shift2[:, :], in_=fm[3][:, :]), reads=[fm[3]], writes=[self.shift2])
            o("dve", lambda e: e.scalar_tensor_tensor(out=self.g2eff[:, :], in0=fm[4][:, :], scalar=1.0, in1=n2g[:, :], op0=ALU.add, op1=ALU.mult), reads=[fm[4], n2g], writes=[self.g2eff])
            if self.debug:
                for i, t in enumerate((self.g1eff, self.shift1, self.g2eff, self.shift2)):
                    self.store("sp", t, d["dbg_mod"][:, i, :], t[:, :])
            self.dma("sp", lambda e: e.dma_start(out=d["gate2_scr"][:, :], in_=mod_bc[:, 5 * D:6 * D]), reads=[mod_bc], writes=[self.dbuf["gate2_scr"]], key=("st", "g2"))
            woring = Ring([sb("wo%d" % i, [128, D]) for i in range(2)])
            wgring = Ring([sb("wg%d" % i, [128, D], BF16) for i in range(2)])
            for kc in range(KC):
                wo = woring.next(); wg = wgring.next()
                self.load("sp", wo, wo[:, :], d["w_out"][kc * 128:(kc + 1) * 128, :])
                o("dve", (lambda wo, wg: lambda e: e.tensor_tensor(out=wg[:, :], in0=wo[:, :], in1=mod_bc[:, 2 * D:3 * D], op=ALU.mult))(wo, wg), reads=[wo, mod_bc], writes=[wg])
                self.store("sp", wg, d["w_out_g"][:, :, kc * 256:(kc + 1) * 256].rearrange("u p n -> p u n"), wg[:, :].rearrange("p (u n) -> p u n", u=8), writes=[self.dbuf["w_out_g"]])
            self.P.emit()

    def load_consts_ret(self, sb, need_q):
        d = self.dram
        self.invf = sb("invf", [128, 64])
        self.load("sp", self.invf, self.invf[:, :], d["invf_bc"][:, :], group="c1")
        self.kdscale = sb("kdscale", [128, H])
        self.load("sp", self.kdscale, self.kdscale[:, :], d["kdscale"][:, :], group="c1")

    def make_trig_work(self, sb, tag):
        tw = {}
        for n in ("ang", "y", "kf", "ra", "r1", "rs", "c1", "tt", "c2"):
            tw[n] = sb(tag + n, [128, 64])
        tw["ki"] = sb(tag + "ki", [128, 64], I32)
        return tw

    def phase1(self):
        nc, d, o, cfg = self.nc, self.dram, self.op, self.cfg
        NP_ = cfg.npre
        if NP_ == 0:
            return
        with ExitStack() as pes:
            sb = lambda n, sh, dt=F32: T(pes.enter_context(nc.sbuf_tensor("p1_" + n, list(sh), dt)), n)
            ps = lambda n, sh, dt=F32: T(pes.enter_context(nc.psum_tensor("p1_" + n, list(sh), dt)), n)
            self.load_consts_ret(sb, False)
            wkv = sb("wkv", [128, KC, 2048], BF16)
            wa = sb("wa", [128, KC, 1024], BF16)
            for j in range(4):
                self.load("pool", wkv, wkv[:, :, j * 512:(j + 1) * 512], d["w_in"][:, OFF_K + j * 512:OFF_K + (j + 1) * 512].rearrange("(k p) n -> p k n", p=128), group="c1p")
            for j in range(2):
                self.load("pool", wa, wa[:, :, j * 512:(j + 1) * 512], d["w_in"][:, OFF_A + j * 512:OFF_A + (j + 1) * 512].rearrange("(k p) n -> p k n", p=128), group="c1p")
            pos_i = sb("pos_i", [128, NP_], I32); pos_f = sb("pos_f", [128, NP_]); flags = sb("flags", [128, NP_])
            self.load("sp", pos_i, pos_i[:, :], d["pos_pre"][:, :], group="c1")
            self.load("sp", flags, flags[:, :], d["flags_pre"][:, :], group="c1")
            o("dve", lambda e: e.tensor_copy(out=pos_f[:, :], in_=pos_i[:, :]), reads=[pos_i], writes=[pos_f])
            xring = Ring([sb("x%d" % i, [128, D]) for i in range(2)])
            smalls = Ring([(sb("ssq%d" % i, [128, 1]), sb("rt%d" % i, [128, 1]), sb("rstd%d" % i, [128, 1])) for i in range(2)])
            xnbs = Ring([sb("xnb%d" % i, [128, D], BF16) for i in range(2)])
            hTs = Ring([sb("hT%d" % i, [128, KC, 128], BF16) for i in range(2)])
            ptrs = [ps("ptr%d" % i, [128, 1024], BF16) for i in range(2)]
            pfr = Ring([ps("pf%d" % i, [128, 512]) for i in range(6)])
            tws = Ring([self.make_trig_work(sb, "tw%d" % i) for i in range(2)])
            coss = Ring([sb("cos%d" % i, [128, 64]) for i in range(2)])
            sins = Ring([sb("sin%d" % i, [128, 64]) for i in range(2)])
            tring = Ring([sb("rt_tmp%d" % i, [128, 256]) for i in range(8)])
            krots = Ring([sb("krot%d" % i, [128, 1024], BF16) for i in range(2)])
            kds = Ring([sb("kd%d" % i, [128, 1024], BF16) for i in range(2)])
            vbs = Ring([sb("vb%d" % i, [128, 1024], BF16) for i in range(2)])
            kdb = self.kdscale[:, :].unsqueeze(2).to_broadcast([128, H, 128])
            cvr = Ring([sb("cv%d" % i, [128, KC, 256], BF16) for i in range(2)])
            zt = sb("zeros", [128, D], BF16)
            o("pool", lambda e: e.memset(zt[:, :], 0.0), writes=[zt])
            jobs = []

            def conv_w_in(j):
                cv = cvr.next()
                self.load("pool", cv, cv[:, :, :], d["w_in"][:, j * 256:(j + 1) * 256].rearrange("(k p) n -> p k n", p=128))
                self.store("sp", cv, d["w_in_b"][j], cv[:, :, :].rearrange("p k n -> p (k n)"), writes=[self.dbuf["w_in_b"]])

            def conv_w_b(nm, j):
                cv = cvr.next()
                cvv = cv[:, :, :].rearrange("p k n -> p (k n)").rearrange("p (k n) -> p k n", k=8)
                self.load("pool", cv, cvv, d[nm][:, j * 512:(j + 1) * 512].rearrange("(k p) n -> p k n", p=128))
                self.store("sp", cv, d[nm + "_b"][j], cv[:, :, :].rearrange("p k n -> p (k n)"), writes=[self.dbuf[nm + "_b"]])

            def zero_rows(r0, nr):
                for q4 in range(nr // 128):
                    self.dma("sp", (lambda r: lambda e: e.dma_start(out=d["xs_scr"][r:r + 128, :], in_=zt[:, :]))(r0 + q4 * 128), reads=[zt], writes=[self.dbuf["xs_scr"]], group="zero")

            for j in range(IN_W // 256):
                jobs.append((conv_w_in, (j,)))
            for nm in ("w_bp", "w_br"):
                for j in range(4):
                    jobs.append((conv_w_b, (nm, j)))
            for r0 in range(0, cfg.rows, 512):
                jobs.append((zero_rows, (r0, min(512, cfg.rows - r0))))
            per_chunk = -(-len(jobs) // NP_)

            def pre_chunk(p):
                for _ in range(per_chunk):
                    if jobs:
                        fn, args = jobs.pop(0)
                        fn(*args)
                xt = xring.next(); small = smalls.next(); xnb = xnbs.next(); hT = hTs.next()
                self.norm_transpose(d["x_pre"][p * 128:(p + 1) * 128, :], xt, xnb, small, xnb, ptrs, hT, 0)
                cos_t = coss.next(); sin_t = sins.next()
                self.trig(pos_f, p, tws.next(), cos_t, sin_t)
                krot = krots.next(); kd = kds.next(); vb = vbs.next()
                for j in range(2):
                    pk = pfr.next()
                    for kc in range(KC):
                        o("pe", (lambda j, kc, pk: lambda e: e.matmul(out=pk[:, :], lhsT=hT[:, kc, :], rhs=wkv[:, kc, j * 512:(j + 1) * 512], start=(kc == 0), stop=(kc == KC - 1)))(j, kc, pk),
                          reads=[hT, wkv], writes=[pk])
                    self.rotary(pk, 4, cos_t, sin_t, tring, krot, j * 512)
                for j in range(2):
                    pv = pfr.next()
                    for kc in range(KC):
                        o("pe", (lambda j, kc, pv: lambda e: e.matmul(out=pv[:, :], lhsT=hT[:, kc, :], rhs=wkv[:, kc, 1024 + j * 512:1024 + (j + 1) * 512], start=(kc == 0), stop=(kc == KC - 1)))(j, kc, pv),
                          reads=[hT, wkv], writes=[pv])
                    o("act", (lambda j, pv: lambda e: e.activation(out=vb[:, j * 512:(j + 1) * 512], in_=pv[:, :], func=AF.Copy, scale=flags[:, p:p + 1]))(j, pv),
                      reads=[pv, flags], writes=[vb])
                o("pool", lambda e: e.tensor_tensor(out=kd[:, :].rearrange("p (h f) -> p h f", h=H), in0=krot[:, :].rearrange("p (h f) -> p h f", h=H), in1=kdb, op=ALU.mult),
                  reads=[krot, self.kdscale], writes=[kd])
                self.state_update(kd, vb, [pfr.next(), pfr.next()])
                if p == NP_ - 1:
                    for j in range(2):
                        pa = pfr.next()
                        for kc in range(KC):
                            o("pe", (lambda j, kc, pa: lambda e: e.matmul(out=pa[:, :], lhsT=hT[:, kc, :], rhs=wa[:, kc, j * 512:(j + 1) * 512], start=(kc == 0), stop=(kc == KC - 1)))(j, kc, pa),
                              reads=[hT, wa], writes=[pa])
                        o("act", (lambda j, pa: lambda e: e.activation(out=self.a_prev0[:, j * 512:(j + 1) * 512], in_=pa[:, :], func=AF.Copy))(j, pa),
                          reads=[pa], writes=[self.a_prev0])

            for p in range(NP_):
                pre_chunk(p)
            if self.debug:
                self.dma("sp", lambda e: e.dma_start(out=d["dbg_state"][:, :, :], in_=self.state[:, :, :]), reads=[self.state], key=("st", "dbgstate1"))
            self.P.emit()

    def state_update(self, kd, vb, pst):
        o = self.op
        for h in range(H):
            pt = pst[h // 4]
            o("pe", (lambda h, pt: lambda e: e.matmul(out=pt[:, (h % 4) * 128:(h % 4 + 1) * 128], lhsT=kd[:, h * 128:(h + 1) * 128], rhs=vb[:, h * 128:(h + 1) * 128], start=True, stop=True))(h, pt),
              reads=[kd, vb], writes=[pt])
        for h in range(H):
            pt = pst[h // 4]
            o("dve", (lambda h, pt: lambda e: e.scalar_tensor_tensor(out=self.state[:, h, :], in0=self.state[:, h, :], scalar=self.cd[h], in1=pt[:, (h % 4) * 128:(h % 4 + 1) * 128], op0=ALU.mult, op1=ALU.add))(h, pt),
              reads=[self.state, pt], writes=[self.state])

    def phase2(self):
        nc, d, o, cfg = self.nc, self.dram, self.op, self.cfg
        NM, TCH, NE, C, NRT = cfg.nmain, cfg.tch, cfg.ne, cfg.cap, cfg.nrt
        TT = TCH * 128
        with ExitStack() as pes:
            sb = lambda n, sh, dt=F32: T(pes.enter_context(nc.sbuf_tensor("p2_" + n, list(sh), dt)), n)
            ps = lambda n, sh, dt=F32: T(pes.enter_context(nc.psum_tensor("p2_" + n, list(sh), dt)), n)
            self.load_consts_ret(sb, True)
            bands = sb("bands", [128, 16, 128], BF16)
            self.load("pool", bands, bands[:, :, :], d["bands"][:, :, :], group="c2p")
            causal = sb("causal", [128, 128]); qsc = sb("qsc", [128, H, 128]); kusc = sb("kusc", [128, H, 128])
            tri = sb("tri", [128, 128], BF16); ones_b = sb("ones_b", [128, 128], BF16)
            iota_e = sb("iota_e", [128, NE]); brt = sb("brt", [128, NRT]); pscale = sb("pscale", [128, 8])
            wrt = sb("wrt", [128, KC, NRT]); wpool = sb("wpool", [128, 8, 256], BF16)
            pos_i = sb("pos_i", [128, NM], I32); pos_f = sb("pos_f", [128, NM])
            for t, src in ((causal, d["causalT"][:, :]), (qsc, d["qscaleT"][:, :, :]), (kusc, d["kuscaleT"][:, :, :]), (iota_e, d["iota_e"][:, :]),
                           (brt, d["brt_bc"][:, :]), (pscale, d["pscale_fm"][:, :]), (pos_i, d["pos_main"][:, :]),
                           (wrt, d["w_rt"][:, :].rearrange("(k p) n -> p k n", p=128))):
                self.load("sp", t, t[(slice(None),) * len(src.shape)], src, group="c2")
            self.load("pool", tri, tri[:, :], d["tri_u"][:, :], group="c2p")
            self.load("pool", wpool, wpool[:, :, :], d["w_pool"][:, :, :].rearrange("g (hh p) n -> p (g hh) n", p=128), group="c2p")
            o("pool", lambda e: e.memset(ones_b[:, :], 1.0), writes=[ones_b])
            o("dve", lambda e: e.tensor_copy(out=pos_f[:, :], in_=pos_i[:, :]), reads=[pos_i], writes=[pos_f])
            o("pool", lambda e: e.tensor_copy(out=self.state_bf[:, :, :], in_=self.state[:, :, :]), reads=[self.state], writes=[self.state_bf])
            msum = sb("msum", [128, NE], BF16)
            o("dve", lambda e: e.memset(msum[:, :], 0.0), writes=[msum])
            xring = Ring([sb("x%d" % i, [128, D]) for i in range(2)])
            smalls = Ring([(sb("ssq%d" % i, [128, 1]), sb("rt%d" % i, [128, 1]), sb("rstd%d" % i, [128, 1])) for i in range(2)])
            xnbs = Ring([sb("xnb%d" % i, [128, D], BF16) for i in range(2)])
            hT = sb("hT", [128, KC, TT], BF16)
            ptrs = [ps("ptr%d" % i, [128, 1024], BF16) for i in range(2)]
            pf = Ring([ps("pf%d" % i, [128, 512]) for i in range(6)])
            wr = Ring([sb("wslot%d" % i, [128, 4096], BF16) for i in range(5)])
            tws = Ring([self.make_trig_work(sb, "tw%d" % i) for i in range(1)])
            cos_l = [sb("cos%d" % i, [128, 64]) for i in range(TCH)]
            sin_l = [sb("sin%d" % i, [128, 64]) for i in range(TCH)]
            tring = Ring([sb("rt_tmp%d" % i, [128, 128]) for i in range(8)])
            a_ring = Ring([sb("a_tok%d" % i, [128, 1024], BF16) for i in range(TCH + 1)])
            bufA = sb("bufA", [128, 8, TT], BF16)
            bufB = sb("bufB", [128, 8, TT], BF16)
            mgT = sb("mgT", [128, KC, TT], BF16)
            sgt_r = Ring([sb("sgt%d" % i, [128, TT]) for i in range(2)])
            tmp2_r = Ring([sb("tmp2_%d" % i, [128, TT], BF16) for i in range(2)])
            qrot = [sb("qrot%d" % i, [128, 1024], BF16) for i in range(TCH)]
            krot = [sb("krot%d" % i, [128, 1024], BF16) for i in range(TCH)]
            kd_l = [sb("kd%d" % i, [128, 1024], BF16) for i in range(TCH)]
            v_l = [sb("v%d" % i, [128, 1024], BF16) for i in range(TCH)]
            srg_l = [sb("srg%d" % i, [128, 1024], BF16) for i in range(TCH)]
            qdT = sb("qdT", [128, H, TT], BF16); kuT = sb("kuT", [128, H, TT], BF16); retT = sb("retT", [128, H, TT], BF16)
            sbf = sb("sbf", [128, H, 128], BF16)
            sqj = sb("sqj", [128, 128], BF16); ont = sb("ont", [128, 1024]); ret_tok = sb("ret_tok", [128, 1024], BF16)
            ssqh = sb("ssqh", [128, H]); rth = sb("rth", [128, H]); rstdh = sb("rstdh", [128, H])
            xp_r = Ring([sb("xp%d" % i, [128, 256]) for i in range(4)])
            xnp_r = Ring([sb("xnp%d" % i, [128, 256]) for i in range(4)])
            junk2 = sb("junk2", [128, 256], BF16)
            ssq2 = [sb("ssq2_%d" % i, [128, 8]) for i in range(TCH)]
            x1t_r = xring
            xn2bf_r = xnbs
            h2T_r = Ring([sb("h2T%d" % i, [128, 4, 128]) for i in range(2)])
            rsm = {n: sb("rs_" + n, [128, w]) for n, w in (("ssum", 1), ("rt2", 1), ("rstd2", 1), ("lg", 4), ("gmax", 1), ("ngmax", 1), ("ohg", 4), ("eg", 4), ("sume", 1), ("pgrp", 1),
                                                               ("pen", 4), ("lem", NE), ("m1", 1), ("oh1", NE), ("lem2", NE), ("m2", 1), ("oh2", NE), ("dd", 1), ("ed", 1), ("den", 1), ("rr", 1),
                                                               ("mm", NE), ("prod", NE), ("rank", 2), ("eidx", 2), ("ovf", 2), ("destf", 2), ("pfx", NE))}
            mbf = sb("mbf", [128, NE], BF16)
            sidx_r = Ring([sb("sidx%d" % i, [128, 2], I32) for i in range(4)])
            kdb = self.kdscale[:, :].unsqueeze(2).to_broadcast([128, H, 128])
            self.p2_sbuf_left = nc.sbuf_bytes_remaining

            def wload(q, src_ap, k, reads=()):
                slot = wr.next()
                view = slot[:, :].rearrange("p (k n) -> p k n", k=k)
                self.load(q, slot, slot[:, :], src_ap, reads=reads)
                return slot, view

            def tile_body(ti, a_prev):
                gch = [ti * TCH + lc for lc in range(TCH)]
                for lc, c in enumerate(gch):
                    xnb_ = xnbs.next()
                    self.norm_transpose(d["x_main"][c * 128:(c + 1) * 128, :], xring.next(), xnb_, smalls.next(), xnb_, ptrs, hT, lc * 128)
                    self.trig(pos_f, c, tws.next(), cos_l[lc], sin_l[lc])
                a_l = [a_ring.next() for _ in range(TCH)]
                for u in range(4):
                    slot, wv = wload("sp", d["w_in_b"][OFF_A // 256 + u], KC, reads=[self.dbuf["w_in_b"]])
                    for lc in range(TCH):
                        pt = pf.next()
                        for kc in range(KC):
                            o("pe", (lambda kc, pt, wv, lc: lambda e: e.matmul(out=pt[:, 0:256], lhsT=hT[:, kc, lc * 128:(lc + 1) * 128], rhs=wv[:, kc, :], start=(kc == 0), stop=(kc == KC - 1)))(kc, pt, wv, lc),
                              reads=[hT, slot], writes=[pt])
                        o("act", (lambda pt, lc, u: lambda e: e.activation(out=a_l[lc][:, u * 256:(u + 1) * 256], in_=pt[:, 0:256], func=AF.Copy))(pt, lc, u), reads=[pt], writes=[a_l[lc]])
                for lc, c in enumerate(gch):
                    var = 8 if c == 0 else 0
                    acur = a_l[lc]; aprv = a_prev if lc == 0 else a_l[lc - 1]
                    for jb in range(2):
                        pt = pf.next()
                        for q4 in range(4):
                            j = jb * 4 + q4; g = j // 2
                            o("pe", (lambda pt, q4, j, g, acur, var: lambda e: e.matmul(out=pt[:, q4 * 128:(q4 + 1) * 128], lhsT=acur[:, j * 128:(j + 1) * 128], rhs=bands[:, var + g * 2, :], start=True, stop=False))(pt, q4, j, g, acur, var),
                              reads=[acur, bands], writes=[pt])
                            o("pe", (lambda pt, q4, j, g, aprv, var: lambda e: e.matmul(out=pt[:, q4 * 128:(q4 + 1) * 128], lhsT=aprv[:, j * 128:(j + 1) * 128], rhs=bands[:, var + g * 2 + 1, :], start=False, stop=True))(pt, q4, j, g, aprv, var),
                              reads=[aprv, bands], writes=[pt])
                        o("dve", (lambda pt, jb, lc: lambda e: e.tensor_copy(out=bufA[:, jb * 4:(jb + 1) * 4, lc * 128:(lc + 1) * 128], in_=pt[:, :].rearrange("p (j t) -> p j t", j=4)))(pt, jb, lc),
                          reads=[pt], writes=[bufA])
                a_prev_next = a_l[TCH - 1]
                for jo in range(8):
                    g, dh = jo // 2, jo % 2
                    pt = pf.next()
                    for hh in range(2):
                        o("pe", (lambda pt, g, dh, hh: lambda e: e.matmul(out=pt[:, 0:TT], lhsT=wpool[:, g * 2 + hh, dh * 128:(dh + 1) * 128], rhs=bufA[:, g * 2 + hh, :], start=(hh == 0), stop=(hh == 1)))(pt, g, dh, hh),
                          reads=[wpool, bufA], writes=[pt])
                    o("act", (lambda pt, jo: lambda e: e.activation(out=bufB[:, jo, :], in_=pt[:, 0:TT], func=AF.Copy, scale=pscale[:, jo:jo + 1]))(pt, jo), reads=[pt, pscale], writes=[bufB])
                self.gated_branch(OFF_GA, "w_bp", bufB, mgT, True, wload, pf, hT, sgt_r, tmp2_r, TT)
                for which, off, rot_l in (("q", OFF_Q, qrot), ("k", OFF_K, krot)):
                    for u in range(4):
                        slot, wv = wload("sp", d["w_in_b"][off // 256 + u], KC, reads=[self.dbuf["w_in_b"]])
                        for lc in range(TCH):
                            pt = pf.next()
                            for kc in range(KC):
                                o("pe", (lambda kc, pt, wv, lc: lambda e: e.matmul(out=pt[:, 0:256], lhsT=hT[:, kc, lc * 128:(lc + 1) * 128], rhs=wv[:, kc, :], start=(kc == 0), stop=(kc == KC - 1)))(kc, pt, wv, lc),
                                  reads=[hT, slot], writes=[pt])
                            self.rotary(pt, 2, cos_l[lc], sin_l[lc], tring, rot_l[lc], u * 256)
                for lc in range(TCH):
                    for rot_l, dstT, sc in ((qrot, qdT, qsc), (krot, kuT, kusc)):
                        pt = ptrs[0] if rot_l is qrot else ptrs[1]
                        for h in range(H):
                            o("pe", (lambda pt, h, r: lambda e: e.transpose(out=pt[:, h * 128:(h + 1) * 128], in_=r[:, h * 128:(h + 1) * 128], identity=self.ident_b[:, :]))(pt, h, rot_l[lc]),
                              reads=[rot_l[lc], self.ident_b], writes=[pt])
                        o("dve", (lambda pt, dstT, sc, lc: lambda e: e.tensor_tensor(out=dstT[:, :, lc * 128:(lc + 1) * 128], in0=pt[:, :].rearrange("p (h t) -> p h t", h=H), in1=sc[:, :, :], op=ALU.mult))(pt, dstT, sc, lc),
                          reads=[pt, sc], writes=[dstT])
                    o("pool", (lambda lc: lambda e: e.tensor_tensor(out=kd_l[lc][:, :].rearrange("p (h f) -> p h f", h=H), in0=krot[lc][:, :].rearrange("p (h f) -> p h f", h=H), in1=kdb, op=ALU.mult))(lc),
                      reads=[krot[lc], self.kdscale], writes=[kd_l[lc]])
                for off, dst_l, fn in ((OFF_V, v_l, AF.Copy), (OFF_RG, srg_l, AF.Silu)):
                    for u in range(4):
                        slot, wv = wload("sp", d["w_in_b"][off // 256 + u], KC, reads=[self.dbuf["w_in_b"]])
                        for lc in range(TCH):
                            pt = pf.next()
                            for kc in range(KC):
                                o("pe", (lambda kc, pt, wv, lc: lambda e: e.matmul(out=pt[:, 0:256], lhsT=hT[:, kc, lc * 128:(lc + 1) * 128], rhs=wv[:, kc, :], start=(kc == 0), stop=(kc == KC - 1)))(kc, pt, wv, lc),
                                  reads=[hT, slot], writes=[pt])
                            o("act", (lambda pt, lc, u, dst_l, fn: lambda e: e.activation(out=dst_l[lc][:, u * 256:(u + 1) * 256], in_=pt[:, 0:256], func=fn))(pt, lc, u, dst_l, fn), reads=[pt], writes=[dst_l[lc]])
                def ret_chunk(lc):
                    cs = slice(lc * 128, (lc + 1) * 128)
                    pS = [pf.next(), pf.next()]
                    for h in range(H):
                        o("pe", (lambda h, cs: lambda e: e.matmul(out=pS[h // 4][:, (h % 4) * 128:(h % 4 + 1) * 128], lhsT=kuT[:, h, cs], rhs=qdT[:, h, cs], start=True, stop=True))(h, cs),
                          reads=[kuT, qdT], writes=[pS[h // 4]])
                    for jb in range(2):
                        o("dve", (lambda jb: lambda e: e.tensor_tensor(out=sbf[:, jb * 4:(jb + 1) * 4, :], in0=pS[jb][:, :].rearrange("p (h t) -> p h t", h=4), in1=causal[:, :].unsqueeze(1).to_broadcast([128, 4, 128]), op=ALU.mult))(jb),
                          reads=[pS[jb], causal], writes=[sbf])
                    pO = [pf.next(), pf.next()]
                    for h in range(H):
                        o("pe", (lambda h: lambda e: e.matmul(out=pO[h // 4][:, (h % 4) * 128:(h % 4 + 1) * 128], lhsT=sbf[:, h, :], rhs=v_l[lc][:, h * 128:(h + 1) * 128], start=True, stop=False))(h),
                          reads=[sbf, v_l[lc]], writes=[pO[h // 4]])
                        o("pe", (lambda h, cs: lambda e: e.matmul(out=pO[h // 4][:, (h % 4) * 128:(h % 4 + 1) * 128], lhsT=qdT[:, h, cs], rhs=self.state_bf[:, h, :], start=False, stop=True))(h, cs),
                          reads=[qdT, self.state_bf], writes=[pO[h // 4]])
                    for h in range(H):
                        o("act", (lambda h: lambda e: e.activation(out=sqj[:, :], in_=pO[h // 4][:, (h % 4) * 128:(h % 4 + 1) * 128], func=AF.Square, accum_out=ssqh[:, h:h + 1]))(h),
                          reads=[pO[h // 4]], writes=[sqj, ssqh])
                    self.rstd_from_ssq(ssqh[:, :], ssqh, rth, rstdh, 1.0 / 128)
                    for jb in range(2):
                        o("dve", (lambda jb: lambda e: e.tensor_tensor(out=ont[:, jb * 512:(jb + 1) * 512].rearrange("p (h f) -> p h f", h=4), in0=pO[jb][:, :].rearrange("p (h f) -> p h f", h=4),
                                                                      in1=rstdh[:, jb * 4:(jb + 1) * 4].unsqueeze(2).to_broadcast([128, 4, 128]), op=ALU.mult))(jb),
                          reads=[pO[jb], rstdh], writes=[ont])
                    o("pool", (lambda lc: lambda e: e.tensor_tensor(out=ret_tok[:, :], in0=ont[:, :], in1=srg_l[lc][:, :], op=ALU.mult))(lc), reads=[ont, srg_l[lc]], writes=[ret_tok])
                    pt = ptrs[0]
                    for h in range(H):
                        o("pe", (lambda pt, h: lambda e: e.transpose(out=pt[:, h * 128:(h + 1) * 128], in_=ret_tok[:, h * 128:(h + 1) * 128], identity=self.ident_b[:, :]))(pt, h),
                          reads=[ret_tok, self.ident_b], writes=[pt])
                    o("act", (lambda pt, cs: lambda e: e.activation(out=retT[:, :, cs], in_=pt[:, :].rearrange("p (h t) -> p h t", h=H), func=AF.Copy))(pt, cs), reads=[pt], writes=[retT])
                    pU = [pf.next(), pf.next()]
                    self.state_update(kd_l[lc], v_l[lc], pU)
                    o("pool", lambda e: e.tensor_copy(out=self.state_bf[:, :, :], in_=self.state[:, :, :]), reads=[self.state], writes=[self.state_bf])
                for lc in range(TCH):
                    ret_chunk(lc)
                self.gated_branch(OFF_GB, "w_br", retT, mgT, False, wload, pf, hT, sgt_r, tmp2_r, TT)
                for u in range(8):
                    slot, wv = wload("sp", d["w_out_g"][u], KC, reads=[self.dbuf["w_out_g"]])
                    for lc, c in enumerate(gch):
                        pt = pf.next()
                        for kc in range(KC):
                            o("pe", (lambda kc, pt, wv, lc: lambda e: e.matmul(out=pt[:, 0:256], lhsT=mgT[:, kc, lc * 128:(lc + 1) * 128], rhs=wv[:, kc, :], start=(kc == 0), stop=(kc == KC - 1)))(kc, pt, wv, lc),
                              reads=[mgT, slot], writes=[pt])
                        xp = xp_r.next(); xnp = xnp_r.next()
                        self.load("pool", xp, xp[:, :], d["x_main"][c * 128:(c + 1) * 128, u * 256:(u + 1) * 256])
                        o("dve", (lambda pt, xp, xnp: lambda e: e.tensor_tensor(out=xnp[:, :], in0=pt[:, 0:256], in1=xp[:, :], op=ALU.add))(pt, xp, xnp), reads=[pt, xp], writes=[xnp])
                        o("act", (lambda xnp, lc, u: lambda e: e.activation(out=junk2[:, :], in_=xnp[:, :], func=AF.Square, accum_out=ssq2[lc][:, u:u + 1]))(xnp, lc, u), reads=[xnp], writes=[junk2, ssq2[lc]])
                        self.store("pool", xnp, d["x1_scr"][c * 128:(c + 1) * 128, u * 256:(u + 1) * 256], xnp[:, :], writes=[self.x1_bufs[c]])
                        if self.debug:
                            self.dma("sp", (lambda xnp, c, u: lambda e: e.dma_start(out=d["dbg_x1"][c * 128:(c + 1) * 128, u * 256:(u + 1) * 256], in_=xnp[:, :]))(xnp, c, u), reads=[xnp], key=("stdbg", id(xnp.b)))
                for lc, c in enumerate(gch):
                    self.route_chunk(lc, c, ssq2[lc], rsm, x1t_r.next(), xn2bf_r.next(), h2T_r, pf, wrt, brt, iota_e, tri, ones_b, msum, mbf, sidx_r)
                return a_prev_next

            a_prev = self.a_prev0
            for ti in range(NM // TCH):
                a_prev = tile_body(ti, a_prev)
            self.overflow_dispatch(sb, pf, ones_b, msum, iota_e, x1t_r, xn2bf_r, sidx_r)
            self.P.emit()

    def gated_branch(self, off, wname, rhsT, mgT, first, wload, pf, hT, sgt_r, tmp2_r, TT):
        d, o = self.dram, self.op
        gslot = gv = bslot = bv = None
        for nch in range(KC):
            if nch % 2 == 0:
                gslot, gv = wload("sp", d["w_in_b"][off // 256 + nch // 2], KC, reads=[self.dbuf["w_in_b"]])
            if nch % 4 == 0:
                bslot, bv = wload("sp", d[wname + "_b"][nch // 4], 8, reads=[self.dbuf[wname + "_b"]])
            pA = pf.next()
            for kc in range(KC):
                o("pe", (lambda kc, pA, gv, nch: lambda e: e.matmul(out=pA[:, 0:TT], lhsT=gv[:, kc, (nch % 2) * 128:(nch % 2 + 1) * 128], rhs=hT[:, kc, :], start=(kc == 0), stop=(kc == KC - 1)))(kc, pA, gv, nch),
                  reads=[gslot, hT], writes=[pA])
            pB = pf.next()
            for kc in range(8):
                o("pe", (lambda kc, pB, bv, nch: lambda e: e.matmul(out=pB[:, 0:TT], lhsT=bv[:, kc, (nch % 4) * 128:(nch % 4 + 1) * 128], rhs=rhsT[:, kc, :], start=(kc == 0), stop=(kc == 7)))(kc, pB, bv, nch),
                  reads=[bslot, rhsT], writes=[pB])
            sgt = sgt_r.next()
            o("act", (lambda pA, sgt: lambda e: e.activation(out=sgt[:, :], in_=pA[:, 0:TT], func=AF.Sigmoid))(pA, sgt), reads=[pA], writes=[sgt])
            if first:
                o("dve", (lambda pB, sgt, nch: lambda e: e.tensor_tensor(out=mgT[:, nch, :], in0=pB[:, 0:TT], in1=sgt[:, :], op=ALU.mult))(pB, sgt, nch), reads=[pB, sgt], writes=[mgT])
            else:
                tmp2 = tmp2_r.next()
                o("dve", (lambda pB, sgt, tmp2: lambda e: e.tensor_tensor(out=tmp2[:, :], in0=pB[:, 0:TT], in1=sgt[:, :], op=ALU.mult))(pB, sgt, tmp2), reads=[pB, sgt], writes=[tmp2])
                o("pool", (lambda tmp2, nch: lambda e: e.tensor_tensor(out=mgT[:, nch, :], in0=mgT[:, nch, :], in1=tmp2[:, :], op=ALU.add))(tmp2, nch), reads=[mgT, tmp2], writes=[mgT])

    def route_chunk(self, lc, c, ssq2_t, R, x1t, xn2bf, h2T_r, pf, wrt, brt, iota_e, tri, ones_b, msum, mbf, sidx_r):
        d, o, cfg = self.dram, self.op, self.cfg
        NE, C, NRT, EPG = cfg.ne, cfg.cap, cfg.nrt, cfg.epg
        ROWS = cfg.rows
        o("dve", lambda e: e.tensor_reduce(out=R["ssum"][:, :], in_=ssq2_t[:, :], axis=AX.X, op=ALU.add), reads=[ssq2_t], writes=[R["ssum"]])
        self.rstd_from_ssq(R["ssum"][:, :], R["ssum"], R["rt2"], R["rstd2"], 1.0 / D)
        o("dve", lambda e: e.tensor_copy(out=self.rstd2_all[:, c:c + 1], in_=R["rstd2"][:, :]), reads=[R["rstd2"]], writes=[self.rt_bufs[c]])
        self.load("sp", x1t, x1t[:, :], d["x1_scr"][c * 128:(c + 1) * 128, :], reads=[self.x1_bufs[c]])
        o("dve", lambda e: e.tensor_scalar(out=x1t[:, :], in0=x1t[:, :], scalar1=R["rstd2"][:, 0:1], scalar2=None, op0=ALU.mult), reads=[x1t, R["rstd2"]], writes=[x1t])
        o("act", lambda e: e.activation(out=xn2bf[:, :], in_=x1t[:, :], func=AF.Copy), reads=[x1t], writes=[xn2bf])
        pL = pf.next()
        for grp in range(4):
            pt = pf.next()
            for q4 in range(4):
                kc = grp * 4 + q4
                o("pe", (lambda pt, q4, kc: lambda e: e.transpose(out=pt[:, q4 * 128:(q4 + 1) * 128], in_=x1t[:, kc * 128:(kc + 1) * 128], identity=self.ident_f[:, :]))(pt, q4, kc),
                  reads=[x1t, self.ident_f], writes=[pt])
            h2 = h2T_r.next()
            for q4 in range(4):
                kc = grp * 4 + q4
                if q4 % 2 == 0:
                    o("act", (lambda pt, q4, kc, h2: lambda e: e.activation(out=h2[:, q4, :], in_=pt[:, q4 * 128:(q4 + 1) * 128], func=AF.Identity, scale=self.g2eff[:, kc:kc + 1], bias=self.shift2[:, kc:kc + 1]))(pt, q4, kc, h2),
                      reads=[pt, self.g2eff, self.shift2], writes=[h2])
                else:
                    o("dve", (lambda pt, q4, kc, h2: lambda e: e.tensor_scalar(out=h2[:, q4, :], in0=pt[:, q4 * 128:(q4 + 1) * 128], scalar1=self.g2eff[:, kc:kc + 1], scalar2=self.shift2[:, kc:kc + 1], op0=ALU.mult, op1=ALU.add))(pt, q4, kc, h2),
                      reads=[pt, self.g2eff, self.shift2], writes=[h2])
            for q4 in range(4):
                kc = grp * 4 + q4
                o("pe", (lambda q4, kc, h2: lambda e: e.matmul(out=pL[:, 0:NRT], lhsT=h2[:, q4, :], rhs=wrt[:, kc, :], start=(kc == 0), stop=(kc == KC - 1)))(q4, kc, h2),
                  reads=[h2, wrt], writes=[pL])
        def ts(out, in0, s1, s2, op0, op1=None, reads=(), writes=()):
            if op1 is None:
                o("dve", lambda e: e.tensor_scalar(out=out, in0=in0, scalar1=s1, scalar2=None, op0=op0), reads=reads, writes=writes)
            else:
                o("dve", lambda e: e.tensor_scalar(out=out, in0=in0, scalar1=s1, scalar2=s2, op0=op0, op1=op1), reads=reads, writes=writes)

        def tt(out, in0, in1, op, reads=(), writes=()):
            o("dve", lambda e: e.tensor_tensor(out=out, in0=in0, in1=in1, op=op), reads=reads, writes=writes)

        def red(out, in_, op, reads=(), writes=()):
            o("dve", lambda e: e.tensor_reduce(out=out, in_=in_, axis=AX.X, op=op), reads=reads, writes=writes)
        lg, gmax, ngmax, ohg, eg, sume, pgrp, pen = (R[n] for n in ("lg", "gmax", "ngmax", "ohg", "eg", "sume", "pgrp", "pen"))
        lem, m1, oh1, lem2, m2, oh2, dd, ed, den, rr = (R[n] for n in ("lem", "m1", "oh1", "lem2", "m2", "oh2", "dd", "ed", "den", "rr"))
        mm, prod, rank, eidx, ovf, destf, pfx = (R[n] for n in ("mm", "prod", "rank", "eidx", "ovf", "destf", "pfx"))
        gwb, dsb = self.gw_bufs[c], self.dest_bufs[c]
        tt(lg[:, :], pL[:, 0:4], brt[:, 0:4], ALU.add, [pL, brt], [lg])
        red(gmax[:, :], lg[:, :], ALU.max, [lg], [gmax])
        ts(ohg[:, :], lg[:, :], gmax[:, 0:1], None, ALU.is_equal, None, [lg, gmax], [ohg])
        ts(ngmax[:, :], gmax[:, :], -1.0, None, ALU.mult, None, [gmax], [ngmax])
        o("act", lambda e: e.activation(out=eg[:, :], in_=lg[:, :], func=AF.Exp, bias=ngmax[:, 0:1], accum_out=sume[:, 0:1]), reads=[lg, ngmax], writes=[eg, sume])
        o("dve", lambda e: e.reciprocal(out=pgrp[:, :], in_=sume[:, :]), reads=[sume], writes=[pgrp])
        ts(pen[:, :], ohg[:, :], 1.0, 1e30, ALU.subtract, ALU.mult, [ohg], [pen])
        tt(lem[:, :], pL[:, 4:4 + NE], brt[:, 4:4 + NE], ALU.add, [pL, brt], [lem])
        tt(lem[:, :].rearrange("p (g e) -> p g e", g=NG), lem[:, :].rearrange("p (g e) -> p g e", g=NG), pen[:, :].unsqueeze(2).to_broadcast([128, NG, EPG]), ALU.add, [lem, pen], [lem])
        red(m1[:, :], lem[:, :], ALU.max, [lem], [m1])
        ts(oh1[:, :], lem[:, :], m1[:, 0:1], None, ALU.is_equal, None, [lem, m1], [oh1])
        o("dve", lambda e: e.scalar_tensor_tensor(out=lem2[:, :], in0=oh1[:, :], scalar=-1e30, in1=lem[:, :], op0=ALU.mult, op1=ALU.add), reads=[oh1, lem], writes=[lem2])
        red(m2[:, :], lem2[:, :], ALU.max, [lem2], [m2])
        ts(oh2[:, :], lem2[:, :], m2[:, 0:1], None, ALU.is_equal, None, [lem2, m2], [oh2])
        tt(dd[:, :], m2[:, :], m1[:, :], ALU.subtract, [m1, m2], [dd])
        o("act", lambda e: e.activation(out=ed[:, :], in_=dd[:, :], func=AF.Exp), reads=[dd], writes=[ed])
        ts(den[:, :], ed[:, :], 1.0, None, ALU.add, None, [ed], [den])
        o("dve", lambda e: e.reciprocal(out=rr[:, :], in_=den[:, :]), reads=[den], writes=[rr])
        tt(self.gw_all[:, c, 0:1], rr[:, :], pgrp[:, :], ALU.mult, [rr, pgrp], [gwb])
        tt(self.gw_all[:, c, 1:2], pgrp[:, :], self.gw_all[:, c, 0:1], ALU.subtract, [pgrp, gwb], [gwb])
        tt(mm[:, :], oh1[:, :], oh2[:, :], ALU.add, [oh1, oh2], [mm])
        o("dve", lambda e: e.tensor_copy(out=mbf[:, :], in_=mm[:, :]), reads=[mm], writes=[mbf])
        pP = pf.next()
        o("pe", lambda e: e.matmul(out=pP[:, 0:NE], lhsT=tri[:, :], rhs=mbf[:, :], start=True, stop=False), reads=[tri, mbf], writes=[pP])
        o("pe", lambda e: e.matmul(out=pP[:, 0:NE], lhsT=ones_b[:, :], rhs=msum[:, :], start=False, stop=True), reads=[ones_b, msum], writes=[pP])
        o("dve", lambda e: e.tensor_copy(out=pfx[:, :], in_=pP[:, 0:NE]), reads=[pP], writes=[pfx])
        for k, oh in ((0, oh1), (1, oh2)):
            tt(prod[:, :], oh[:, :], pfx[:, :], ALU.mult, [oh, pfx], [prod])
            red(rank[:, k:k + 1], prod[:, :], ALU.add, [prod], [rank])
            tt(prod[:, :], oh[:, :], iota_e[:, :], ALU.mult, [oh, iota_e], [prod])
            red(eidx[:, k:k + 1], prod[:, :], ALU.add, [prod], [eidx])
        ts(ovf[:, :], rank[:, :], float(C), float(ROWS), ALU.is_ge, ALU.mult, [rank], [ovf])
        o("dve", lambda e: e.scalar_tensor_tensor(out=destf[:, :], in0=eidx[:, :], scalar=float(C), in1=rank[:, :], op0=ALU.mult, op1=ALU.add), reads=[eidx, rank], writes=[destf])
        tt(destf[:, :], destf[:, :], ovf[:, :], ALU.add, [destf, ovf], [destf])
        ts(destf[:, :], destf[:, :], float(ROWS + 7), None, ALU.min, None, [destf], [destf])
        sidx = sidx_r.next()
        o("dve", lambda e: e.tensor_copy(out=sidx[:, :], in_=destf[:, :]), reads=[destf], writes=[sidx])
        o("dve", lambda e: e.tensor_copy(out=self.destf_all[:, c, :], in_=destf[:, :]), reads=[destf], writes=[self.rt_bufs[c]])
        o("dve", lambda e: e.tensor_copy(out=self.rank_all[:, c, :], in_=rank[:, :]), reads=[rank], writes=[self.rt_bufs[c]])
        o("dve", lambda e: e.tensor_copy(out=self.eidx_all[:, c, :], in_=eidx[:, :]), reads=[eidx], writes=[self.rt_bufs[c]])
        tt(msum[:, :], msum[:, :], mm[:, :], ALU.add, [msum, mm], [msum])
        for k in range(2):
            self.dma("pool", (lambda k: lambda e: e.indirect_dma_start(out=d["xs_scr"][:, :], out_offset=bass.IndirectOffsetOnAxis(ap=sidx[:, k:k + 1], axis=0),
                                                                      in_=xn2bf[:, :], in_offset=None, bounds_check=self.bound_reg(e, ROWS - 1), oob_is_err=False))(k),
                     reads=[xn2bf, sidx], writes=[self.dbuf["xs_scr"]], key=("st", id(xn2bf.b)))
        if self.debug and c == cfg.nmain - 1:
            self.dma("sp", lambda e: e.dma_start(out=d["dbg_gw"][:, :, :], in_=self.gw_all[:, :, :]), reads=self.gw_bufs, key=("st", "dbggw"))
            self.dma("sp", lambda e: e.dma_start(out=d["dbg_state"][:, :, :], in_=self.state[:, :, :]), reads=[self.state], key=("st", "dbgstate"))

    def overflow_dispatch(self, sb, pf, ones_b, msum, iota_e, x1t_r, xn2bf_r, sidx_r):
        d, o, cfg = self.dram, self.op, self.cfg
        NE, C, NS, NM, ROWS = cfg.ne, cfg.cap, cfg.ns, cfg.nmain, cfg.rows
        OOB = float(ROWS + 7)
        HALF = C / 2.0 - 0.5
        cnt = sb("od_cnt", [128, NE]); t1 = sb("od_t1", [128, NE]); ni = sb("od_ni", [128, NE], I32)
        nov = sb("od_nov", [128, NE]); ca = sb("od_ca", [128, NE]); cb = sb("od_cb", [128, NE]); obase = sb("od_obase", [128, NE])
        esf = sb("od_esf", [128, NS]); tmpe = sb("od_tmpe", [128, NE])
        pC = pf.next()
        o("pe", lambda e: e.matmul(out=pC[:, 0:NE], lhsT=ones_b[:, :], rhs=msum[:, :], start=True, stop=True), reads=[ones_b, msum], writes=[pC])
        o("dve", lambda e: e.tensor_copy(out=cnt[:, :], in_=pC[:, 0:NE]), reads=[pC], writes=[cnt])
        o("dve", lambda e: e.tensor_scalar(out=t1[:, :], in0=cnt[:, :], scalar1=float(-C), scalar2=0.0, op0=ALU.add, op1=ALU.max), reads=[cnt], writes=[t1])
        o("dve", lambda e: e.tensor_scalar(out=t1[:, :], in0=t1[:, :], scalar1=HALF, scalar2=1.0 / C, op0=ALU.add, op1=ALU.mult), reads=[t1], writes=[t1])
        o("dve", lambda e: e.tensor_copy(out=ni[:, :], in_=t1[:, :]), reads=[t1], writes=[ni])
        o("dve", lambda e: e.tensor_copy(out=nov[:, :], in_=ni[:, :]), reads=[ni], writes=[nov])
        cur, nxt = nov, ca
        sh = 1
        while sh < NE:
            o("dve", (lambda cur, nxt, sh: lambda e: e.tensor_copy(out=nxt[:, 0:sh], in_=cur[:, 0:sh]))(cur, nxt, sh), reads=[cur], writes=[nxt])
            o("dve", (lambda cur, nxt, sh: lambda e: e.tensor_tensor(out=nxt[:, sh:NE], in0=cur[:, sh:NE], in1=cur[:, 0:NE - sh], op=ALU.add))(cur, nxt, sh), reads=[cur], writes=[nxt])
            cur, nxt = nxt, (cb if nxt is ca else ca)
            sh *= 2
        oincl = cur
        o("dve", lambda e: e.tensor_tensor(out=obase[:, :], in0=oincl[:, :], in1=nov[:, :], op=ALU.subtract), reads=[oincl, nov], writes=[obase])
        for s_ in range(NS):
            o("dve", (lambda s_: lambda e: e.tensor_scalar(out=tmpe[:, :], in0=oincl[:, :], scalar1=float(s_), scalar2=None, op0=ALU.is_le))(s_), reads=[oincl], writes=[tmpe])
            o("dve", (lambda s_: lambda e: e.tensor_reduce(out=esf[:, s_:s_ + 1], in_=tmpe[:, :], axis=AX.X, op=ALU.add))(s_), reads=[tmpe], writes=[esf])
        iop = sb("od_iop", [128, 1]); esc = sb("od_esc", [128, NS]); esc4 = sb("od_esc4", [128, NS * 4])
        self.load("sp", iop, iop[:, :], d["iota_p"][:, :])
        o("dve", lambda e: e.tensor_scalar(out=esc[:, :], in0=esf[:, :], scalar1=512.0, scalar2=iop[:, 0:1], op0=ALU.mult, op1=ALU.add), reads=[esf, iop], writes=[esc])
        for u in range(4):
            o("dve", (lambda u: lambda e: e.tensor_scalar(out=esc4[:, :].rearrange("p (s u) -> p s u", u=4)[:, :, u], in0=esc[:, :], scalar1=float(u * 128), scalar2=None, op0=ALU.add))(u), reads=[esc], writes=[esc4])
        o("dve", lambda e: e.tensor_copy(out=self.slot_idx[:, 0:4 * NS], in_=esc4[:, :]), reads=[esc4], writes=[self.slot_idx])
        sm = {n: sb("od_" + n, [128, w]) for n, w in (("ov", 1), ("q", 1), ("qf", 1), ("oh", NE), ("ob", 1), ("a", 1), ("b", 1), ("fl", 1), ("dl", 2), ("sx", 2), ("df", 2))}
        qi = sb("od_qi", [128, 1], I32)

        def chunk(c):
            x1t = x1t_r.next(); xn2bf = xn2bf_r.next(); sidx = sidx_r.next()
            self.load("sp", x1t, x1t[:, :], d["x1_scr"][c * 128:(c + 1) * 128, :], reads=[self.x1_bufs[c]])
            o("dve", lambda e: e.tensor_scalar(out=x1t[:, :], in0=x1t[:, :], scalar1=self.rstd2_all[:, c:c + 1], scalar2=None, op0=ALU.mult), reads=[x1t, self.rt_bufs[c]], writes=[x1t])
            o("act", lambda e: e.activation(out=xn2bf[:, :], in_=x1t[:, :], func=AF.Copy), reads=[x1t], writes=[xn2bf])
            ov, q, qf, oh, ob, a, b, fl, dl, sx, df = (sm[n] for n in ("ov", "q", "qf", "oh", "ob", "a", "b", "fl", "dl", "sx", "df"))
            def per_k(k):
                rk = self.rank_all[:, c, k:k + 1]; ek = self.eidx_all[:, c, k:k + 1]
                o("dve", lambda e: e.tensor_scalar(out=ov[:, :], in0=rk, scalar1=float(-C), scalar2=None, op0=ALU.add), reads=[self.rt_bufs[c]], writes=[ov])
                o("dve", lambda e: e.tensor_scalar(out=q[:, :], in0=ov[:, :], scalar1=-HALF, scalar2=1.0 / C, op0=ALU.add, op1=ALU.mult), reads=[ov], writes=[q])
                o("dve", lambda e: e.tensor_copy(out=qi[:, :], in_=q[:, :]), reads=[q], writes=[qi])
                o("dve", lambda e: e.tensor_copy(out=qf[:, :], in_=qi[:, :]), reads=[qi], writes=[qf])
                o("dve", lambda e: e.tensor_scalar(out=oh[:, :], in0=iota_e[:, :], scalar1=ek, scalar2=None, op0=ALU.is_equal), reads=[iota_e, self.rt_bufs[c]], writes=[oh])
                o("dve", lambda e: e.tensor_tensor(out=oh[:, :], in0=oh[:, :], in1=obase[:, :], op=ALU.mult), reads=[oh, obase], writes=[oh])
                o("dve", lambda e: e.tensor_reduce(out=ob[:, :], in_=oh[:, :], axis=AX.X, op=ALU.add), reads=[oh], writes=[ob])
                o("dve", lambda e: e.scalar_tensor_tensor(out=a[:, :], in0=ob[:, :], scalar=float(NE), in1=qf[:, :], op0=ALU.add, op1=ALU.add), reads=[ob, qf], writes=[a])
                o("dve", lambda e: e.scalar_tensor_tensor(out=b[:, :], in0=qf[:, :], scalar=float(-C), in1=ov[:, :], op0=ALU.mult, op1=ALU.add), reads=[qf, ov], writes=[b])
                o("dve", lambda e: e.scalar_tensor_tensor(out=a[:, :], in0=a[:, :], scalar=float(C), in1=b[:, :], op0=ALU.mult, op1=ALU.add), reads=[a, b], writes=[a])
                o("dve", lambda e: e.tensor_scalar(out=fl[:, :], in0=ov[:, :], scalar1=0.0, scalar2=None, op0=ALU.is_ge), reads=[ov], writes=[fl])
                o("dve", lambda e: e.scalar_tensor_tensor(out=dl[:, k:k + 1], in0=a[:, :], scalar=-OOB, in1=fl[:, :], op0=ALU.add, op1=ALU.mult), reads=[a, fl], writes=[dl])
            per_k(0)
            per_k(1)
            o("dve", lambda e: e.tensor_scalar(out=sx[:, :], in0=dl[:, :], scalar1=OOB, scalar2=None, op0=ALU.add), reads=[dl], writes=[sx])
            o("dve", lambda e: e.tensor_copy(out=sidx[:, :], in_=sx[:, :]), reads=[sx], writes=[sidx])
            o("dve", lambda e: e.tensor_tensor(out=df[:, :], in0=self.destf_all[:, c, :], in1=dl[:, :], op=ALU.add), reads=[self.rt_bufs[c], dl], writes=[df])
            o("dve", lambda e: e.tensor_copy(out=self.dest_all[:, c, :], in_=df[:, :]), reads=[df], writes=[self.dest_bufs[c]])
            for k in range(2):
                self.dma("pool", (lambda k: lambda e: e.indirect_dma_start(out=d["xs_scr"][:, :], out_offset=bass.IndirectOffsetOnAxis(ap=sidx[:, k:k + 1], axis=0),
                                                                          in_=xn2bf[:, :], in_offset=None, bounds_check=self.bound_reg(e, ROWS - 1), oob_is_err=False))(k),
                         reads=[xn2bf, sidx], writes=[self.dbuf["xs_scr"]], key=("st", id(xn2bf.b)))

        for c in range(NM):
            chunk(c)
        if self.debug:
            self.dma("sp", lambda e: e.dma_start(out=d["dbg_dest"][:, :, :], in_=self.dest_all[:, :, :]), reads=self.dest_bufs, key=("st", "dbgdest"))

    def phase3(self):
        nc, d, o, cfg = self.nc, self.dram, self.op, self.cfg
        NE, C, NB = cfg.ne, cfg.cap, cfg.nb
        with ExitStack() as pes:
            sb = lambda n, sh, dt=F32: T(pes.enter_context(nc.sbuf_tensor("p3_" + n, list(sh), dt)), n)
            ps = lambda n, sh, dt=F32: T(pes.enter_context(nc.psum_tensor("p3_" + n, list(sh), dt)), n)
            xst_r = Ring([sb("xst%d" % i, [128, D], BF16) for i in range(3)])
            xbT_r = Ring([sb("xbT%d" % i, [128, KC, cfg.wcap], BF16) for i in range(2 * cfg.nwin)])
            hidT_r = Ring([sb("hidT%d" % i, [128, FC, cfg.wcap], BF16) for i in range(2 * cfg.nwin)])
            w13_r = Ring([sb("w13_%d" % i, [128, KC, 256], BF16) for i in range(6)])
            w2_r = Ring([sb("w2_%d" % i, [128, FC, 512], BF16) for i in range(4)])
            st_r = Ring([sb("silu%d" % i, [128, cfg.wcap]) for i in range(2)])
            yt_r = Ring([sb("yt%d" % i, [128, 512]) for i in range(6)])
            ptrs = [ps("ptr%d" % i, [128, 1024], BF16) for i in range(2)]
            pf = Ring([ps("pf%d" % i, [128, 512]) for i in range(6)])
            NW, WC = cfg.nwin, cfg.wcap

            wstg_r = Ring([sb("wstg%d" % i, [128, 4096]) for i in range(3)])

            def wdma(dst, name, ex, u):
                dst2 = dst[:, :, :].rearrange("p k n -> p (k n)")
                if isinstance(ex, tuple):
                    j = ex[1] * 4 + u
                    stg = wstg_r.next()
                    self.dma("pool", lambda e: e.indirect_dma_start(out=stg[:, :], out_offset=None, in_=d[name][:, :],
                                                                   in_offset=bass.IndirectOffsetOnAxis(ap=self.slot_idx[:, j:j + 1], axis=0),
                                                                   bounds_check=self.bound_reg(e, NE * 512 - 1), oob_is_err=False),
                             reads=[self.slot_idx], writes=[stg], key=stg)
                    self._cast_i = getattr(self, "_cast_i", 0) + 1
                    if self._cast_i % 2 == 0:
                        return o("act", lambda e: e.activation(out=dst2, in_=stg[:, :], func=AF.Copy), reads=[stg], writes=[dst])
                    return o("dve", lambda e: e.tensor_copy(out=dst2, in_=stg[:, :]), reads=[stg], writes=[dst])
                return self.load("pool", dst, dst2, d[name][(ex * 4 + u) * 128:(ex * 4 + u + 1) * 128, :])

            def expert_body(ex, rbase=None):
                if rbase is not None:
                    ex = ("dyn", ex)
                else:
                    rbase = ex * C
                xbTs = [xbT_r.next() for _ in range(NW)]
                hidTs = [hidT_r.next() for _ in range(NW)]
                for w in range(NW):
                    for b in range(NB):
                        xst = xst_r.next()
                        r0 = rbase + w * WC + b * 128
                        self.load("sp", xst, xst[:, :], d["xs_scr"][r0:r0 + 128, :], reads=[self.dbuf["xs_scr"]])
                        for kc in range(KC):
                            pt = ptrs[kc // 8]
                            o("pe", (lambda kc, pt, xst: lambda e: e.transpose(out=pt[:, (kc % 8) * 128:(kc % 8 + 1) * 128], in_=xst[:, kc * 128:(kc + 1) * 128], identity=self.ident_b[:, :]))(kc, pt, xst),
                              reads=[xst, self.ident_b], writes=[pt])
                        self.evac_mod(ptrs, xbTs[w], b * 128, self.g2eff, self.shift2)
                def fetch13(fu):
                    w1u = w13_r.next(); w3u = w13_r.next()
                    wdma(w1u, "w1", ex, fu)
                    wdma(w3u, "w3", ex, fu)
                    return w1u, w3u

                def fetch2(nu):
                    w2u = w2_r.next()
                    wdma(w2u, "w2", ex, nu)
                    return w2u

                nxt13 = fetch13(0)
                nxt2 = None
                for fu in range(4):
                    w1u, w3u = nxt13
                    if fu + 1 < 4:
                        nxt13 = fetch13(fu + 1)
                    else:
                        nxt2 = fetch2(0)
                    for w in range(NW):
                        xbT = xbTs[w]; hidT = hidTs[w]
                        for fh in range(2):
                            fc = fu * 2 + fh
                            p1 = pf.next(); p3 = pf.next()
                            for wu, pp in ((w1u, p1), (w3u, p3)):
                                for kc in range(KC):
                                    o("pe", (lambda wu, pp, kc, fh, xbT: lambda e: e.matmul(out=pp[:, 0:WC], lhsT=wu[:, kc, fh * 128:(fh + 1) * 128], rhs=xbT[:, kc, :], start=(kc == 0), stop=(kc == KC - 1)))(wu, pp, kc, fh, xbT),
                                      reads=[wu, xbT], writes=[pp])
                            st = st_r.next()
                            o("act", (lambda p1, st: lambda e: e.activation(out=st[:, :], in_=p1[:, 0:WC], func=AF.Silu))(p1, st), reads=[p1], writes=[st])
                            o("dve", (lambda p3, st, fc, hidT: lambda e: e.tensor_tensor(out=hidT[:, fc, :], in0=p3[:, 0:WC], in1=st[:, :], op=ALU.mult))(p3, st, fc, hidT), reads=[p3, st], writes=[hidT])
                for nu in range(4):
                    w2u = nxt2
                    if nu + 1 < 4:
                        nxt2 = fetch2(nu + 1)
                    for w in range(NW):
                        hidT = hidTs[w]
                        for b in range(NB):
                            py = pf.next()
                            for fc in range(FC):
                                o("pe", (lambda py, fc, b, w2u, hidT: lambda e: e.matmul(out=py[:, :], lhsT=hidT[:, fc, b * 128:(b + 1) * 128], rhs=w2u[:, fc, :], start=(fc == 0), stop=(fc == FC - 1)))(py, fc, b, w2u, hidT),
                                  reads=[hidT, w2u], writes=[py])
                            yt = yt_r.next()
                            if (nu * NB + b) % 2 == 0:
                                o("act", (lambda py, yt: lambda e: e.activation(out=yt[:, :], in_=py[:, :], func=AF.Copy))(py, yt), reads=[py], writes=[yt])
                            else:
                                o("dve", (lambda py, yt: lambda e: e.tensor_copy(out=yt[:, :], in_=py[:, :]))(py, yt), reads=[py], writes=[yt])
                            r0 = rbase + w * WC + b * 128
                            self.store("sp", yt, d["ys_scr"][r0:r0 + 128, nu * 512:(nu + 1) * 512], yt[:, :], writes=[self.dbuf["ys_scr"]])

            for ex in range(NE):
                expert_body(ex)
            for s_ in range(cfg.ns):
                expert_body(s_, rbase=(NE + s_) * C)
            self.P.emit()

    def phase4(self):
        nc, d, o, cfg = self.nc, self.dram, self.op, self.cfg
        NM, NE, C = cfg.nmain, cfg.ne, cfg.cap
        with ExitStack() as pes:
            sb = lambda n, sh, dt=F32: T(pes.enter_context(nc.sbuf_tensor("p4_" + n, list(sh), dt)), n)
            gate2 = sb("gate2", [128, D]); fgain = sb("fgain", [128, D])
            self.load("sp", gate2, gate2[:, :], d["gate2_scr"][:, :], reads=[self.dbuf["gate2_scr"]], group="c4")
            self.load("sp", fgain, fgain[:, :], d["fgain_bc"][:, :], group="c4")
            x1_r = Ring([sb("x1_%d" % i, [128, D]) for i in range(2)])
            y0_r = Ring([sb("y0_%d" % i, [128, D]) for i in range(2)])
            y1_r = Ring([sb("y1_%d" % i, [128, D]) for i in range(2)])
            junk = sb("junk", [128, D], BF16)
            smalls = Ring([(sb("ssq%d" % i, [128, 1]), sb("rt%d" % i, [128, 1]), sb("rstd%d" % i, [128, 1])) for i in range(2)])
            for c in range(NM):
                x1 = x1_r.next(); y0 = y0_r.next(); y1 = y1_r.next(); ssq, rt, rstd = smalls.next()
                self.load("sp", x1, x1[:, :], d["x1_scr"][c * 128:(c + 1) * 128, :], reads=[self.x1_bufs[c]])
                for k, y in ((0, y0), (1, y1)):
                    o("pool", (lambda y: lambda e: e.memset(y[:, :], 0.0))(y), writes=[y])
                    self.dma("pool", (lambda k, y, c: lambda e: e.indirect_dma_start(out=y[:, :], out_offset=None, in_=d["ys_scr"][:, :],
                                                                                   in_offset=bass.IndirectOffsetOnAxis(ap=self.dest_all[:, c, k:k + 1], axis=0),
                                                                                   bounds_check=self.bound_reg(e, cfg.rows - 1), oob_is_err=False))(k, y, c),
                             reads=[self.dbuf["ys_scr"], self.dest_bufs[c]], writes=[y], key=y)
                o("dve", (lambda y0, c: lambda e: e.tensor_scalar(out=y0[:, :], in0=y0[:, :], scalar1=self.gw_all[:, c, 0:1], scalar2=None, op0=ALU.mult))(y0, c), reads=[y0, self.gw_bufs[c]], writes=[y0])
                o("dve", (lambda y0, y1, c: lambda e: e.scalar_tensor_tensor(out=y1[:, :], in0=y1[:, :], scalar=self.gw_all[:, c, 1:2], in1=y0[:, :], op0=ALU.mult, op1=ALU.add))(y0, y1, c),
                  reads=[y0, y1, self.gw_bufs[c]], writes=[y1])
                o("pool", (lambda y1: lambda e: e.tensor_tensor(out=y1[:, :], in0=y1[:, :], in1=gate2[:, :], op=ALU.mult))(y1), reads=[y1, gate2], writes=[y1])
                o("dve", (lambda y1, x1: lambda e: e.tensor_tensor(out=x1[:, :], in0=y1[:, :], in1=x1[:, :], op=ALU.add))(y1, x1), reads=[y1, x1], writes=[x1])
                o("act", (lambda x1, ssq: lambda e: e.activation(out=junk[:, :], in_=x1[:, :], func=AF.Square, accum_out=ssq[:, 0:1]))(x1, ssq), reads=[x1], writes=[junk, ssq])
                self.rstd_from_ssq(ssq[:, 0:1], ssq, rt, rstd, 1.0 / D)
                o("dve", (lambda x1, y0, rstd: lambda e: e.scalar_tensor_tensor(out=y0[:, :], in0=x1[:, :], scalar=rstd[:, 0:1], in1=fgain[:, :], op0=ALU.mult, op1=ALU.mult))(x1, y0, rstd),
                  reads=[x1, rstd, fgain], writes=[y0])
                self.store("sp", y0, d["out"][c * 128:(c + 1) * 128, :], y0[:, :])
            self.P.emit()


def make_consts(cfg, first_seg):
    gam = np.array([1.0 - 2.0 ** (-5.0 - h) for h in range(H)], np.float64)
    idx = np.arange(128, dtype=np.float64)
    c = {}
    c["ident_f"] = np.eye(128, dtype=np.float32)
    bands = np.zeros((128, 16, 128), np.float64)
    for g, w in enumerate(POOL_WINDOWS):
        cur = np.zeros((128, 128)); prv = np.zeros((128, 128)); cur0 = np.zeros((128, 128))
        for t in range(128):
            for j in range(w):
                tp = t - j
                if tp >= 0:
                    cur[tp, t] += 1.0 / w
                else:
                    prv[128 + tp, t] += 1.0 / w
            cnt = min(t + 1, w)
            for j in range(cnt):
                cur0[t - j, t] += 1.0 / cnt
            cur[t, t] -= 1.0
            cur0[t, t] -= 1.0
        bands[:, g * 2, :] = cur
        bands[:, g * 2 + 1, :] = prv
        if first_seg:
            bands[:, 8 + g * 2, :] = cur0
        else:
            bands[:, 8 + g * 2, :] = cur
            bands[:, 8 + g * 2 + 1, :] = prv
    c["bands"] = bands.astype(np.float32)
    c["causalT"] = (idx[None, :] >= idx[:, None]).astype(np.float32)
    qs = gam[:, None] ** (idx[None, :] + 1.0)
    ku = gam[:, None] ** (-(idx[None, :] + 1.0)) * (128.0 ** -0.5)
    kd = gam[:, None] ** (127.0 - idx[None, :]) * (128.0 ** -0.5)
    c["qscaleT"] = np.broadcast_to(qs[None], (128, H, 128)).astype(np.float32).copy()
    c["kuscaleT"] = np.broadcast_to(ku[None], (128, H, 128)).astype(np.float32).copy()
    c["kdscale"] = kd.T.astype(np.float32).copy()
    c["tri_u"] = (idx[:, None] < idx[None, :]).astype(np.float32)
    c["iota_p"] = idx.astype(np.float32).reshape(128, 1).copy()
    c["iota_e"] = np.broadcast_to(np.arange(cfg.ne, dtype=np.float32)[None], (128, cfg.ne)).copy()
    invf = (10000.0 ** (-np.arange(64, dtype=np.float32) / np.float32(64))).astype(np.float32)
    c["invf_bc"] = np.broadcast_to(invf[None], (128, 64)).copy()
    return c


def shared_inputs(inp, cfg):
    f = lambda a: np.ascontiguousarray(np.asarray(a), dtype=np.float32)
    col = lambda v: np.ascontiguousarray(f(v).reshape(-1, 128).T)
    bc = lambda v: np.ascontiguousarray(np.broadcast_to(f(v).reshape(1, -1), (128, f(v).size)))
    s = {}
    s["b_ada_bc"] = bc(inp["b_ada"][0])
    s["n1g_fm"] = col(inp["norm1_gain"][0])
    s["n2g_fm"] = col(inp["norm2_gain"][0])
    s["fgain_bc"] = bc(inp["final_gain"])
    s["pscale_fm"] = col(inp["pool_scale"][0])
    s["brt_bc"] = bc(np.concatenate([f(inp["b_group"][0]), f(inp["b_router"][0])]))
    s["w_ada"] = f(inp["w_ada"][0]); s["w_in"] = f(inp["w_in"][0]); s["w_pool"] = f(inp["w_pool"][0])
    s["w_bp"] = f(inp["w_branch_pool"][0]); s["w_br"] = f(inp["w_branch_ret"][0]); s["w_out"] = f(inp["w_out"][0])
    s["w_rt"] = np.ascontiguousarray(np.concatenate([f(inp["w_group"][0]), f(inp["w_router"][0])], axis=1))
    ne = np.asarray(inp["w1"]).shape[1]
    relay = lambda w, k, n: np.ascontiguousarray(f(w).reshape(ne, k, 128, 4, n).transpose(0, 3, 2, 1, 4)).reshape(ne * 512, k * n)
    s["w1"] = relay(inp["w1"][0], KC, 256); s["w3"] = relay(inp["w3"][0], KC, 256); s["w2"] = relay(inp["w2"][0], FC, 512)
    return s


def core_inputs(inp, shared, cfg, b, start):
    NM, NP_ = cfg.nmain, cfg.npre
    x = np.asarray(inp["x"]); pos = np.asarray(inp["positions"])
    m = dict(shared)
    m.update(make_consts(cfg, start == 0))
    m["x_main"] = np.ascontiguousarray(x[b, start:start + NM * 128], dtype=np.float32)
    m["pos_main"] = np.ascontiguousarray(pos[b, start:start + NM * 128].reshape(NM, 128).T.astype(np.int32))
    npre_tok = NP_ * 128
    xp = np.zeros((max(npre_tok, 128), D), np.float32)
    pp = np.zeros((max(npre_tok, 128),), np.int32)
    fl = np.zeros((max(NP_, 1),), np.float32)
    lo = start - npre_tok
    for p in range(NP_):
        t0 = lo + p * 128
        if t0 >= 0:
            xp[p * 128:(p + 1) * 128] = x[b, t0:t0 + 128]
            pp[p * 128:(p + 1) * 128] = pos[b, t0:t0 + 128]
            fl[p] = 1.0
    m["x_pre"] = xp[:max(npre_tok, 128)]
    m["pos_pre"] = np.ascontiguousarray(pp.reshape(-1, 128).T)
    m["flags_pre"] = np.ascontiguousarray(np.broadcast_to(fl[None], (128, fl.size)))
    m["c_col"] = np.ascontiguousarray(np.asarray(inp["c"], dtype=np.float32)[b].reshape(-1, 128).T)
    return m


_NC_CACHE = {}


def get_nc(cfg_key, debug=False):
    key = (cfg_key, debug)
    if key not in _NC_CACHE:
        cfg = Cfg(*cfg_key)
        _NC_CACHE[key] = K(cfg, debug=debug).build()
    return _NC_CACHE[key]


def kernel(**inputs):
    cfg_key = (32, 96, 2, 8, 3, 1)
    cfg = Cfg(*cfg_key)
    nc = get_nc(cfg_key)
    shared = shared_inputs(inputs, cfg)
    B, S = 2, 16384
    seg = cfg.nmain * 128
    in_maps = []
    for core in range(8):
        b, sg = core // 4, core % 4
        in_maps.append(core_inputs(inputs, shared, cfg, b, sg * seg))
    res = run_bass_kernel_spmd(nc, in_maps, core_ids=list(range(8)))
    out = np.empty((B, S, D), np.float32)
    for core in range(8):
        b, sg = core // 4, core % 4
        out[b, sg * seg:(sg + 1) * seg] = np.asarray(res.results[core]["out"])
    return out
```
